# Optimizing a Trainium2 kernel written in Bass

```python
import math
import jax, jax.numpy as jnp
from jax import lax
import numpy as np

D_MODEL = 1024
BATCH = 2
SEQ = 16384
DEPTH = 2

CTX_LEN = 256
GRID_W = 64
HEAD_DIM = 64
ROPE_BASE = 10000.0
NEG_INF = -1e30

A_HEADS = 8
A_KV_HEADS = 2
A_WINDOW = 128
A_BLOCK = 128
B_HEADS = 8
NA_ROWS = 8
NA_COLS = 16
C_HEADS = 8
C_Q_RANK = 384
C_KV_RANK = 256
C_NOPE = 64
C_ROPE = 32
C_V = 64
D_HEADS = 4
D_QK = 64
D_V = 128
Q_BLOCK = 128
N_GROUPS = 4
EXP_PER_GROUP = 8
N_EXPERTS = N_GROUPS * EXP_PER_GROUP
TOP_K = 2
D_EXPERT = 512
MOE_BLOCK = 128

DN_ALPHA = (2 * DEPTH) ** 0.25
DN_BETA = (8 * DEPTH) ** -0.25

A_QW = A_HEADS * HEAD_DIM
A_KVW = A_KV_HEADS * HEAD_DIM
B_W = B_HEADS * HEAD_DIM
AB_SPLITS = (A_QW, A_QW + A_KVW, A_QW + 2 * A_KVW, A_QW + 2 * A_KVW + B_W, A_QW + 2 * A_KVW + 2 * B_W)
AB_IN = A_QW + 2 * A_KVW + 3 * B_W
AB_OUT = A_QW + B_W
D_QW = D_HEADS * 2 * D_QK
D_VW = D_HEADS * D_V
CD_SPLITS = (C_Q_RANK, C_Q_RANK + C_KV_RANK, C_Q_RANK + C_KV_RANK + C_ROPE, C_Q_RANK + C_KV_RANK + C_ROPE + D_QW, C_Q_RANK + C_KV_RANK + C_ROPE + 2 * D_QW)
CD_IN = C_Q_RANK + C_KV_RANK + C_ROPE + 2 * D_QW + D_VW
CD_OUT = C_HEADS * C_V + D_VW
N_EVEN = (DEPTH + 1) // 2
N_ODD = DEPTH // 2

kernel_name = 'hybrid_dit_window_natten_mla_diff_hmoe'


def layer_norm(x, g, b, eps=1e-5):
    xf = x.astype(jnp.float32)
    mu = jnp.mean(xf, -1, keepdims=True)
    var = jnp.mean(jnp.square(xf - mu), -1, keepdims=True)
    return ((xf - mu) * lax.rsqrt(var + eps) * g + b).astype(x.dtype)


def rms_norm(x, g, eps=1e-6):
    xf = x.astype(jnp.float32)
    return (xf * lax.rsqrt(jnp.mean(xf * xf, -1, keepdims=True) + eps) * g).astype(x.dtype)


def axial_rope_tables(n_tok, dim):
    t = jnp.arange(n_tok)
    pos_r = (t // GRID_W).astype(jnp.float32)
    pos_c = (t % GRID_W).astype(jnp.float32)
    quarter = dim // 4
    inv = ROPE_BASE ** (-jnp.arange(quarter, dtype=jnp.float32) / quarter)
    ang_r = pos_r[:, None] * inv
    ang_c = pos_c[:, None] * inv
    ang = jnp.concatenate([ang_r, ang_r, ang_c, ang_c], -1)
    return jnp.cos(ang), jnp.sin(ang)


def apply_rope(x, cos, sin):
    r1, r2, c1, c2 = jnp.split(x, 4, axis=-1)
    rot = jnp.concatenate([-r2, r1, -c2, c1], -1)
    y = x.astype(jnp.float32) * cos[:, None] + rot.astype(jnp.float32) * sin[:, None]
    return y.astype(x.dtype)


def joint_softmax(parts):
    m = parts[0].max(-1, keepdims=True)
    for p in parts[1:]:
        m = jnp.maximum(m, p.max(-1, keepdims=True))
    es = [jnp.exp(p - m) for p in parts]
    denom = sum(e.sum(-1, keepdims=True) for e in es)
    return [e / denom for e in es]


def sweep_query_blocks(fn, q):
    Bn, S = q.shape[0], q.shape[1]
    nblk = S // Q_BLOCK
    qb = jnp.moveaxis(q.reshape((Bn, nblk, Q_BLOCK) + q.shape[2:]), 1, 0)
    out = lax.map(fn, qb)
    return jnp.moveaxis(out, 0, 1).reshape((Bn, S) + out.shape[3:])


def window_gqa(q, k, v, q_c, k_c, v_c, sink, need_ctx):
    Bn, S = q.shape[0], q.shape[1]
    L = k_c.shape[1]
    G = A_HEADS // A_KV_HEADS
    nblk = S // A_BLOCK
    scale = HEAD_DIM ** -0.5
    pad = ((0, 0), (A_BLOCK, A_BLOCK), (0, 0), (0, 0))

    def band(t):
        tb = jnp.pad(t, pad).reshape(Bn, nblk + 2, A_BLOCK, A_KV_HEADS, HEAD_DIM)
        return jnp.concatenate([tb[:, :-2], tb[:, 1:-1], tb[:, 2:]], axis=2)

    kb, vb = band(k), band(v)
    qb = q.reshape(Bn, nblk, A_BLOCK, A_KV_HEADS, G, HEAD_DIM)
    s_loc = jnp.einsum('bnqhgd,bnkhd->bnhgqk', qb, kb).astype(jnp.float32) * scale
    qi = jnp.arange(A_BLOCK)
    kj = jnp.arange(3 * A_BLOCK)
    rel = kj[None, :] - A_BLOCK - qi[:, None]
    kpos = jnp.arange(nblk)[:, None] * A_BLOCK + kj[None, :] - A_BLOCK
    valid = (jnp.abs(rel)[None] <= A_WINDOW) & ((kpos >= 0) & (kpos < S))[:, None, :]
    s_loc = jnp.where(valid[None, :, None, None], s_loc, NEG_INF)
    s_ctx = jnp.einsum('bnqhgd,blhd->bnhgql', qb, k_c).astype(jnp.float32) * scale
    s_sink = jnp.broadcast_to(sink.astype(jnp.float32).reshape(A_KV_HEADS, G, 1, 1), s_ctx.shape[:-1] + (1,))
    _, p_ctx, p_loc = joint_softmax([s_sink, s_ctx, s_loc])
    o = (jnp.einsum('bnhgql,blhd->bnqhgd', p_ctx.astype(v.dtype), v_c)
         + jnp.einsum('bnhgqk,bnkhd->bnqhgd', p_loc.astype(v.dtype), vb))
    o = o.reshape(Bn, S, A_HEADS * HEAD_DIM)
    o_c = None
    if need_ctx:
        qc = q_c.reshape(Bn, L, A_KV_HEADS, G, HEAD_DIM)
        sc = jnp.einsum('blhgd,bmhd->bhglm', qc, k_c).astype(jnp.float32) * scale
        sc_sink = jnp.broadcast_to(sink.astype(jnp.float32).reshape(A_KV_HEADS, G, 1, 1), sc.shape[:-1] + (1,))
        _, pc = joint_softmax([sc_sink, sc])
        o_c = jnp.einsum('bhglm,bmhd->blhgd', pc.astype(v.dtype), v_c).reshape(Bn, L, A_HEADS * HEAD_DIM)
    return o, o_c


def neighbourhood_attn(q, k, v, q_c, k_c, v_c, rpb, need_ctx):
    Bn, S = q.shape[0], q.shape[1]
    L = k_c.shape[1]
    rows = S // GRID_W
    kr = min(NA_ROWS, rows)
    scale = HEAD_DIM ** -0.5
    qg = q.reshape(Bn, rows, GRID_W, B_HEADS, HEAD_DIM)
    r = jnp.arange(rows)
    row_idx = jnp.clip(r - kr // 2, 0, rows - kr)[:, None] + jnp.arange(kr)[None, :]
    kg = k.reshape(Bn, rows, GRID_W, B_HEADS, HEAD_DIM)[:, row_idx]
    vg = v.reshape(Bn, rows, GRID_W, B_HEADS, HEAD_DIM)[:, row_idx]
    s_loc = jnp.einsum('brqhd,brxkhd->brhqxk', qg, kg).astype(jnp.float32) * scale
    col = jnp.arange(GRID_W)
    col_start = jnp.clip(col - NA_COLS // 2, 0, GRID_W - NA_COLS)
    valid = (col[None, :] >= col_start[:, None]) & (col[None, :] < col_start[:, None] + NA_COLS)
    dr = row_idx - r[:, None] + (NA_ROWS - 1)
    dc = jnp.clip(col[None, :] - col[:, None], -(NA_COLS - 1), NA_COLS - 1) + (NA_COLS - 1)
    bias = rpb.astype(jnp.float32)[:, dr][:, :, :, dc]
    s_loc = jnp.where(valid[:, None, :], s_loc + bias.transpose(1, 0, 3, 2, 4), NEG_INF)
    s_loc = s_loc.reshape(Bn, rows, B_HEADS, GRID_W, kr * GRID_W)
    s_ctx = jnp.einsum('brqhd,blhd->brhql', qg, k_c).astype(jnp.float32) * scale
    p_ctx, p_loc = joint_softmax([s_ctx, s_loc])
    p_loc = p_loc.reshape(Bn, rows, B_HEADS, GRID_W, kr, GRID_W)
    o = (jnp.einsum('brhql,blhd->brqhd', p_ctx.astype(v.dtype), v_c)
         + jnp.einsum('brhqxk,brxkhd->brqhd', p_loc.astype(v.dtype), vg))
    o = o.reshape(Bn, S, B_HEADS * HEAD_DIM)
    o_c = None
    if need_ctx:
        sc = jnp.einsum('blhd,bmhd->bhlm', q_c, k_c).astype(jnp.float32) * scale
        pc = jax.nn.softmax(sc, axis=-1).astype(v.dtype)
        o_c = jnp.einsum('bhlm,bmhd->blhd', pc, v_c).reshape(Bn, L, B_HEADS * HEAD_DIM)
    return o, o_c


def mixer_ab(h, hc, w_in, sink, rpb, w_out, cos, sin, need_ctx):
    Bn, S = h.shape[0], h.shape[1]
    L = hc.shape[1]

    def project(t, n):
        aq, ak, av, bq, bk, bv = jnp.split(t @ w_in, AB_SPLITS, axis=-1)
        return (aq.reshape(Bn, n, A_HEADS, HEAD_DIM), ak.reshape(Bn, n, A_KV_HEADS, HEAD_DIM),
                av.reshape(Bn, n, A_KV_HEADS, HEAD_DIM), bq.reshape(Bn, n, B_HEADS, HEAD_DIM),
                bk.reshape(Bn, n, B_HEADS, HEAD_DIM), bv.reshape(Bn, n, B_HEADS, HEAD_DIM))

    aq, ak, av, bq, bk, bv = project(h, S)
    aqc, akc, avc, bqc, bkc, bvc = project(hc, L)
    aq = apply_rope(aq, cos, sin)
    ak = apply_rope(ak, cos, sin)
    oa, oac = window_gqa(aq, ak, av, aqc, akc, avc, sink, need_ctx)
    ob, obc = neighbourhood_attn(bq, bk, bv, bqc, bkc, bvc, rpb, need_ctx)
    y = jnp.concatenate([oa, ob], -1) @ w_out
    yc = jnp.concatenate([oac, obc], -1) @ w_out if need_ctx else None
    return y, yc


def mla_attend(q, k, v):
    s = jnp.einsum('bqhd,bkhd->bhqk', q, k).astype(jnp.float32) * (C_NOPE + C_ROPE) ** -0.5
    p = jax.nn.softmax(s, axis=-1).astype(v.dtype)
    return jnp.einsum('bhqk,bkhd->bqhd', p, v)


def diff_attend(q, k, v, lam):
    s = jnp.einsum('bqhid,bkhid->bhiqk', q, k).astype(jnp.float32) * D_QK ** -0.5
    p = jax.nn.softmax(s, axis=-1)
    pd = (p[:, :, 0] - lam * p[:, :, 1]).astype(v.dtype)
    return jnp.einsum('bhqk,bkhd->bqhd', pd, v)


def mixer_cd(h, hc, w_in, q_norm, w_uq, kv_norm, w_ukv, lam_p, subln, w_out,
             cos32, sin32, cos64, sin64, lam_init, need_ctx):
    Bn, S = h.shape[0], h.shape[1]
    L = hc.shape[1]

    def project(t, n, rope):
        cq, ckv, krope, dq, dk, dv = jnp.split(t @ w_in, CD_SPLITS, axis=-1)
        q = (rms_norm(cq, q_norm) @ w_uq).reshape(Bn, n, C_HEADS, C_NOPE + C_ROPE)
        kv = (rms_norm(ckv, kv_norm) @ w_ukv).reshape(Bn, n, C_HEADS, C_NOPE + C_V)
        q_nope, q_rope = jnp.split(q, [C_NOPE], axis=-1)
        k_nope, cv = jnp.split(kv, [C_NOPE], axis=-1)
        k_rope = krope.reshape(Bn, n, 1, C_ROPE)
        dq = dq.reshape(Bn, n, 2 * D_HEADS, D_QK)
        dk = dk.reshape(Bn, n, 2 * D_HEADS, D_QK)
        if rope:
            q_rope = apply_rope(q_rope, cos32, sin32)
            k_rope = apply_rope(k_rope, cos32, sin32)
            dq = apply_rope(dq, cos64, sin64)
            dk = apply_rope(dk, cos64, sin64)
        cq_full = jnp.concatenate([q_nope, q_rope], -1)
        ck_full = jnp.concatenate([k_nope, jnp.broadcast_to(k_rope, (Bn, n, C_HEADS, C_ROPE))], -1)
        return (cq_full, ck_full, cv, dq.reshape(Bn, n, D_HEADS, 2, D_QK),
                dk.reshape(Bn, n, D_HEADS, 2, D_QK), dv.reshape(Bn, n, D_HEADS, D_V))

    cq, ck, cv, dq, dk, dv = project(h, S, True)
    cqc, ckc, cvc, dqc, dkc, dvc = project(hc, L, False)
    lp = lam_p.astype(jnp.float32)
    lam = jnp.exp(jnp.sum(lp[0] * lp[1])) - jnp.exp(jnp.sum(lp[2] * lp[3])) + lam_init

    ck_all = jnp.concatenate([ckc, ck], 1)
    cv_all = jnp.concatenate([cvc, cv], 1)
    dk_all = jnp.concatenate([dkc, dk], 1)
    dv_all = jnp.concatenate([dvc, dv], 1)
    oc = sweep_query_blocks(lambda qb: mla_attend(qb, ck_all, cv_all), cq)
    od = sweep_query_blocks(lambda qb: diff_attend(qb, dk_all, dv_all, lam), dq)
    od = rms_norm(od, subln) * (1.0 - lam_init)
    y = jnp.concatenate([oc.reshape(Bn, S, C_HEADS * C_V), od.reshape(Bn, S, D_VW)], -1) @ w_out
    yc = None
    if need_ctx:
        occ = mla_attend(cqc, ckc, cvc).reshape(Bn, L, C_HEADS * C_V)
        odc = rms_norm(diff_attend(dqc, dkc, dvc, lam), subln) * (1.0 - lam_init)
        yc = jnp.concatenate([occ, odc.reshape(Bn, L, D_VW)], -1) @ w_out
    return y, yc


def hier_moe(h, w_group, b_group, w_er, b_er, w_gu, w_dn):
    T, D = h.shape
    g_prob = jax.nn.softmax((h @ w_group + b_group).astype(jnp.float32), axis=-1)
    g_val, g_idx = lax.top_k(g_prob, 1)
    g_val, g_idx = g_val[:, 0], g_idx[:, 0]
    e_logits = (h @ w_er + b_er).astype(jnp.float32).reshape(T, N_GROUPS, EXP_PER_GROUP)
    e_prob = jax.nn.softmax(e_logits[jnp.arange(T), g_idx], axis=-1)
    e_val, e_idx = lax.top_k(e_prob, TOP_K)
    wts = g_val[:, None] * e_val / e_val.sum(-1, keepdims=True)
    experts = g_idx[:, None] * EXP_PER_GROUP + e_idx

    A = T * TOP_K
    flat_e = experts.reshape(-1)
    flat_tok = jnp.broadcast_to(jnp.arange(T)[:, None], (T, TOP_K)).reshape(-1)
    flat_w = wts.reshape(-1)
    order = jnp.argsort(flat_e)
    se, stok, sw = flat_e[order], flat_tok[order], flat_w[order]
    counts = jnp.bincount(flat_e, length=N_EXPERTS)
    starts = jnp.cumsum(counts) - counts
    pcounts = ((counts + MOE_BLOCK - 1) // MOE_BLOCK) * MOE_BLOCK
    pends = jnp.cumsum(pcounts)
    pstarts = pends - pcounts
    dest = pstarts[se] + (jnp.arange(A) - starts[se])
    NB = -(-A // MOE_BLOCK) + N_EXPERTS
    R = NB * MOE_BLOCK
    row_tok = jnp.full((R,), T, jnp.int32).at[dest].set(stok)
    row_w = jnp.zeros((R,), h.dtype).at[dest].set(sw.astype(h.dtype))
    block_e = jnp.minimum(jnp.searchsorted(pends, jnp.arange(NB) * MOE_BLOCK, side='right'), N_EXPERTS - 1)
    h_pad = jnp.concatenate([h, jnp.zeros((1, D), h.dtype)], 0)
    xs = h_pad[row_tok].reshape(NB, MOE_BLOCK, D)

    def expert_block(args):
        xb, e = args
        gate, up = jnp.split(xb @ w_gu[e], 2, axis=-1)
        return (jax.nn.silu(gate) * up) @ w_dn[e]

    ys = lax.map(expert_block, (xs, block_e)).reshape(R, D)
    out = jnp.zeros((T + 1, D), h.dtype).at[row_tok].add(ys * row_w[:, None])
    return out[:T]


def setup_inputs(seed: int = 0) -> dict:
    key = jax.random.key(seed)
    ks = jax.random.split(key, 26)
    f32 = jnp.float32
    D = D_MODEL

    def nrm(k, shape, scale):
        return jax.random.normal(k, shape, f32) * scale

    return {
        'x': nrm(ks[0], (BATCH, SEQ, D), 1.0),
        'c': nrm(ks[1], (BATCH, D), 1.0),
        'ctx': nrm(ks[2], (BATCH, CTX_LEN, D), 1.0),
        'c_ctx': nrm(ks[3], (D,), 1.0),
        'w_ada': nrm(ks[4], (DEPTH, D, 6 * D), 0.5 * D ** -0.5),
        'b_ada': nrm(ks[5], (DEPTH, 6 * D), 0.02),
        'ln_g': 1.0 + nrm(ks[6], (DEPTH, 2, D), 0.02),
        'ln_b': nrm(ks[7], (DEPTH, 2, D), 0.02),
        'ab_w_in': nrm(ks[8], (N_EVEN, D, AB_IN), D ** -0.5),
        'a_sink': nrm(ks[9], (N_EVEN, A_HEADS), 0.5),
        'b_rpb': nrm(ks[10], (N_EVEN, B_HEADS, 2 * NA_ROWS - 1, 2 * NA_COLS - 1), 0.1),
        'ab_w_out': nrm(ks[11], (N_EVEN, AB_OUT, D), DN_BETA * AB_OUT ** -0.5),
        'cd_w_in': nrm(ks[12], (N_ODD, D, CD_IN), D ** -0.5),
        'c_q_norm': 1.0 + nrm(ks[13], (N_ODD, C_Q_RANK), 0.02),
        'c_w_uq': nrm(ks[14], (N_ODD, C_Q_RANK, C_HEADS * (C_NOPE + C_ROPE)), C_Q_RANK ** -0.5),
        'c_kv_norm': 1.0 + nrm(ks[15], (N_ODD, C_KV_RANK), 0.02),
        'c_w_ukv': nrm(ks[16], (N_ODD, C_KV_RANK, C_HEADS * (C_NOPE + C_V)), C_KV_RANK ** -0.5),
        'd_lambda': nrm(ks[17], (N_ODD, 4, D_QK), 0.1),
        'd_subln': 1.0 + nrm(ks[18], (N_ODD, D_V), 0.02),
        'cd_w_out': nrm(ks[19], (N_ODD, CD_OUT, D), DN_BETA * CD_OUT ** -0.5),
        'w_group': nrm(ks[20], (DEPTH, D, N_GROUPS), D ** -0.5),
        'b_group': nrm(ks[21], (DEPTH, N_GROUPS), 0.01),
        'w_exp_router': nrm(ks[22], (DEPTH, D, N_EXPERTS), D ** -0.5),
        'b_exp_router': nrm(ks[23], (DEPTH, N_EXPERTS), 0.01),
        'w_gate_up': nrm(ks[24], (DEPTH, N_EXPERTS, D, 2 * D_EXPERT), D ** -0.5),
        'w_down': nrm(ks[25], (DEPTH, N_EXPERTS, D_EXPERT, D), DN_BETA * D_EXPERT ** -0.5),
    }


def reference(x, c, ctx, c_ctx, w_ada, b_ada, ln_g, ln_b, ab_w_in, a_sink, b_rpb, ab_w_out,
              cd_w_in, c_q_norm, c_w_uq, c_kv_norm, c_w_ukv, d_lambda, d_subln, cd_w_out,
              w_group, b_group, w_exp_router, b_exp_router, w_gate_up, w_down):
    Bn, S, D = x.shape
    L = ctx.shape[1]
    cos64, sin64 = axial_rope_tables(S, HEAD_DIM)
    cos32, sin32 = axial_rope_tables(S, C_ROPE)
    for l in range(DEPTH):
        need_ctx = l < DEPTH - 1
        mod = jax.nn.silu(c) @ w_ada[l] + b_ada[l]
        mod_c = jax.nn.silu(c_ctx) @ w_ada[l] + b_ada[l]
        sh1, sc1, g1, sh2, sc2, g2 = [m[:, None, :] for m in jnp.split(mod, 6, axis=-1)]
        sh1c, sc1c, g1c, sh2c, sc2c, g2c = jnp.split(mod_c, 6, axis=-1)

        h = x * (1.0 + sc1) + sh1
        hc = ctx * (1.0 + sc1c) + sh1c
        if l % 2 == 0:
            e = l // 2
            y, yc = mixer_ab(h, hc, ab_w_in[e], a_sink[e], b_rpb[e], ab_w_out[e], cos64, sin64, need_ctx)
        else:
            o = l // 2
            lam_init = 0.8 - 0.6 * math.exp(-0.3 * l)
            y, yc = mixer_cd(h, hc, cd_w_in[o], c_q_norm[o], c_w_uq[o], c_kv_norm[o], c_w_ukv[o],
                             d_lambda[o], d_subln[o], cd_w_out[o], cos32, sin32, cos64, sin64,
                             lam_init, need_ctx)
        x = layer_norm(DN_ALPHA * x + g1 * y, ln_g[l, 0], ln_b[l, 0])
        if need_ctx:
            ctx = layer_norm(DN_ALPHA * ctx + g1c * yc, ln_g[l, 0], ln_b[l, 0])

        h = (x * (1.0 + sc2) + sh2).reshape(Bn * S, D)
        if need_ctx:
            hc = (ctx * (1.0 + sc2c) + sh2c).reshape(Bn * L, D)
            h = jnp.concatenate([h, hc], 0)
        y_all = hier_moe(h, w_group[l], b_group[l], w_exp_router[l], b_exp_router[l], w_gate_up[l], w_down[l])
        y = y_all[:Bn * S].reshape(Bn, S, D)
        x = layer_norm(DN_ALPHA * x + g2 * y, ln_g[l, 1], ln_b[l, 1])
        if need_ctx:
            yc = y_all[Bn * S:].reshape(Bn, L, D)
            ctx = layer_norm(DN_ALPHA * ctx + g2c * yc, ln_g[l, 1], ln_b[l, 1])
    return x
```

```python
import math
from contextlib import ExitStack
import numpy as np
import concourse.bass as bass
import concourse.mybir as mybir
from concourse.bass_utils import run_bass_kernel_spmd

F32 = mybir.dt.float32
BF16 = mybir.dt.bfloat16
ALU = mybir.AluOpType
AF = mybir.ActivationFunctionType
AX = mybir.AxisListType

D = 1024
S = 16384
L = 256
DEPTH = 2
ALPHA = (2 * DEPTH) ** 0.25
LN_EPS = 1e-5 / (ALPHA * ALPHA)
NEG = -30000.0
LAM_INIT = 0.8 - 0.6 * math.exp(-0.3 * 1)
NEXT = 4608


class H:
    __slots__ = ("w", "r")

    def __init__(self):
        self.w = None
        self.r = []


class T:
    def __init__(self, ap):
        self.ap = ap
        self.h = H()


class Prog:
    NDMA = 8

    def __init__(self, nc):
        self.nc = nc
        self.eng = {"pe": nc.tensor, "act": nc.scalar, "dve": nc.vector,
                    "pool": nc.gpsimd, "sp": nc.sync}
        self.sem = {}
        self.cnt = {}
        for k in ("pe", "act", "dve", "pool"):
            self.sem[k] = nc.alloc_semaphore("s_" + k)
            self.cnt[k] = 0
        self.seen = {k: {} for k in self.eng}
        self.dma_i = {}
        self.dma_sems = {}
        for q in ("sp", "pool", "act"):
            self.dma_i[q] = 0
            self.dma_sems[q] = []
            for i in range(self.NDMA):
                key = ("dma", q, i)
                self.sem[key] = nc.alloc_semaphore("d_%s_%d" % (q, i))
                self.cnt[key] = 0
                self.dma_sems[q].append(key)
        self.n_ins = 0

    def _wait(self, e, key, val):
        if self.seen[e].get(key, 0) < val:
            self.eng[e].wait_ge(self.sem[key], val)
            self.seen[e][key] = val

    def _deps(self, e, reads, writes, skip_own_waw=False):
        deps = {}
        for h in reads:
            if h.w is not None:
                k, v = h.w
                if deps.get(k, 0) < v:
                    deps[k] = v
        for h in writes:
            if h.w is not None:
                k, v = h.w
                if not (skip_own_waw and k == e):
                    if deps.get(k, 0) < v:
                        deps[k] = v
            for (k, v) in h.r:
                if deps.get(k, 0) < v:
                    deps[k] = v
        for k, v in deps.items():
            self._wait(e, k, v)

    def _mark(self, tok, reads, writes):
        for h in reads:
            if len(h.r) > 16:
                d = {}
                for (k, v) in h.r:
                    if d.get(k, 0) < v:
                        d[k] = v
                h.r = list(d.items())
            h.r.append(tok)
        for h in writes:
            h.w = tok
            h.r = []

    def op(self, e, fn, reads=(), writes=(), inc=True, acc=False):
        self._deps(e, reads, writes, skip_own_waw=acc)
        ins = fn(self.eng[e])
        self.n_ins += 1
        if inc:
            self.cnt[e] += 1
            ins.then_inc(self.sem[e], 1)
            tok = (e, self.cnt[e])
        else:
            tok = (e, self.cnt[e] + 1)
        self._mark(tok, reads, writes)
        return tok

    def dma(self, q, out, in_, reads=(), writes=()):
        self._deps(q, reads, writes)
        i = self.dma_i[q]
        self.dma_i[q] += 1
        key = self.dma_sems[q][i % self.NDMA]
        if self.cnt[key] > 0:
            self._wait(q, key, self.cnt[key])
        self.cnt[key] += 16
        self.eng[q].dma_start(out=out, in_=in_).then_inc(self.sem[key], 16)
        self.n_ins += 1
        tok = (key, self.cnt[key])
        self._mark(tok, reads, writes)
        return tok

    def barrier(self):
        for e in self.eng:
            for k, v in self.cnt.items():
                if v > 0:
                    self._wait(e, k, v)


class KB:
    def __init__(self):
        self.nc = bass.Bass("TRN2", target_bir_lowering=False)
        self.P = Prog(self.nc)
        nc = self.nc
        psall = nc.alloc_psum_tensor("psall", [128, 4096], F32).ap()
        self.psall = psall
        self.ps = [T(psall[:, i * 512:(i + 1) * 512]) for i in range(8)]
        self.stack = None
        self.nm = 0
        self.ones_f = self.gsb([128, 128], F32)
        self.ones_b = self.gsb([128, 128], BF16)
        self.op("dve", lambda e: e.memset(self.ones_f.ap, 1.0), [], [self.ones_f])
        self.op("dve", lambda e: e.memset(self.ones_b.ap, 1.0), [], [self.ones_b])

    def name(self, p="t"):
        self.nm += 1
        return "%s%d" % (p, self.nm)

    def gsb(self, shape, dt):
        return T(self.nc.alloc_sbuf_tensor(self.name("g"), list(shape), dt).ap())

    def sb(self, shape, dt):
        t = self.stack.enter_context(self.nc.sbuf_tensor(self.name("s"), list(shape), dt))
        return T(t.ap())

    def din(self, name, shape, dt=F32):
        return self.nc.dram_tensor(name, list(shape), dt, kind="ExternalInput").ap()

    def dout(self, name, shape, dt=F32):
        return self.nc.dram_tensor(name, list(shape), dt, kind="ExternalOutput").ap()

    def dscr(self, name, shape, dt):
        return T(self.nc.dram_tensor(name, list(shape), dt, kind="Internal").ap())

    def op(self, e, fn, reads, writes, inc=True, acc=False):
        return self.P.op(e, fn, [t.h for t in reads], [t.h for t in writes], inc=inc, acc=acc)

    def dma(self, q, out, in_, reads, writes):
        return self.P.dma(q, out, in_, [t.h for t in reads], [t.h for t in writes])

    def mm(self, o, o_ap, l, l_ap, r, r_ap, start=True, stop=True, acc=None, skip=False, inc=None):
        if acc is None:
            acc = not start
        if inc is None:
            inc = stop
        self.op("pe", lambda e: e.matmul(o_ap, lhsT=l_ap, rhs=r_ap, start=start, stop=stop, skip_group_check=skip),
                [l, r], [o], inc=inc, acc=acc)

    def tt(self, eng, o, o_ap, a, a_ap, b, b_ap, op):
        self.op(eng, lambda e: e.tensor_tensor(out=o_ap, in0=a_ap, in1=b_ap, op=op), [a, b], [o])

    def stt(self, eng, o, o_ap, a, a_ap, sc, b, b_ap, op0, op1, extra=()):
        self.op(eng, lambda e: e.scalar_tensor_tensor(out=o_ap, in0=a_ap, scalar=sc, in1=b_ap, op0=op0, op1=op1),
                [a, b] + list(extra), [o])

    def ts(self, eng, o, o_ap, a, a_ap, s1, s2, op0, op1=None, extra=()):
        if op1 is None:
            self.op(eng, lambda e: e.tensor_scalar(out=o_ap, in0=a_ap, scalar1=s1, scalar2=None, op0=op0),
                    [a] + list(extra), [o])
        else:
            self.op(eng, lambda e: e.tensor_scalar(out=o_ap, in0=a_ap, scalar1=s1, scalar2=s2, op0=op0, op1=op1),
                    [a] + list(extra), [o])

    def act(self, o, o_ap, a, a_ap, func, scale=1.0, bias=None, extra=()):
        if bias is None:
            self.op("act", lambda e: e.activation(out=o_ap, in_=a_ap, func=func, scale=scale),
                    [a] + list(extra), [o])
        else:
            self.op("act", lambda e: e.activation(out=o_ap, in_=a_ap, func=func, scale=scale, bias=bias),
                    [a] + list(extra), [o])

    def cp(self, eng, o, o_ap, a, a_ap):
        if eng == "act":
            self.op("act", lambda e: e.copy(out=o_ap, in_=a_ap), [a], [o])
        else:
            self.op(eng, lambda e: e.tensor_copy(out=o_ap, in_=a_ap), [a], [o])

    def memset(self, eng, o, o_ap, v):
        self.op(eng, lambda e: e.memset(o_ap, v), [], [o])

    def rstd(self, o, o_ap, a, a_ap, scale, eps, epsT):
        self.act(o, o_ap, a, a_ap, AF.Ln, scale=scale, bias=epsT.ap[0:o_ap.shape[0] + o_ap.base_partition(), 0:1][o_ap.base_partition():, :], extra=[epsT])
        self.act(o, o_ap, o, o_ap, AF.Exp, scale=-0.5)


def phase_mod(kb, w_ada, b_adaT, cT):
    modT = kb.gsb([128, 48, 2], F32)
    with ExitStack() as st:
        kb.stack = st
        cs = kb.sb([128, 8, 2], F32)
        kb.dma("sp", cs.ap, cT, [], [cs])
        kb.act(cs, cs.ap, cs, cs.ap, AF.Silu)
        bT = kb.sb([128, 48], F32)
        kb.dma("sp", bT.ap, b_adaT, [], [bT])
        wv = w_ada.rearrange("(kc p) f -> p kc f", p=128)
        bufs = [kb.sb([128, 8, 512], F32) for _ in range(2)]
        ps = kb.ps[0]
        for pc in range(12):
            wb = bufs[pc % 2]
            kb.dma("sp", wb.ap, wv[:, :, pc * 512:(pc + 1) * 512], [], [wb])
            for fc in range(4):
                ch = pc * 4 + fc
                for kc in range(8):
                    kb.mm(ps, ps.ap[:, 2 * ch:2 * ch + 2], wb, wb.ap[:, kc, fc * 128:(fc + 1) * 128],
                          cs, cs.ap[:, kc, :], start=(kc == 0), stop=(kc == 7), acc=(not (pc == 0 and fc == 0 and kc == 0)))
        pv = ps.ap[:, 0:96].rearrange("p (c j) -> p c j", j=2)
        kb.tt("dve", modT, modT.ap, ps, pv, bT, bT.ap.unsqueeze(2).to_broadcast([128, 48, 2]), ALU.add)
        for w in (1, 4):
            kb.ts("dve", modT, modT.ap[:, w * 8:(w + 1) * 8, :], modT, modT.ap[:, w * 8:(w + 1) * 8, :], 1.0, None, ALU.add)
        for w in (2, 5):
            kb.ts("dve", modT, modT.ap[:, w * 8:(w + 1) * 8, :], modT, modT.ap[:, w * 8:(w + 1) * 8, :], 1.0 / ALPHA, None, ALU.mult)
        kb.P.barrier()
    return modT


def load_ln(kb, ln_gT, ln_bT):
    g = kb.gsb([128, 2, 8], F32)
    b = kb.gsb([128, 2, 8], F32)
    kb.dma("sp", g.ap, ln_gT, [], [g])
    kb.dma("sp", b.ap, ln_bT, [], [b])
    return g, b


class LNCtx:
    def __init__(self, kb):
        self.z = kb.sb([128, 8, 128], F32)
        self.zsq = kb.sb([128, 8, 128], F32)
        self.xo = [kb.sb([128, 8, 128], F32) for _ in range(2)]
        self.m = kb.sb([128, 128], F32)
        self.msq = kb.sb([128, 128], F32)
        self.var = kb.sb([128, 128], F32)
        self.eps = kb.sb([128, 1], F32)
        kb.memset("dve", self.eps, self.eps.ap, LN_EPS)
        self.i = 0


def ln_epilogue(kb, lc, y_fn, xres_ap, xres_reads, gmod_ap, modT, lng_ap, lnb_ap, lnp, out_ap, out_T, stat_ps):
    z, zsq = lc.z, lc.zsq
    kb.dma("sp", z.ap, xres_ap, xres_reads, [z])
    for dc in range(8):
        yt, yap = y_fn(dc)
        kb.stt("dve", z, z.ap[:, dc, :], yt, yap, gmod_ap[:, dc, :], z, z.ap[:, dc, :], ALU.mult, ALU.add, extra=[modT])
    kb.tt("pool", zsq, zsq.ap, z, z.ap, z, z.ap, ALU.mult)
    for dc in range(8):
        kb.mm(stat_ps, stat_ps.ap[:, 0:128], kb.ones_f, kb.ones_f.ap, z, z.ap[:, dc, :], start=(dc == 0), stop=(dc == 7))
    for dc in range(8):
        kb.mm(stat_ps, stat_ps.ap[:, 128:256], kb.ones_f, kb.ones_f.ap, zsq, zsq.ap[:, dc, :], start=(dc == 0), stop=(dc == 7), acc=True)
    m, msq, var = lc.m, lc.msq, lc.var
    kb.act(m, m.ap, stat_ps, stat_ps.ap[:, 0:128], AF.Copy, scale=1.0 / D)
    kb.tt("dve", msq, msq.ap, m, m.ap, m, m.ap, ALU.mult)
    kb.stt("dve", var, var.ap, stat_ps, stat_ps.ap[:, 128:256], 1.0 / D, msq, msq.ap, ALU.mult, ALU.subtract)
    kb.act(var, var.ap, var, var.ap, AF.Ln, scale=1.0, bias=lc.eps.ap[:, 0:1], extra=[lc.eps])
    kb.act(var, var.ap, var, var.ap, AF.Exp, scale=-0.5)
    xo = lc.xo[lc.i % 2]
    lc.i += 1
    kb.tt("dve", z, z.ap, z, z.ap, m, m.ap.unsqueeze(1).to_broadcast([128, 8, 128]), ALU.subtract)
    kb.tt("pool", z, z.ap, z, z.ap, var, var.ap.unsqueeze(1).to_broadcast([128, 8, 128]), ALU.mult)
    kb.tt("dve", z, z.ap, z, z.ap, lnp[0], lng_ap.unsqueeze(2).to_broadcast([128, 8, 128]), ALU.mult)
    kb.tt("pool", xo, xo.ap, z, z.ap, lnp[1], lnb_ap.unsqueeze(2).to_broadcast([128, 8, 128]), ALU.add)
    kb.dma("pool", out_ap, xo.ap, [xo], [out_T])


class AttCtx:
    def __init__(self, kb, s_banks, o_banks, bc_bank):
        self.sb_ = [kb.sb([128, 512], F32) for _ in range(2)]
        self.E = [kb.sb([128, 512], BF16) for _ in range(3)]
        self.zr = kb.sb([128, 512], F32)
        self.bcs = kb.sb([64, 512], F32)
        self.s_banks = s_banks
        self.o_banks = o_banks
        self.bc = bc_bank
        self.si = 0
        self.ei = 0
        self.oi = 0
        self.bi = 0


def attn_unit(kb, ac, tiles, scale, out_T, out_ap, ncols=512, sink=None):
    ob = ac.o_banks[ac.oi % len(ac.o_banks)]
    ac.oi += 1
    nt = len(tiles)
    for ti, tl in enumerate(tiles):
        sbk = ac.s_banks[ac.si % len(ac.s_banks)]
        ac.si += 1
        first = True
        for (kT, kap, qT, qap, c0, n) in tl["qk"]:
            kb.mm(sbk, sbk.ap[:, c0:c0 + n], kT, kap, qT, qap, start=True, stop=True, acc=(not first))
            first = False
        E = ac.E[ac.ei % 3]
        ac.ei += 1
        if tl["bias"] is not None:
            bT, bap = tl["bias"]
            sbuf = ac.sb_[ac.bi % 2]
            ac.bi += 1
            kb.stt("dve", sbuf, sbuf.ap[:, 0:ncols], sbk, sbk.ap[:, 0:ncols], scale, bT, bap, ALU.mult, ALU.add)
            kb.act(E, E.ap[:, 0:ncols], sbuf, sbuf.ap[:, 0:ncols], AF.Exp)
        else:
            kb.act(E, E.ap[:, 0:ncols], sbk, sbk.ap[:, 0:ncols], AF.Exp, scale=scale)
        last = (ti == nt - 1) and sink is None
        for pi, (vT, vap, c0, n) in enumerate(tl["pv"]):
            kb.mm(ob, ob.ap[0:65, c0:c0 + n], vT, vap, E, E.ap[:, c0:c0 + n], start=(ti == 0 and pi == 0), stop=last,
                  acc=not (ti == 0 and pi == 0), skip=True, inc=last)
    if sink is not None:
        e64, srow_T, srow_ap = sink
        kb.mm(ob, ob.ap[0:65, 0:ncols], e64, e64.ap, srow_T, srow_ap, start=False, stop=True, acc=True)
    zr = ac.zr
    kb.op("dve", lambda e: e.reciprocal(out=zr.ap[64:65, 0:ncols], in_=ob.ap[64:65, 0:ncols]), [ob], [zr])
    bc = ac.bc
    kb.mm(bc, bc.ap[0:64, 0:ncols], kb.ones_f, kb.ones_f.ap[64:65, 0:64], zr, zr.ap[64:65, 0:ncols])
    kb.cp("act", ac.bcs, ac.bcs.ap[:, 0:ncols], bc, bc.ap[0:64, 0:ncols])
    kb.tt("dve", out_T, out_ap, ob, ob.ap[0:64, 0:ncols], ac.bcs, ac.bcs.ap[:, 0:ncols], ALU.mult)


def phase_l0_attn(kb, d, modT, lng, lnb, x1T, with_ctx=True):
    P = kb.P
    with ExitStack() as st:
        kb.stack = st
        wA = kb.sb([128, 8, 640], BF16)
        wArot = kb.sb([128, 8, 640], BF16)
        wR = kb.sb([128, 8, 1664], BF16)
        wout = kb.sb([64, 16, 1024], BF16)
        win = d["ab_w_in"].rearrange("(kc p) c -> p kc c", p=128)
        for slot, hd in enumerate([0, 4, 1, 5, 2, 6, 3, 7]):
            kb.dma("pool", wA.ap[:, :, slot * 64:(slot + 1) * 64], win[:, :, hd * 64:(hd + 1) * 64], [], [wA])
        kb.dma("pool", wA.ap[:, :, 512:640], win[:, :, 512:640], [], [wA])
        kb.dma("pool", wR.ap[:, :, 0:1024], win[:, :, 768:1792], [], [wR])
        kb.dma("pool", wR.ap[:, :, 1024:1152], win[:, :, 640:768], [], [wR])
        kb.dma("pool", wR.ap[:, :, 1152:1664], win[:, :, 1792:2304], [], [wR])
        kb.dma("pool", wout.ap, d["ab_w_out"].rearrange("(h p) c -> p h c", p=64), [], [wout])
        src = wA.ap.rearrange("p k (g two s) -> p (k g) two s", two=2, s=16)
        dst = wArot.ap.rearrange("p k (g two s) -> p (k g) two s", two=2, s=16)
        kb.ts("dve", wArot, dst[:, :, 0, :], wA, src[:, :, 1, :], -1.0, None, ALU.mult)
        kb.cp("dve", wArot, dst[:, :, 1, :], wA, src[:, :, 0, :])
        biasG = kb.sb([128, 5, 8, 128], BF16)
        biasE = kb.sb([128, 5, 8, 128], BF16)
        kb.dma("pool", biasG.ap, d["biasB"][:, 2], [], [biasG])
        maskA = kb.sb([128, 4, 128], BF16)
        kb.dma("pool", maskA.ap, d["maskA"], [], [maskA])
        sinkrow = kb.sb([1, 8, 128], F32)
        sk = kb.sb([1, 8], F32)
        kb.dma("sp", sk.ap, d["a_sink"], [], [sk])
        kb.act(sk, sk.ap, sk, sk.ap, AF.Exp)
        kb.cp("dve", sinkrow, sinkrow.ap, sk, sk.ap.unsqueeze(2).to_broadcast([1, 8, 128]))
        e64 = kb.sb([1, 65], F32)
        kb.memset("dve", e64, e64.ap, 0.0)
        kb.memset("dve", e64, e64.ap[:, 64:65], 1.0)
        def mkslot():
            s = dict(q=kb.sb([128, 8, 128], BF16), k=kb.sb([128, 5, 128], BF16), v=kb.sb([128, 10, 66], BF16))
            kb.memset("pool", s["v"], s["v"].ap[:, :, 64:65], 1.0)
            return s
        ring = [mkslot() for _ in range(6)]
        cslots = [mkslot() for _ in range(2)]
        xb = [kb.sb([128, 8, 128], F32) for _ in range(2)]
        hTs = [kb.sb([128, 8, 128], BF16) for _ in range(2)]
        tmpf = kb.sb([128, 8, 128], F32)
        ropeT = [kb.sb([128, 2, 128], F32) for _ in range(2)]
        r1 = kb.sb([128, 4, 128], F32)
        r2 = kb.sb([128, 4, 128], F32)
        oT = kb.sb([64, 16, 128], BF16)
        lc = LNCtx(kb)
        ac = AttCtx(kb, [kb.ps[3], kb.ps[4]], [kb.ps[5], kb.ps[6]], kb.ps[7])
        ps0, ps1, ps2 = kb.ps[0], kb.ps[1], kb.ps[2]
        xTv = d["xT"].rearrange("(kc p) t -> p kc t", p=128)
        cTv = d["ctxT"].rearrange("(kc p) t -> p kc t", p=128)
        x1v = x1T.ap.rearrange("(kc p) t -> p kc t", p=128)
        cnt = [0]
        import os
        LIM = int(os.environ.get("LIM", "99"))

        def project(src_ap, col, slot, rope_t):
            i = cnt[0]
            cnt[0] += 1
            xt = xb[i % 2]
            hT = hTs[i % 2]
            kb.dma("sp", xt.ap, src_ap, [], [xt])
            s_b = modT.ap[:, 8:16, col:col + 1].to_broadcast([128, 8, 128])
            sh_b = modT.ap[:, 0:8, col:col + 1].to_broadcast([128, 8, 128])
            kb.tt("dve", tmpf, tmpf.ap, xt, xt.ap, modT, s_b, ALU.mult)
            kb.tt("pool", hT, hT.ap, tmpf, tmpf.ap, modT, sh_b, ALU.add)
            rope = rope_t is not None
            if rope:
                rt = ropeT[i % 2]
                kb.dma("sp", rt.ap, d["ropeT"][:, :, rope_t * 128:(rope_t + 1) * 128], [], [rt])
            for c in range(4):
                for kc in range(8):
                    kb.mm(ps0, ps0.ap[:, c * 128:(c + 1) * 128], wA, wA.ap[:, kc, c * 128:(c + 1) * 128], hT, hT.ap[:, kc, :],
                          start=(kc == 0), stop=(kc == 7), acc=not (c == 0 and kc == 0))
            if rope:
                for c in range(4):
                    for kc in range(8):
                        kb.mm(ps1, ps1.ap[:, c * 128:(c + 1) * 128], wArot, wArot.ap[:, kc, c * 128:(c + 1) * 128], hT, hT.ap[:, kc, :],
                              start=(kc == 0), stop=(kc == 7), acc=not (c == 0 and kc == 0))
            for kc in range(8):
                kb.mm(ps2, ps2.ap[:, 0:128], wA, wA.ap[:, kc, 512:640], hT, hT.ap[:, kc, :], start=(kc == 0), stop=(kc == 7), acc=(kc != 0))
            if rope:
                for kc in range(8):
                    kb.mm(ps2, ps2.ap[:, 128:256], wArot, wArot.ap[:, kc, 512:640], hT, hT.ap[:, kc, :], start=(kc == 0), stop=(kc == 7), acc=True)
            for kc in range(8):
                kb.mm(ps2, ps2.ap[:, 256:384], hT, hT.ap[:, kc, :], wR, wR.ap[:, kc, 1024:1152], start=(kc == 0), stop=(kc == 7), acc=True)
            q, k, v = slot["q"], slot["k"], slot["v"]
            if rope:
                cosb = rt.ap[:, 0:1, :].to_broadcast([128, 4, 128])
                sinb = rt.ap[:, 1:2, :].to_broadcast([128, 4, 128])
                p0v = ps0.ap.rearrange("p (c t) -> p c t", t=128)
                p1v = ps1.ap.rearrange("p (c t) -> p c t", t=128)
                kb.tt("dve", r1, r1.ap, ps0, p0v, rt, cosb, ALU.mult)
                kb.tt("dve", r2, r2.ap, ps1, p1v, rt, sinb, ALU.mult)
                kb.tt("pool", q, q.ap[:, 0:4, :], r1, r1.ap, r2, r2.ap, ALU.add)
                kb.tt("dve", r1, r1.ap[:, 0, :], ps2, ps2.ap[:, 0:128], rt, rt.ap[:, 0, :], ALU.mult)
                kb.tt("dve", r2, r2.ap[:, 0, :], ps2, ps2.ap[:, 128:256], rt, rt.ap[:, 1, :], ALU.mult)
                kb.tt("pool", k, k.ap[:, 0, :], r1, r1.ap[:, 0, :], r2, r2.ap[:, 0, :], ALU.add)
            else:
                kb.cp("act", q, q.ap[:, 0:4, :], ps0, ps0.ap.rearrange("p (c t) -> p c t", t=128))
                kb.cp("act", k, k.ap[:, 0, :], ps2, ps2.ap[:, 0:128])
            kb.cp("act", v, v.ap[:, 0:2, 0:64], ps2, ps2.ap[:, 256:384].rearrange("p (h e) -> p h e", e=64))
            for c in range(4):
                for kc in range(8):
                    kb.mm(ps0, ps0.ap[:, c * 128:(c + 1) * 128], wR, wR.ap[:, kc, c * 128:(c + 1) * 128], hT, hT.ap[:, kc, :],
                          start=(kc == 0), stop=(kc == 7), acc=not (c == 0 and kc == 0))
            kb.cp("act", q, q.ap[:, 4:8, :], ps0, ps0.ap.rearrange("p (c t) -> p c t", t=128))
            for c in range(4):
                for kc in range(8):
                    kb.mm(ps1, ps1.ap[:, c * 128:(c + 1) * 128], wR, wR.ap[:, kc, 512 + c * 128:512 + (c + 1) * 128], hT, hT.ap[:, kc, :],
                          start=(kc == 0), stop=(kc == 7), acc=not (c == 0 and kc == 0))
            kb.cp("act", k, k.ap[:, 1:5, :], ps1, ps1.ap.rearrange("p (c t) -> p c t", t=128))
            for kc in range(8):
                kb.mm(ps2, ps2.ap, hT, hT.ap[:, kc, :], wR, wR.ap[:, kc, 1152:1664], start=(kc == 0), stop=(kc == 7), acc=(kc != 0))
            kb.cp("act", v, v.ap[:, 2:10, 0:64], ps2, ps2.ap.rearrange("p (h e) -> p h e", e=64))

        def attend(qslot, loc_tiles, bias_tab, mask_prev, mask_next, xres_ap, col, out_col):
            q = qslot["q"]
            for g in range(2):
                pb = 64 * g
                tiles = []
                for cs_ in cslots:
                    tiles.append(dict(qk=[(cs_["k"], cs_["k"].ap[pb:pb + 64, 0, :], q, q.ap[pb:pb + 64, 0:4, :], 0, 512)],
                                      bias=None, pv=[(cs_["v"], cs_["v"].ap[:, g, 0:65], 0, 512)]))
                if loc_tiles is not None:
                    for idx, mk in ((1, mask_prev), (2, None), (3, mask_next)):
                        sl = loc_tiles[idx]
                        bias = None
                        if mk is not None:
                            bias = (maskA, maskA.ap[:, mk:mk + 1, :].to_broadcast([128, 4, 128]))
                        tiles.append(dict(qk=[(sl["k"], sl["k"].ap[pb:pb + 64, 0, :], q, q.ap[pb:pb + 64, 0:4, :], 0, 512)],
                                          bias=bias, pv=[(sl["v"], sl["v"].ap[:, g, 0:65], 0, 512)]))
                for tl in tiles:
                    if tl["bias"] is not None:
                        tl["bias"] = (tl["bias"][0], tl["bias"][1])
                attn_unit_l0(kb, ac, tiles, 0.125, oT, oT.ap[:, 4 * g:4 * g + 4, :],
                             sink=(e64, sinkrow, sinkrow.ap[0:1, 4 * g:4 * g + 4, :]))
            if LIM == 4:
                return
            for u in range(2):
                tiles = []
                srcs = [(cs_, None) for cs_ in cslots]
                if loc_tiles is not None:
                    srcs += [(loc_tiles[t], t) for t in range(5)]
                for (sl, t) in srcs:
                    qk = []
                    pv = []
                    for hh in range(4):
                        h = 4 * u + hh
                        pb = 64 * (h % 2)
                        qk.append((sl["k"], sl["k"].ap[pb:pb + 64, 1 + h // 2, :], q, q.ap[pb:pb + 64, 4 + h // 2, :], hh * 128, 128))
                        pv.append((sl["v"], sl["v"].ap[:, 2 + h, 0:65], hh * 128, 128))
                    bias = None
                    if t is not None:
                        bias = (bias_tab, bias_tab.ap[:, t, 4 * u:4 * u + 4, :].rearrange("p (a two) q -> p two a q", two=2))
                    tiles.append(dict(qk=qk, bias=bias, pv=pv))
                attn_unit_b(kb, ac, tiles, 0.125, oT, oT.ap[:, 8 + 4 * u:8 + 4 * u + 4, :])
            if LIM == 5:
                return
            for dc in range(8):
                pb_ = ps0 if dc < 4 else ps1
                c0 = (dc % 4) * 128
                for h in range(16):
                    kb.mm(pb_, pb_.ap[:, c0:c0 + 128], wout, wout.ap[:, h, dc * 128:(dc + 1) * 128], oT, oT.ap[:, h, :],
                          start=(h == 0), stop=(h == 15), acc=not (dc % 4 == 0 and h == 0))

            if LIM == 6:
                return

            def y_fn(dc):
                pb_ = ps0 if dc < 4 else ps1
                return pb_, pb_.ap[:, (dc % 4) * 128:(dc % 4 + 1) * 128]
            ln_epilogue(kb, lc, y_fn, xres_ap, [], modT.ap[:, 16:24, col:col + 1], modT,
                        lng.ap[:, 0, :], lnb.ap[:, 0, :], (lng, lnb), x1v[:, :, out_col:out_col + 128], x1T, kb.ps[7])

        def attn_unit_l0(kb_, ac_, tiles, scale, out_T, out_ap3, sink):
            for tl in tiles:
                if tl["bias"] is not None:
                    tl["bias3"] = True
            attn_unit3(kb_, ac_, tiles, scale, out_T, out_ap3, sink)

        import os
        LIM = int(os.environ.get("LIM", "99"))
        if LIM == 0:
            kb.P.barrier()
            return
        for c in range(2):
            project(cTv[:, :, c * 128:(c + 1) * 128], 1, cslots[c], None)
        if LIM == 1:
            kb.P.barrier()
            return
        for Tt in range(4):
            project(xTv[:, :, Tt * 128:(Tt + 1) * 128], 0, ring[Tt % 6], Tt)
        if LIM == 2:
            kb.P.barrier()
            return
        for Tt in range(4, 36):
            if LIM in (3, 4, 5, 6) and Tt == 5:
                kb.P.barrier()
                return
            project(xTv[:, :, Tt * 128:(Tt + 1) * 128], 0, ring[Tt % 6], Tt)
            j = Tt - 4
            if j in (0, 1, 30, 31):
                var = {0: 0, 1: 1, 30: 3, 31: 4}[j]
                kb.dma("pool", biasE.ap, d["biasB"][:, var], [], [biasE])
                btab = biasE
            else:
                btab = biasG
            loc = [ring[(j + t) % 6] for t in range(5)]
            attend(ring[(j + 2) % 6], loc, btab, 0 if j == 0 else 1, 3 if j == 31 else 2,
                   xTv[:, :, (j + 2) * 128:(j + 3) * 128], 0, j * 128)
        if with_ctx:
            for c in range(2):
                attend(cslots[c], None, None, None, None, cTv[:, :, c * 128:(c + 1) * 128], 1, 4096 + c * 128)
        kb.P.barrier()


def attn_unit3(kb, ac, tiles, scale, out_T, out_ap3, sink):
    ob = ac.o_banks[ac.oi % len(ac.o_banks)]
    ac.oi += 1
    nt = len(tiles)
    for ti, tl in enumerate(tiles):
        sbk = ac.s_banks[ac.si % len(ac.s_banks)]
        ac.si += 1
        first = True
        for (kT, kap, qT, qap, c0, n) in tl["qk"]:
            kb.mm(sbk, sbk.ap[:, c0:c0 + n], kT, kap, qT, qap, start=True, stop=True, acc=(not first))
            first = False
        E = ac.E[ac.ei % 3]
        ac.ei += 1
        if tl["bias"] is not None:
            bT, bap = tl["bias"]
            sbuf = ac.sb_[ac.bi % 2]
            ac.bi += 1
            kb.stt("dve", sbuf, sbuf.ap.rearrange("p (c t) -> p c t", t=128), sbk, sbk.ap.rearrange("p (c t) -> p c t", t=128),
                   scale, bT, bap, ALU.mult, ALU.add)
            kb.act(E, E.ap, sbuf, sbuf.ap, AF.Exp)
        else:
            kb.act(E, E.ap, sbk, sbk.ap, AF.Exp, scale=scale)
        last = (ti == nt - 1) and sink is None
        for pi, (vT, vap, c0, n) in enumerate(tl["pv"]):
            kb.mm(ob, ob.ap[0:65, c0:c0 + n], vT, vap, E, E.ap[:, c0:c0 + n], start=(ti == 0 and pi == 0), stop=last,
                  acc=not (ti == 0 and pi == 0), skip=True, inc=last)
    if sink is not None:
        e64, srow_T, srow_ap = sink
        kb.mm(ob, ob.ap[0:65, :], e64, e64.ap, srow_T, srow_ap, start=False, stop=True, acc=True, skip=True)
    zr = ac.zr
    kb.op("dve", lambda e: e.reciprocal(out=zr.ap[64:65, :], in_=ob.ap[64:65, :]), [ob], [zr])
    bc = ac.bc
    kb.mm(bc, bc.ap[0:64, :], kb.ones_f, kb.ones_f.ap[64:65, 0:64], zr, zr.ap[64:65, :])
    kb.cp("act", ac.bcs, ac.bcs.ap, bc, bc.ap[0:64, :])
    kb.tt("dve", out_T, out_ap3, ob, ob.ap[0:64, :].rearrange("p (c t) -> p c t", t=128),
          ac.bcs, ac.bcs.ap.rearrange("p (c t) -> p c t", t=128), ALU.mult)


def phase_moe(kb, d, modT, lng, lnb, xin, passes, ctx_tile0, out_fn):
    BIG = 1.0e30
    with ExitStack() as st:
        kb.stack = st
        PT = max(len(p) for p in passes)
        wr = kb.sb([128, 8, 36], F32)
        kb.dma("sp", wr.ap[:, :, 0:4], d["w_group"].rearrange("(kc p) g -> p kc g", p=128), [], [wr])
        kb.dma("sp", wr.ap[:, :, 4:36], d["w_er"].rearrange("(kc p) g -> p kc g", p=128), [], [wr])
        br = kb.sb([36, 1], F32)
        kb.dma("sp", br.ap[0:4, :], d["b_group"], [], [br])
        kb.dma("sp", br.ap[4:36, :], d["b_er"], [], [br])
        ident = kb.sb([128, 128], F32)
        kb.dma("sp", ident.ap, d["ident"], [], [ident])
        sel = kb.sb([32, 32, 128], BF16)
        kb.dma("pool", sel.ap, d["sel"], [], [sel])
        acc = kb.sb([128, 8, PT * 128], F32)
        hT = kb.sb([128, 8, PT * 128], BF16)
        WdT = kb.sb([32, PT * 128], BF16)
        wgu = [kb.sb([128, 8, 1024], BF16) for _ in range(2)]
        wdn = [kb.sb([128, 4, 1024], BF16) for _ in range(2)]
        actT = [kb.sb([128, 4, 512], BF16) for _ in range(2)]
        sg = [kb.sb([128, 512], F32) for _ in range(2)]
        tmp = [kb.sb([128, 512], F32) for _ in range(2)]
        wbs = [kb.sb([128, 512], F32) for _ in range(2)]
        xt = [kb.sb([128, 8, 128], F32) for _ in range(2)]
        hf = kb.sb([128, 8, 128], F32)
        lT = kb.sb([36, 128], F32)
        Lt = kb.sb([128, 36], F32)
        sm = kb.sb([128, 16], F32)
        gm = kb.sb([128, 4], F32)
        pen = kb.sb([128, 4], F32)
        elm = kb.sb([128, 32], F32)
        elm2 = kb.sb([128, 32], F32)
        m1 = kb.sb([128, 32], F32)
        ew = kb.sb([128, 32], F32)
        Wd = kb.sb([128, 32], F32)
        lc = LNCtx(kb)
        xv = xin.ap.rearrange("(kc p) t -> p kc t", p=128)
        wguv = d["w_gate_up"]
        wdnv = d["w_down"]
        ps = kb.ps
        ecount = 0
        xi = 0
        for tiles in passes:
            for li, tile in enumerate(tiles):
                col = 1 if tile >= ctx_tile0 else 0
                x_ = xt[xi % 2]
                xi += 1
                kb.dma("sp", x_.ap, xv[:, :, tile * 128:(tile + 1) * 128], [xin], [x_])
                s_b = modT.ap[:, 32:40, col:col + 1].to_broadcast([128, 8, 128])
                sh_b = modT.ap[:, 24:32, col:col + 1].to_broadcast([128, 8, 128])
                kb.tt("dve", hf, hf.ap, x_, x_.ap, modT, s_b, ALU.mult)
                kb.tt("pool", hf, hf.ap, hf, hf.ap, modT, sh_b, ALU.add)
                kb.cp("act", hT, hT.ap[:, :, li * 128:(li + 1) * 128], hf, hf.ap)
                p7 = ps[7]
                for kc in range(8):
                    kb.mm(p7, p7.ap[0:36, 0:128], wr, wr.ap[:, kc, :], hf, hf.ap[:, kc, :], start=(kc == 0), stop=(kc == 7))
                kb.act(lT, lT.ap, p7, p7.ap[0:36, 0:128], AF.Identity, scale=1.0, bias=br.ap[:, 0:1], extra=[br])
                kb.mm(p7, p7.ap[:, 128:164], lT, lT.ap, ident, ident.ap[0:36, 0:36], acc=True)
                kb.cp("dve", Lt, Lt.ap, p7, p7.ap[:, 128:164])
                gl = Lt.ap[:, 0:4]
                el = Lt.ap[:, 4:36]
                kb.op("dve", lambda e: e.reduce_max(out=sm.ap[:, 0:1], in_=gl, axis=AX.X), [Lt], [sm])
                kb.ts("dve", sm, sm.ap[:, 1:2], sm, sm.ap[:, 0:1], -1.0, None, ALU.mult)
                kb.op("act", lambda e: e.activation(out=gm.ap, in_=gl, func=AF.Exp, bias=sm.ap[:, 1:2], scale=1.0, accum_out=sm.ap[:, 2:3]),
                      [Lt, sm], [gm, sm])
                kb.op("dve", lambda e: e.reciprocal(out=sm.ap[:, 3:4], in_=sm.ap[:, 2:3]), [sm], [sm])
                kb.ts("dve", gm, gm.ap, Lt, gl, sm.ap[:, 0:1], None, ALU.is_ge, extra=[sm])
                kb.ts("dve", pen, pen.ap, gm, gm.ap, BIG, -BIG, ALU.mult, ALU.add)
                kb.tt("dve", elm, elm.ap.rearrange("p (g e) -> p g e", e=8), Lt, el.rearrange("p (g e) -> p g e", e=8),
                      pen, pen.ap.unsqueeze(2).to_broadcast([128, 4, 8]), ALU.add)
                kb.op("dve", lambda e: e.reduce_max(out=sm.ap[:, 4:5], in_=elm.ap, axis=AX.X), [elm], [sm])
                kb.ts("dve", m1, m1.ap, elm, elm.ap, sm.ap[:, 4:5], None, ALU.is_ge, extra=[sm])
                kb.stt("dve", elm2, elm2.ap, m1, m1.ap, -BIG, elm, elm.ap, ALU.mult, ALU.add)
                kb.op("dve", lambda e: e.reduce_max(out=sm.ap[:, 6:7], in_=elm2.ap, axis=AX.X), [elm2], [sm])
                kb.ts("dve", m1, m1.ap, elm, elm.ap, sm.ap[:, 6:7], None, ALU.is_ge, extra=[sm])
                kb.ts("dve", sm, sm.ap[:, 5:6], sm, sm.ap[:, 4:5], -1.0, None, ALU.mult)
                kb.act(ew, ew.ap, elm, elm.ap, AF.Exp, scale=1.0, bias=sm.ap[:, 5:6], extra=[sm])
                kb.act(sm, sm.ap[:, 7:8], sm, sm.ap[:, 6:7], AF.Exp, scale=1.0, bias=sm.ap[:, 5:6])
                kb.ts("dve", sm, sm.ap[:, 7:8], sm, sm.ap[:, 7:8], 1.0, None, ALU.add)
                kb.op("dve", lambda e: e.reciprocal(out=sm.ap[:, 7:8], in_=sm.ap[:, 7:8]), [sm], [sm])
                kb.tt("dve", sm, sm.ap[:, 8:9], sm, sm.ap[:, 7:8], sm, sm.ap[:, 3:4], ALU.mult)
                kb.stt("dve", Wd, Wd.ap, ew, ew.ap, sm.ap[:, 8:9], m1, m1.ap, ALU.mult, ALU.mult, extra=[sm])
                kb.mm(p7, p7.ap[0:32, 256:384], Wd, Wd.ap, ident, ident.ap, acc=True)
                kb.cp("act", WdT, WdT.ap[:, li * 128:(li + 1) * 128], p7, p7.ap[0:32, 256:384])
            nt = len(tiles)
            groups = []
            c = 0
            while c < nt:
                n = min(4, nt - c)
                groups.append((c * 128, n * 128))
                c += n
            gi = 0
            for e in range(32):
                wg = wgu[ecount % 2]
                wd = wdn[ecount % 2]
                ecount += 1
                gv = wguv[e].rearrange("(kc p) f -> p kc f", p=128)
                kb.dma("pool", wg.ap[:, 0:4, :], gv[:, 0:4, :], [], [wg])
                kb.dma("pool", wg.ap[:, 4:8, :], gv[:, 4:8, :], [], [wg])
                kb.dma("pool", wd.ap, wdnv[e].rearrange("(kc p) f -> p kc f", p=128), [], [wd])
                for (c0, n) in groups:
                    aT = actT[gi % 2]
                    wb_ = wbs[gi % 2]
                    gi += 1
                    kb.mm(ps[4], ps[4].ap[:, 0:n], sel, sel.ap[:, e, :], WdT, WdT.ap[:, c0:c0 + n])
                    kb.cp("act", wb_, wb_.ap[:, 0:n], ps[4], ps[4].ap[:, 0:n])
                    for j in range(4):
                        G = ps[j % 2]
                        U = ps[2 + j % 2]
                        for kc in range(8):
                            kb.mm(G, G.ap[:, 0:n], wg, wg.ap[:, kc, j * 128:(j + 1) * 128], hT, hT.ap[:, kc, c0:c0 + n],
                                  start=(kc == 0), stop=(kc == 7))
                        for kc in range(8):
                            kb.mm(U, U.ap[:, 0:n], wg, wg.ap[:, kc, 512 + j * 128:512 + (j + 1) * 128], hT, hT.ap[:, kc, c0:c0 + n],
                                  start=(kc == 0), stop=(kc == 7))
                        s_ = sg[j % 2]
                        t_ = tmp[j % 2]
                        kb.act(s_, s_.ap[:, 0:n], G, G.ap[:, 0:n], AF.Silu)
                        kb.tt("dve", t_, t_.ap[:, 0:n], U, U.ap[:, 0:n], s_, s_.ap[:, 0:n], ALU.mult)
                        kb.tt("pool", aT, aT.ap[:, j, 0:n], t_, t_.ap[:, 0:n], wb_, wb_.ap[:, 0:n], ALU.mult)
                    for dc in range(8):
                        Y = ps[5 + dc % 2]
                        for j in range(4):
                            kb.mm(Y, Y.ap[:, 0:n], wd, wd.ap[:, j, dc * 128:(dc + 1) * 128], aT, aT.ap[:, j, 0:n],
                                  start=(j == 0), stop=(j == 3))
                        if e == 0:
                            kb.cp("dve", acc, acc.ap[:, dc, c0:c0 + n], Y, Y.ap[:, 0:n])
                        else:
                            kb.tt("dve", acc, acc.ap[:, dc, c0:c0 + n], Y, Y.ap[:, 0:n], acc, acc.ap[:, dc, c0:c0 + n], ALU.add)
            for li, tile in enumerate(tiles):
                col = 1 if tile >= ctx_tile0 else 0
                oap, oT_ = out_fn(tile)

                def y_fn(dc, li=li):
                    return acc, acc.ap[:, dc, li * 128:(li + 1) * 128]
                ln_epilogue(kb, lc, y_fn, xv[:, :, tile * 128:(tile + 1) * 128], [xin], modT.ap[:, 40:48, col:col + 1], modT,
                            lng.ap[:, 1, :], lnb.ap[:, 1, :], (lng, lnb), oap, oT_, ps[7])
        kb.P.barrier()


def _rope_np(pos, dim):
    pos = np.asarray(pos)
    pos_r = (pos // 64).astype(np.float32)
    pos_c = (pos % 64).astype(np.float32)
    quarter = dim // 4
    inv = np.power(np.float32(10000.0), -(np.arange(quarter, dtype=np.float32) / np.float32(quarter))).astype(np.float32)
    ang_r = pos_r[:, None] * inv
    ang_c = pos_c[:, None] * inv
    ang = np.concatenate([ang_r, ang_r, ang_c, ang_c], -1).astype(np.float32)
    return np.cos(ang).astype(np.float32), np.sin(ang).astype(np.float32)


def _ext_tokens(r):
    base = r * 4096 - 256 + np.arange(NEXT)
    if r == 0:
        base[0:256] = 256 + np.arange(256)
    if r == 3:
        base[4352:4608] = 248 * 64 + np.arange(256)
    return base


def _bias_tables(rpb, r):
    out = np.full((128, 5, 5, 8, 128), NEG, np.float32)
    kk = np.arange(128)
    kr, kc = kk // 64, kk % 64
    qr, qc = kk // 64, kk % 64
    cs = np.clip(qc - 8, 0, 48)
    validc = (kc[:, None] >= cs[None, :]) & (kc[:, None] < cs[None, :] + 16)
    dc = np.clip(kc[:, None] - qc[None, :], -15, 15) + 15
    for vi, j in enumerate([0, 1, 2, 30, 31]):
        for t in range(5):
            ext_row = 2 * j + 2 * t + kr
            lr = 2 * j + qr
            xw = ext_row[:, None] - lr[None, :]
            validr = (xw >= 0) & (xw <= 7)
            if r == 0:
                act = np.where(ext_row < 4, ext_row + 4, ext_row - 4)
            elif r == 3:
                act = np.where(ext_row >= 68, 248 + (ext_row - 68), 188 + ext_row)
            else:
                act = r * 64 - 4 + ext_row
            rq = r * 64 + lr
            dr = act[:, None] - rq[None, :] + 7
            ws = np.clip(rq - 4, 0, 248)
            inwin = (act[:, None] >= ws[None, :]) & (act[:, None] < ws[None, :] + 8)
            assert np.array_equal(validr & inwin, validr), (r, j, t)
            valid = validr & validc
            drc = np.clip(dr, 0, 14)
            vals = rpb[:, drc, dc]
            out[:, vi, t, :, :] = np.where(valid[:, None, :], vals.transpose(1, 0, 2), np.float32(NEG))
    return out


def _mask_a(r):
    kk = np.arange(128)
    prev = np.where(kk[:, None] >= kk[None, :], 0.0, NEG).astype(np.float32)
    nxt = np.where(kk[:, None] <= kk[None, :], 0.0, NEG).astype(np.float32)
    allneg = np.full((128, 128), NEG, np.float32)
    m = np.stack([allneg if r == 0 else prev, prev, nxt, allneg if r == 3 else nxt], axis=1)
    return np.ascontiguousarray(m)


def _fm(v, nch):
    return np.ascontiguousarray(np.asarray(v, np.float32).reshape(nch, 128).T)


def _moe_inputs(inp, l):
    sel = np.zeros((32, 32, 128), np.float32)
    for e in range(32):
        sel[e, e, :] = 1.0
    return {
        "w_group": np.ascontiguousarray(inp["w_group"][l]),
        "b_group": np.ascontiguousarray(inp["b_group"][l].reshape(4, 1)),
        "w_er": np.ascontiguousarray(inp["w_exp_router"][l]),
        "b_er": np.ascontiguousarray(inp["b_exp_router"][l].reshape(32, 1)),
        "w_gate_up": np.ascontiguousarray(inp["w_gate_up"][l]),
        "w_down": np.ascontiguousarray(inp["w_down"][l]),
        "ident": np.eye(128, dtype=np.float32),
        "sel": sel,
    }


def _common_inputs(inp, l, b):
    cT = np.stack([_fm(inp["c"][b], 8), _fm(inp["c_ctx"], 8)], axis=2)
    lg = np.stack([_fm(inp["ln_g"][l, 0], 8), _fm(inp["ln_g"][l, 1], 8)], axis=1)
    lb = np.stack([_fm(inp["ln_b"][l, 0], 8), _fm(inp["ln_b"][l, 1], 8)], axis=1)
    return {
        "cT": np.ascontiguousarray(cT),
        "w_ada": np.ascontiguousarray(inp["w_ada"][l]),
        "b_adaT": _fm(inp["b_ada"][l], 48),
        "ln_gT": np.ascontiguousarray(lg),
        "ln_bT": np.ascontiguousarray(lb),
    }


def _declare_moe(kb, sfx=""):
    return {
        "w_group": kb.din("w_group" + sfx, [1024, 4]), "b_group": kb.din("b_group" + sfx, [4, 1]),
        "w_er": kb.din("w_er" + sfx, [1024, 32]), "b_er": kb.din("b_er" + sfx, [32, 1]),
        "w_gate_up": kb.din("w_gate_up" + sfx, [32, 1024, 1024]), "w_down": kb.din("w_down" + sfx, [32, 512, 1024]),
        "ident": kb.din("ident" + sfx, [128, 128]), "sel": kb.din("sel" + sfx, [32, 32, 128]),
    }


def _declare_common(kb, sfx=""):
    return {
        "cT": kb.din("cT" + sfx, [128, 8, 2]), "w_ada": kb.din("w_ada" + sfx, [1024, 6144]),
        "b_adaT": kb.din("b_adaT" + sfx, [128, 48]), "ln_gT": kb.din("ln_gT" + sfx, [128, 2, 8]), "ln_bT": kb.din("ln_bT" + sfx, [128, 2, 8]),
    }


def build_launch1(debug=False, stop=None):
    kb = KB()
    d = _declare_common(kb)
    d.update(_declare_moe(kb))
    d.update({
        "xT": kb.din("xT", [1024, NEXT]), "ctxT": kb.din("ctxT", [1024, 256]),
        "ab_w_in": kb.din("ab_w_in", [1024, 2304]), "ab_w_out": kb.din("ab_w_out", [1024, 1024]),
        "a_sink": kb.din("a_sink", [1, 8]), "biasB": kb.din("biasB", [128, 5, 5, 8, 128]),
        "maskA": kb.din("maskA", [128, 4, 128]), "ropeT": kb.din("ropeT", [128, 2, NEXT]),
    })
    out = T(kb.dout("x2T", [1024, 4352]))
    if debug:
        x1T = T(kb.dout("x1T", [1024, 4352]))
    else:
        x1T = kb.dscr("x1T", [1024, 4352], F32)
    modT = phase_mod(kb, d["w_ada"], d["b_adaT"], d["cT"])
    lng, lnb = load_ln(kb, d["ln_gT"], d["ln_bT"])
    if stop == "mod":
        dbg = T(kb.dout("modT", [128, 48, 2]))
        kb.dma("sp", dbg.ap, modT.ap, [modT], [dbg])
        kb.P.barrier()
        return kb
    phase_l0_attn(kb, d, modT, lng, lnb, x1T)
    if stop == "l0":
        kb.P.barrier()
        return kb
    ov = out.ap.rearrange("(kc p) t -> p kc t", p=128)
    passes = [list(range(0, 10)), list(range(10, 18)), list(range(18, 26)), list(range(26, 34))]
    phase_moe(kb, d, modT, lng, lnb, x1T, passes, 32, lambda tile: (ov[:, :, tile * 128:(tile + 1) * 128], out))
    kb.P.barrier()
    return kb


def launch1_inputs(inp):
    maps = []
    rpb = np.asarray(inp["b_rpb"][0], np.float32)
    moe = _moe_inputs(inp, 0)
    tabs = {r: _bias_tables(rpb, r) for r in range(4)}
    for core in range(8):
        b, r = core // 4, core % 4
        et = _ext_tokens(r)
        cos, sin = _rope_np(et, 64)
        rope = np.stack([np.concatenate([cos.T, cos.T], 0), np.concatenate([sin.T, sin.T], 0)], axis=1)
        m = _common_inputs(inp, 0, b)
        m.update(moe)
        m.update({
            "xT": np.ascontiguousarray(inp["x"][b][et].T),
            "ctxT": np.ascontiguousarray(inp["ctx"][b].T),
            "ab_w_in": np.ascontiguousarray(inp["ab_w_in"][0]),
            "ab_w_out": np.ascontiguousarray(inp["ab_w_out"][0]),
            "a_sink": np.ascontiguousarray(inp["a_sink"][0].reshape(1, 8)),
            "biasB": tabs[r], "maskA": _mask_a(r), "ropeT": np.ascontiguousarray(rope),
        })
        maps.append(m)
    return maps


def attn_unit_b(kb, ac, tiles, scale, out_T, out_ap3):
    ob = ac.o_banks[ac.oi % len(ac.o_banks)]
    ac.oi += 1
    X, Y = ac.s_banks[0], ac.s_banks[1]
    bi0 = kb.ps.index(X)
    assert kb.ps.index(Y) == bi0 + 1
    pview = kb.psall[:, bi0 * 512:(bi0 + 2) * 512].rearrange("p (b c) -> p b c", b=2)[:, :, 0:256]
    nt = len(tiles)
    for ti, tl in enumerate(tiles):
        for hh, (kT, kap, qT, qap, c0, n) in enumerate(tl["qk"]):
            bk = X if hh % 2 == 0 else Y
            cc = (hh // 2) * 128
            kb.mm(bk, bk.ap[:, cc:cc + 128], kT, kap, qT, qap, start=True, stop=True, acc=(hh >= 2))
        E = ac.E[ac.ei % 3]
        ac.ei += 1
        ev = E.ap.rearrange("p (b c) -> p b c", b=2)
        if tl["bias"] is not None:
            bT, bap = tl["bias"]
            sbuf = ac.sb_[ac.bi % 2]
            ac.bi += 1
            for b_, bk_ in enumerate((X, Y)):
                kb.stt("dve", sbuf, sbuf.ap[:, b_ * 256:(b_ + 1) * 256].rearrange("p (a q) -> p a q", a=2),
                       bk_, bk_.ap[:, 0:256].rearrange("p (a q) -> p a q", a=2), scale, bT, bap[:, b_, :, :], ALU.mult, ALU.add)
            kb.act(E, E.ap, sbuf, sbuf.ap, AF.Exp)
        else:
            kb.op("act", lambda e: e.activation(out=ev, in_=pview, func=AF.Exp, scale=scale), [X, Y], [E])
        last = (ti == nt - 1)
        for hh, (vT, vap, c0, n) in enumerate(tl["pv"]):
            ec = (hh % 2) * 256 + (hh // 2) * 128
            kb.mm(ob, ob.ap[0:65, hh * 128:(hh + 1) * 128], vT, vap, E, E.ap[:, ec:ec + 128], start=(ti == 0 and hh == 0), stop=last,
                  acc=not (ti == 0 and hh == 0), skip=True, inc=last)
    zr = ac.zr
    kb.op("dve", lambda e: e.reciprocal(out=zr.ap[64:65, :], in_=ob.ap[64:65, :]), [ob], [zr])
    bc = ac.bc
    kb.mm(bc, bc.ap[0:64, :], kb.ones_f, kb.ones_f.ap[64:65, 0:64], zr, zr.ap[64:65, :])
    kb.cp("act", ac.bcs, ac.bcs.ap, bc, bc.ap[0:64, :])
    kb.tt("dve", out_T, out_ap3, ob, ob.ap[0:64, :].rearrange("p (c t) -> p c t", t=128),
          ac.bcs, ac.bcs.ap.rearrange("p (c t) -> p c t", t=128), ALU.mult)


NK = 16640
NKT = NK // 128


class Banks:
    def __init__(self, kb, idx):
        self.kb = kb
        self.idx = list(idx)
        self.i = 0

    def get(self):
        b = self.kb.ps[self.idx[self.i % len(self.idx)]]
        self.i += 1
        return b


def l1_scratch(kb):
    return dict(
        kTm=kb.dscr("kTm", [8, 96, NK], BF16), Vm=kb.dscr("Vm", [8, 128, NKT, 66], BF16),
        kTd=kb.dscr("kTd", [4, 128, NK], BF16), Vd=kb.dscr("Vd", [4, 128, NKT, 128], BF16),
        qTm=kb.dscr("qTm", [8, 96, 4096], BF16), qTd=kb.dscr("qTd", [4, 128, 4096], BF16),
        oTs=kb.dscr("oTs", [12, 128, 4096], BF16), x1b=kb.dscr("x1b", [1024, 4096], F32))


def phase_l1_proj(kb, d, modT, scr):
    with ExitStack() as st:
        kb.stack = st
        win = d["cd_w_in"].rearrange("(kc p) c -> p kc c", p=128)
        wq_c = kb.sb([128, 8, 384], BF16)
        wkv = kb.sb([128, 8, 256], BF16)
        wkr = kb.sb([128, 8, 96], BF16)
        wkr_r = kb.sb([128, 8, 96], BF16)
        wdq = kb.sb([128, 8, 512], BF16)
        wdq_r = kb.sb([128, 8, 512], BF16)
        wdk = kb.sb([128, 8, 512], BF16)
        wdk_r = kb.sb([128, 8, 512], BF16)
        wdv = kb.sb([128, 8, 512], BF16)
        kb.dma("pool", wq_c.ap, win[:, :, 0:384], [], [wq_c])
        kb.dma("pool", wkv.ap, win[:, :, 384:640], [], [wkv])
        kb.memset("dve", wkr, wkr.ap, 0.0)
        kb.memset("dve", wkr_r, wkr_r.ap, 0.0)
        kb.dma("pool", wkr.ap[:, :, 64:96], win[:, :, 640:672], [], [wkr])
        kb.dma("pool", wdq.ap, win[:, :, 672:1184], [], [wdq])
        kb.dma("pool", wdk.ap, win[:, :, 1184:1696], [], [wdk])
        kb.dma("pool", wdv.ap, win[:, :, 1696:2208], [], [wdv])

        def mkrot(dst, src, dap, sap, s_):
            sv = sap.rearrange("p k (g two s) -> p k g two s", two=2, s=s_)
            dv = dap.rearrange("p k (g two s) -> p k g two s", two=2, s=s_)
            for kc in range(sap.shape[1]):
                kb.ts("dve", dst, dv[:, kc, :, 0, :], src, sv[:, kc, :, 1, :], -1.0, None, ALU.mult)
                kb.cp("dve", dst, dv[:, kc, :, 1, :], src, sv[:, kc, :, 0, :])
        mkrot(wkr_r, wkr, wkr_r.ap[:, :, 64:96], wkr.ap[:, :, 64:96], 8)
        mkrot(wdq_r, wdq, wdq_r.ap, wdq.ap, 16)
        mkrot(wdk_r, wdk, wdk_r.ap, wdk.ap, 16)
        wuq = kb.sb([128, 3, 768], BF16)
        wuq_r = kb.sb([128, 3, 768], BF16)
        kb.dma("pool", wuq.ap, d["c_w_uq"].rearrange("(kc p) c -> p kc c", p=128), [], [wuq])
        kb.memset("dve", wuq_r, wuq_r.ap, 0.0)
        for h in range(8):
            mkrot(wuq_r, wuq, wuq_r.ap[:, :, h * 96 + 64:h * 96 + 96], wuq.ap[:, :, h * 96 + 64:h * 96 + 96], 8)
        wukv = kb.sb([128, 2, 1024], BF16)
        kb.dma("pool", wukv.ap, d["c_w_ukv"].rearrange("(kc p) c -> p kc c", p=128), [], [wukv])
        qg = kb.sb([128, 3], F32)
        kvg = kb.sb([128, 2], F32)
        kb.dma("sp", qg.ap, d["c_q_normT"], [], [qg])
        kb.dma("sp", kvg.ap, d["c_kv_normT"], [], [kvg])
        eps6 = kb.sb([128, 1], F32)
        kb.memset("dve", eps6, eps6.ap, 1e-6)

        xb = [kb.sb([128, 8, 512], F32) for _ in range(2)]
        hTs = [kb.sb([128, 8, 512], BF16) for _ in range(2)]
        r64 = [kb.sb([128, 2, 512], F32) for _ in range(2)]
        r32 = [kb.sb([128, 2, 512], F32) for _ in range(2)]
        sq = kb.sb([128, 3, 512], BF16)
        rs = kb.sb([128, 512], F32)
        ckvn = kb.sb([128, 2, 512], BF16)
        cqn = kb.sb([128, 3, 512], BF16)
        kn = kb.sb([64, 8, 512], BF16)
        krs = kb.sb([96, 512], BF16)
        t1 = kb.sb([128, 2, 512], F32)
        t2 = kb.sb([128, 2, 512], F32)
        dks = [kb.sb([128, 4, 512], BF16) for _ in range(2)]
        vms = kb.sb([128, 8, 4, 66], BF16)
        kb.memset("pool", vms, vms.ap[:, :, :, 64:66], 1.0)
        vds = kb.sb([128, 4, 4, 128], BF16)
        qs = [kb.sb([96, 512], BF16) for _ in range(2)]
        bk = Banks(kb, range(8))
        xv = d["x1T"].rearrange("(kc p) t -> p kc t", p=128)
        cv = d["ctx1T"].rearrange("(kc p) t -> p kc t", p=128)
        xrd = d.get("x_reads", [])
        di = [0]

        def rms(chunks, nch, gT, dstT, n):
            for c, (pt, pap) in enumerate(chunks):
                kb.act(sq, sq.ap[:, c, 0:n], pt, pap, AF.Square)
            ssb = bk.get()
            for c in range(nch):
                kb.mm(ssb, ssb.ap[:, 0:n], kb.ones_b, kb.ones_b.ap, sq, sq.ap[:, c, 0:n], start=(c == 0), stop=(c == nch - 1))
            kb.act(rs, rs.ap[:, 0:n], ssb, ssb.ap[:, 0:n], AF.Ln, scale=1.0 / (128 * nch), bias=eps6.ap[:, 0:1], extra=[eps6])
            kb.act(rs, rs.ap[:, 0:n], rs, rs.ap[:, 0:n], AF.Exp, scale=-0.5)
            for c, (pt, pap) in enumerate(chunks):
                kb.stt("dve", dstT, dstT.ap[:, c, 0:n], pt, pap, gT.ap[:, c:c + 1], rs, rs.ap[:, 0:n], ALU.mult, ALU.mult, extra=[gT])

        def proj_fm(w, c0, hT, n, M=128):
            b = bk.get()
            for kc in range(8):
                kb.mm(b, b.ap[0:M, 0:n], w, w.ap[:, kc, c0:c0 + M], hT, hT.ap[:, kc, 0:n], start=(kc == 0), stop=(kc == 7))
            return b

        def rope_pair(dstT, dst_ap, p1, p2, tab, rows, n, eng2="pool"):
            lo, hi = rows
            kb.tt("dve", t1, t1.ap[lo:hi, 0, 0:n], p1, p1.ap[lo:hi, 0:n], tab, tab.ap[lo:hi, 0, 0:n], ALU.mult)
            kb.tt("dve", t2, t2.ap[lo:hi, 0, 0:n], p2, p2.ap[lo:hi, 0:n], tab, tab.ap[lo:hi, 1, 0:n], ALU.mult)
            kb.tt(eng2, dstT, dst_ap, t1, t1.ap[lo:hi, 0, 0:n], t2, t2.ap[lo:hi, 0, 0:n], ALU.add)

        for g in range(33):
            ctx = (g == 32)
            own = (g < 8)
            n = 256 if ctx else 512
            col = 1 if ctx else 0
            k0 = g * 512
            i = di[0]
            di[0] += 1
            xt, hT = xb[i % 2], hTs[i % 2]
            src = cv if ctx else xv[:, :, k0:k0 + 512]
            kb.dma("sp", xt.ap[:, :, 0:n], src, xrd, [xt])
            s_b = modT.ap[:, 8:16, col:col + 1].to_broadcast([128, 8, n])
            sh_b = modT.ap[:, 0:8, col:col + 1].to_broadcast([128, 8, n])
            kb.tt("dve", xt, xt.ap[:, :, 0:n], xt, xt.ap[:, :, 0:n], modT, s_b, ALU.mult)
            kb.tt("pool", hT, hT.ap[:, :, 0:n], xt, xt.ap[:, :, 0:n], modT, sh_b, ALU.add)
            if not ctx:
                a64, a32 = r64[i % 2], r32[i % 2]
                kb.dma("sp", a64.ap, d["rope64"][:, :, k0:k0 + 512], [], [a64])
                kb.dma("sp", a32.ap, d["rope32"][:, :, k0:k0 + 512], [], [a32])
            pc = [proj_fm(wkv, c * 128, hT, n) for c in range(2)]
            rms([(p, p.ap[:, 0:n]) for p in pc], 2, kvg, ckvn, n)
            for h in range(8):
                b = bk.get()
                for c in range(2):
                    kb.mm(b, b.ap[0:64, 0:n], wukv, wukv.ap[:, c, h * 128:h * 128 + 64], ckvn, ckvn.ap[:, c, 0:n], start=(c == 0), stop=(c == 1))
                kb.cp("act", kn, kn.ap[:, h, 0:n], b, b.ap[0:64, 0:n])
            kb.dma("pool", scr["kTm"].ap[:, 0:64, k0:k0 + n].rearrange("h p n -> p h n"), kn.ap[:, :, 0:n], [kn], [scr["kTm"]])
            p1 = proj_fm(wkr, 0, hT, n, M=96)
            if not ctx:
                p2 = proj_fm(wkr_r, 0, hT, n, M=96)
                rope_pair(krs, krs.ap[64:96, 0:n], p1, p2, a32, (64, 96), n)
            else:
                kb.cp("act", krs, krs.ap[64:96, 0:n], p1, p1.ap[64:96, 0:n])
            for h in range(8):
                kb.dma("pool", scr["kTm"].ap[h, 64:96, k0:k0 + n], krs.ap[64:96, 0:n], [krs], [scr["kTm"]])
            nt = n // 128
            for tt in range(nt):
                b = bk.get()
                for c in range(2):
                    kb.mm(b, b.ap, ckvn, ckvn.ap[:, c, tt * 128:(tt + 1) * 128],
                          wukv, wukv.ap[:, c, :].rearrange("p (h x) -> p h x", x=128)[:, :, 64:128], start=(c == 0), stop=(c == 1))
                kb.cp("act", vms, vms.ap[:, :, tt, 0:64], b, b.ap.rearrange("p (h e) -> p h e", e=64))
            kb.dma("pool", scr["Vm"].ap[:, :, g * 4:g * 4 + nt, :].rearrange("h p t e -> p h t e"), vms.ap[:, :, 0:nt, :], [vms], [scr["Vm"]])
            dk_ = dks[i % 2]
            for half in range(2):
                pl = [proj_fm(wdk, (2 * half + c) * 128, hT, n) for c in range(2)]
                if not ctx:
                    pr = [proj_fm(wdk_r, (2 * half + c) * 128, hT, n) for c in range(2)]
                    for c in range(2):
                        rope_pair(dk_, dk_.ap[:, 2 * half + c, 0:n], pl[c], pr[c], a64, (0, 128), n)
                else:
                    for c in range(2):
                        kb.cp("act", dk_, dk_.ap[:, 2 * half + c, 0:n], pl[c], pl[c].ap[:, 0:n])
            kb.dma("pool", scr["kTd"].ap[:, :, k0:k0 + n].rearrange("h p n -> p h n"), dk_.ap[:, :, 0:n], [dk_], [scr["kTd"]])
            for tt in range(nt):
                b = bk.get()
                for kc in range(8):
                    kb.mm(b, b.ap, hT, hT.ap[:, kc, tt * 128:(tt + 1) * 128], wdv, wdv.ap[:, kc, :], start=(kc == 0), stop=(kc == 7))
                kb.cp("act", vds, vds.ap[:, :, tt, :], b, b.ap.rearrange("p (h e) -> p h e", e=128))
            kb.dma("pool", scr["Vd"].ap[:, :, g * 4:g * 4 + nt, :].rearrange("h p t e -> p h t e"), vds.ap[:, :, 0:nt, :], [vds], [scr["Vd"]])
            if own:
                pq = [proj_fm(wq_c, c * 128, hT, n) for c in range(3)]
                rms([(p, p.ap[:, 0:n]) for p in pq], 3, qg, cqn, n)
                for h in range(8):
                    q_ = qs[h % 2]
                    b1 = bk.get()
                    b2 = bk.get()
                    for c in range(3):
                        kb.mm(b1, b1.ap[0:96, 0:n], wuq, wuq.ap[:, c, h * 96:(h + 1) * 96], cqn, cqn.ap[:, c, 0:n], start=(c == 0), stop=(c == 2))
                    for c in range(3):
                        kb.mm(b2, b2.ap[0:96, 0:n], wuq_r, wuq_r.ap[:, c, h * 96:(h + 1) * 96], cqn, cqn.ap[:, c, 0:n], start=(c == 0), stop=(c == 2))
                    kb.cp("act", q_, q_.ap[0:64, 0:n], b1, b1.ap[0:64, 0:n])
                    rope_pair(q_, q_.ap[64:96, 0:n], b1, b2, a32, (64, 96), n)
                    kb.dma("pool", scr["qTm"].ap[h, :, k0:k0 + n], q_.ap[:, 0:n], [q_], [scr["qTm"]])
                dq_ = dks[(i + 1) % 2]
                for half in range(2):
                    pl = [proj_fm(wdq, (2 * half + c) * 128, hT, n) for c in range(2)]
                    pr = [proj_fm(wdq_r, (2 * half + c) * 128, hT, n) for c in range(2)]
                    for c in range(2):
                        rope_pair(dq_, dq_.ap[:, 2 * half + c, 0:n], pl[c], pr[c], a64, (0, 128), n)
                kb.dma("pool", scr["qTd"].ap[:, :, k0:k0 + n].rearrange("h p n -> p h n"), dq_.ap[:, :, 0:n], [dq_], [scr["qTd"]])
        kb.P.barrier()


def phase_l1_attn(kb, d, scr):
    with ExitStack() as st:
        kb.stack = st
        lp = kb.sb([128, 256], F32)
        kb.dma("sp", lp.ap, d["d_lambda"].rearrange("a b -> (a b)").partition_broadcast(128), [], [lp])
        pr_ = kb.sb([128, 128], F32)
        lpv = lp.ap.rearrange("p (a b) -> p a b", b=64)
        kb.tt("dve", pr_, pr_.ap.rearrange("p (a b) -> p a b", b=64), lp, lpv[:, 0:4:2, :], lp, lpv[:, 1:4:2, :], ALU.mult)
        ssum = kb.sb([128, 4], F32)
        kb.op("dve", lambda e: e.reduce_sum(out=ssum.ap[:, 0:2], in_=pr_.ap.rearrange("p (a b) -> p a b", b=64), axis=AX.X), [pr_], [ssum])
        kb.act(ssum, ssum.ap[:, 0:2], ssum, ssum.ap[:, 0:2], AF.Exp)
        kb.tt("dve", ssum, ssum.ap[:, 2:3], ssum, ssum.ap[:, 1:2], ssum, ssum.ap[:, 0:1], ALU.subtract)
        kb.ts("dve", ssum, ssum.ap[:, 2:3], ssum, ssum.ap[:, 2:3], -LAM_INIT, None, ALU.add)
        subl = kb.sb([128, 1], F32)
        kb.dma("sp", subl.ap, d["d_sublnT"], [], [subl])
        kb.ts("dve", subl, subl.ap, subl, subl.ap, 1.0 - LAM_INIT, None, ALU.mult)
        eps6 = kb.sb([128, 1], F32)
        kb.memset("dve", eps6, eps6.ap, 1e-6)

        E = [kb.sb([128, 2, 512], BF16) for _ in range(3)]
        kT = kb.sb([128, NK], BF16)
        V = kb.sb([128, NKT, 128], BF16)
        qT = kb.sb([128, 4096], BF16)
        zr = kb.sb([128, 512], F32)
        bcs = kb.sb([128, 512], F32)
        ta = kb.sb([128, 512], F32)
        tb = kb.sb([128, 512], F32)
        osb = [kb.sb([128, 512], BF16) for _ in range(2)]
        ei = 0
        oi = 0
        sp_i = 0
        ps = kb.ps

        def load(dst, dst_ap_fn, src_T, src_ap_fn, total, nsplit, reads):
            step = total // nsplit
            for s_ in range(nsplit):
                kb.dma("sp", dst_ap_fn(s_ * step, (s_ + 1) * step), src_ap_fn(s_ * step, (s_ + 1) * step), reads, [dst])

        sc_m = 96 ** -0.5
        Vm_v = V.ap.rearrange("p t e -> p (t e)")[:, 0:NKT * 66].rearrange("p (t e) -> p t e", e=66)
        for h in range(8):
            load(kT, lambda a, b: kT.ap[0:96, a:b], scr["kTm"], lambda a, b: scr["kTm"].ap[h, :, a:b], NK, 4, [scr["kTm"]])
            load(V, lambda a, b: Vm_v[:, a:b, :], scr["Vm"], lambda a, b: scr["Vm"].ap[h, :, a:b, :], NKT, 2, [scr["Vm"]])
            kb.dma("sp", qT.ap[0:96, :], scr["qTm"].ap[h], [scr["qTm"]], [qT])
            for qb in range(8):
                ob = ps[4 + oi % 2]
                oi += 1
                qap = qT.ap[0:96, qb * 512:(qb + 1) * 512]
                for kp in range(NKT // 2):
                    pb_ = 2 * (sp_i % 2)
                    sp_i += 1
                    X, Y = ps[pb_], ps[pb_ + 1]
                    kb.mm(X, X.ap, kT, kT.ap[0:96, (2 * kp) * 128:(2 * kp + 1) * 128], qT, qap)
                    kb.mm(Y, Y.ap, kT, kT.ap[0:96, (2 * kp + 1) * 128:(2 * kp + 2) * 128], qT, qap)
                    E_ = E[ei % 3]
                    ei += 1
                    kb.op("act", lambda e: e.activation(out=E_.ap.rearrange("p a b -> p (a b)"), in_=kb.psall[:, pb_ * 512:(pb_ + 2) * 512],
                                                        func=AF.Exp, scale=sc_m), [X, Y], [E_])
                    for a_ in range(2):
                        first = (kp == 0 and a_ == 0)
                        last = (kp == NKT // 2 - 1 and a_ == 1)
                        kb.mm(ob, ob.ap[0:65, :], V, Vm_v[:, 2 * kp + a_, 0:65], E_, E_.ap[:, a_, :], start=first, stop=last, acc=not first)
                kb.op("dve", lambda e: e.reciprocal(out=zr.ap[64:65, :], in_=ob.ap[64:65, :]), [ob], [zr])
                bc = ps[6]
                kb.mm(bc, bc.ap[0:64, :], kb.ones_f, kb.ones_f.ap[64:65, 0:64], zr, zr.ap[64:65, :])
                kb.cp("act", bcs, bcs.ap[0:64, :], bc, bc.ap[0:64, :])
                o_ = osb[oi % 2]
                kb.tt("dve", o_, o_.ap[0:64, :], ob, ob.ap[0:64, :], bcs, bcs.ap[0:64, :], ALU.mult)
                kb.dma("pool", scr["oTs"].ap[h, 0:64, qb * 512:(qb + 1) * 512], o_.ap[0:64, :], [o_], [scr["oTs"]])
        for h in range(4):
            load(kT, lambda a, b: kT.ap[:, a:b], scr["kTd"], lambda a, b: scr["kTd"].ap[h, :, a:b], NK, 4, [scr["kTd"]])
            load(V, lambda a, b: V.ap[:, a:b, :], scr["Vd"], lambda a, b: scr["Vd"].ap[h, :, a:b, :], NKT, 2, [scr["Vd"]])
            kb.dma("sp", qT.ap, scr["qTd"].ap[h], [scr["qTd"]], [qT])
            for qb in range(8):
                o0, o1, Z0, Z1 = ps[4], ps[5], ps[6], ps[7]
                for kt in range(NKT):
                    pb_ = 2 * (sp_i % 2)
                    sp_i += 1
                    X, Y = ps[pb_], ps[pb_ + 1]
                    ksl = slice(kt * 128, (kt + 1) * 128)
                    qsl = slice(qb * 512, (qb + 1) * 512)
                    kb.mm(X, X.ap, kT, kT.ap[0:64, ksl], qT, qT.ap[0:64, qsl])
                    kb.mm(Y, Y.ap, kT, kT.ap[64:128, ksl], qT, qT.ap[64:128, qsl])
                    E_ = E[ei % 3]
                    ei += 1
                    kb.op("act", lambda e: e.activation(out=E_.ap.rearrange("p a b -> p (a b)"), in_=kb.psall[:, pb_ * 512:(pb_ + 2) * 512],
                                                        func=AF.Exp, scale=0.125), [X, Y], [E_])
                    first = (kt == 0)
                    last = (kt == NKT - 1)
                    for a_, (o_b, z_b) in enumerate(((o0, Z0), (o1, Z1))):
                        kb.mm(o_b, o_b.ap, V, V.ap[:, kt, :], E_, E_.ap[:, a_, :], start=first, stop=last, acc=not first)
                        kb.mm(z_b, z_b.ap, kb.ones_b, kb.ones_b.ap, E_, E_.ap[:, a_, :], start=first, stop=last, acc=not first)
                kb.op("dve", lambda e: e.reciprocal(out=zr.ap, in_=Z0.ap), [Z0], [zr])
                kb.tt("dve", ta, ta.ap, o0, o0.ap, zr, zr.ap, ALU.mult)
                kb.op("dve", lambda e: e.reciprocal(out=zr.ap, in_=Z1.ap), [Z1], [zr])
                kb.tt("dve", tb, tb.ap, o1, o1.ap, zr, zr.ap, ALU.mult)
                kb.stt("dve", ta, ta.ap, tb, tb.ap, ssum.ap[:, 2:3], ta, ta.ap, ALU.mult, ALU.add, extra=[ssum])
                kb.tt("pool", tb, tb.ap, ta, ta.ap, ta, ta.ap, ALU.mult)
                sb_ = ps[0]
                kb.mm(sb_, sb_.ap, kb.ones_f, kb.ones_f.ap, tb, tb.ap)
                kb.act(bcs, bcs.ap, sb_, sb_.ap, AF.Ln, scale=1.0 / 128, bias=eps6.ap[:, 0:1], extra=[eps6])
                kb.act(bcs, bcs.ap, bcs, bcs.ap, AF.Exp, scale=-0.5)
                o_ = osb[oi % 2]
                oi += 1
                kb.stt("dve", o_, o_.ap, ta, ta.ap, subl.ap[:, 0:1], bcs, bcs.ap, ALU.mult, ALU.mult, extra=[subl])
                kb.dma("pool", scr["oTs"].ap[8 + h, :, qb * 512:(qb + 1) * 512], o_.ap, [o_], [scr["oTs"]])
        kb.P.barrier()


def phase_l1_out(kb, d, modT, lng, lnb, scr):
    with ExitStack() as st:
        kb.stack = st
        wC = kb.sb([64, 8, 1024], BF16)
        wD = kb.sb([128, 4, 1024], BF16)
        kb.dma("pool", wC.ap, d["cd_w_out"][0:512, :].rearrange("(h p) c -> p h c", p=64), [], [wC])
        kb.dma("pool", wD.ap, d["cd_w_out"][512:1024, :].rearrange("(h p) c -> p h c", p=128), [], [wD])
        oTb = [kb.sb([128, 12, 128], BF16) for _ in range(2)]
        lc = LNCtx(kb)
        xv = d["x1T"].rearrange("(kc p) t -> p kc t", p=128)
        ov = scr["x1b"].ap.rearrange("(kc p) t -> p kc t", p=128)
        ps = kb.ps
        for tile in range(32):
            oT = oTb[tile % 2]
            cs_ = slice(tile * 128, (tile + 1) * 128)
            kb.dma("sp", oT.ap[0:64, 0:8, :], scr["oTs"].ap[0:8, 0:64, cs_].rearrange("h p n -> p h n"), [scr["oTs"]], [oT])
            kb.dma("sp", oT.ap[:, 8:12, :], scr["oTs"].ap[8:12, :, cs_].rearrange("h p n -> p h n"), [scr["oTs"]], [oT])
            y0, y1 = ps[(2 * tile) % 4], ps[(2 * tile) % 4 + 1]
            for dc in range(8):
                pb_ = y0 if dc < 4 else y1
                c0 = (dc % 4) * 128
                for h in range(12):
                    if h < 8:
                        l_, lap, rap = wC, wC.ap[:, h, dc * 128:(dc + 1) * 128], oT.ap[0:64, h, :]
                    else:
                        l_, lap, rap = wD, wD.ap[:, h - 8, dc * 128:(dc + 1) * 128], oT.ap[:, h, :]
                    kb.mm(pb_, pb_.ap[:, c0:c0 + 128], l_, lap, oT, rap, start=(h == 0), stop=(h == 11), acc=not (dc % 4 == 0 and h == 0))

            def y_fn(dc, y0=y0, y1=y1):
                pb_ = y0 if dc < 4 else y1
                return pb_, pb_.ap[:, (dc % 4) * 128:(dc % 4 + 1) * 128]
            ln_epilogue(kb, lc, y_fn, xv[:, :, cs_], d.get("x_reads", []), modT.ap[:, 16:24, 0:1], modT, lng.ap[:, 0, :], lnb.ap[:, 0, :], (lng, lnb),
                        ov[:, :, cs_], scr["x1b"], ps[7])
        kb.P.barrier()


def build_launch2(debug=False, stop=None):
    kb = KB()
    d = _declare_common(kb)
    d.update(_declare_moe(kb))
    d.update({
        "x1T": kb.din("x1T", [1024, 16384]), "ctx1T": kb.din("ctx1T", [1024, 256]),
        "cd_w_in": kb.din("cd_w_in", [1024, 2208]), "cd_w_out": kb.din("cd_w_out", [1024, 1024]),
        "c_w_uq": kb.din("c_w_uq", [384, 768]), "c_w_ukv": kb.din("c_w_ukv", [256, 1024]),
        "c_q_normT": kb.din("c_q_normT", [128, 3]), "c_kv_normT": kb.din("c_kv_normT", [128, 2]),
        "d_lambda": kb.din("d_lambda", [4, 64]), "d_sublnT": kb.din("d_sublnT", [128, 1]),
        "rope64": kb.din("rope64", [128, 2, 16384]), "rope32": kb.din("rope32", [128, 2, 16384]),
    })
    out = T(kb.dout("outT", [1024, 4096]))
    scr = l1_scratch(kb)
    if debug:
        scr["x1b"] = T(kb.dout("x1b_dbg", [1024, 4096]))
    modT = phase_mod(kb, d["w_ada"], d["b_adaT"], d["cT"])
    lng, lnb = load_ln(kb, d["ln_gT"], d["ln_bT"])
    phase_l1_proj(kb, d, modT, scr)
    phase_l1_attn(kb, d, scr)
    phase_l1_out(kb, d, modT, lng, lnb, scr)
    if stop == "attn":
        kb.P.barrier()
        return kb
    ov = out.ap.rearrange("(kc p) t -> p kc t", p=128)
    passes = [list(range(8 * i, 8 * i + 8)) for i in range(4)]
    phase_moe(kb, d, modT, lng, lnb, scr["x1b"], passes, 99, lambda tile: (ov[:, :, tile * 128:(tile + 1) * 128], out))
    kb.P.barrier()
    return kb


def _own_first(r):
    idx = np.arange(S)
    own = idx[r * 4096:(r + 1) * 4096]
    rest = np.concatenate([idx[:r * 4096], idx[(r + 1) * 4096:]])
    return np.concatenate([own, rest])


def launch2_inputs(inp, x1, ctx1):
    maps = []
    moe = _moe_inputs(inp, 1)
    for core in range(8):
        b, r = core // 4, core % 4
        perm = _own_first(r)
        cos64, sin64 = _rope_np(perm, 64)
        cos32, sin32 = _rope_np(perm, 32)
        rope64 = np.stack([np.concatenate([cos64.T, cos64.T], 0), np.concatenate([sin64.T, sin64.T], 0)], axis=1)
        z = np.zeros((64, S), np.float32)
        z2 = np.zeros((32, S), np.float32)
        rope32 = np.stack([np.concatenate([z, cos32.T, z2], 0), np.concatenate([z, sin32.T, z2], 0)], axis=1)
        m = _common_inputs(inp, 1, b)
        m.update(moe)
        m.update({
            "x1T": np.ascontiguousarray(x1[b][perm].T), "ctx1T": np.ascontiguousarray(ctx1[b].T),
            "cd_w_in": np.ascontiguousarray(inp["cd_w_in"][0]), "cd_w_out": np.ascontiguousarray(inp["cd_w_out"][0]),
            "c_w_uq": np.ascontiguousarray(inp["c_w_uq"][0]), "c_w_ukv": np.ascontiguousarray(inp["c_w_ukv"][0]),
            "c_q_normT": _fm(inp["c_q_norm"][0], 3), "c_kv_normT": _fm(inp["c_kv_norm"][0], 2),
            "d_lambda": np.ascontiguousarray(inp["d_lambda"][0]), "d_sublnT": _fm(inp["d_subln"][0], 1),
            "rope64": np.ascontiguousarray(rope64), "rope32": np.ascontiguousarray(rope32),
        })
        maps.append(m)
    return maps


_CACHE = {}


def _kernel2(**inp):
    inp = {k: np.asarray(v) for k, v in inp.items()}
    if "k1" not in _CACHE:
        _CACHE["k1"] = build_launch1()
        _CACHE["k2"] = build_launch2()
    k1, k2 = _CACHE["k1"], _CACHE["k2"]
    res1 = run_bass_kernel_spmd(k1.nc, launch1_inputs(inp), core_ids=list(range(8)))
    x1 = np.empty((2, S, D), np.float32)
    ctx1 = np.empty((2, L, D), np.float32)
    for core in range(8):
        b, r = core // 4, core % 4
        o = res1.results[core]["x2T"]
        x1[b, r * 4096:(r + 1) * 4096] = o[:, 0:4096].T
        if r == 0:
            ctx1[b] = o[:, 4096:4352].T
    res2 = run_bass_kernel_spmd(k2.nc, launch2_inputs(inp, x1, ctx1), core_ids=list(range(8)))
    out = np.empty((2, S, D), np.float32)
    for core in range(8):
        b, r = core // 4, core % 4
        out[b, r * 4096:(r + 1) * 4096] = res2.results[core]["outT"].T
    return out


def build_fused():
    kb = KB()
    c0 = _declare_common(kb, "0")
    c1 = _declare_common(kb, "1")
    m0 = _declare_moe(kb, "0")
    m1 = _declare_moe(kb, "1")
    xT = kb.din("xT", [4, 1024, NEXT])
    biasB = kb.din("biasB", [4, 128, 5, 5, 8, 128])
    maskA = kb.din("maskA", [4, 128, 4, 128])
    ropeT = kb.din("ropeT", [4, 128, 2, NEXT])
    l0 = {"ctxT": kb.din("ctxT", [1024, 256]), "ab_w_in": kb.din("ab_w_in", [1024, 2304]),
          "ab_w_out": kb.din("ab_w_out", [1024, 1024]), "a_sink": kb.din("a_sink", [1, 8])}
    l1 = {
        "cd_w_in": kb.din("cd_w_in", [1024, 2208]), "cd_w_out": kb.din("cd_w_out", [1024, 1024]),
        "c_w_uq": kb.din("c_w_uq", [384, 768]), "c_w_ukv": kb.din("c_w_ukv", [256, 1024]),
        "c_q_normT": kb.din("c_q_normT", [128, 3]), "c_kv_normT": kb.din("c_kv_normT", [128, 2]),
        "d_lambda": kb.din("d_lambda", [4, 64]), "d_sublnT": kb.din("d_sublnT", [128, 1]),
        "rope64": kb.din("rope64", [128, 2, 16384]), "rope32": kb.din("rope32", [128, 2, 16384]),
    }
    out = T(kb.dout("outT", [1024, 4096]))
    x1T = kb.dscr("x1T_s", [1024, 4352], F32)
    x2T = kb.dscr("x2T_s", [1024, S + L], F32)
    x2v = x2T.ap.rearrange("(kc p) t -> p kc t", p=128)
    mod0 = phase_mod(kb, c0["w_ada"], c0["b_adaT"], c0["cT"])
    lng0, lnb0 = load_ln(kb, c0["ln_gT"], c0["ln_bT"])
    for seg in range(4):
        dseg = dict(l0)
        dseg.update({"xT": xT[seg], "biasB": biasB[seg], "maskA": maskA[seg], "ropeT": ropeT[seg]})
        phase_l0_attn(kb, dseg, mod0, lng0, lnb0, x1T, with_ctx=(seg == 0))
        ntile = 34 if seg == 0 else 32
        passes = [list(range(0, 10)), list(range(10, 18)), list(range(18, 26)), list(range(26, 34))] if seg == 0 else \
            [list(range(8 * i, 8 * i + 8)) for i in range(4)]

        def out_fn(tile, seg=seg):
            if tile < 32:
                c = seg * 4096 + tile * 128
            else:
                c = S + (tile - 32) * 128
            return x2v[:, :, c:c + 128], x2T
        phase_moe(kb, m0, mod0, lng0, lnb0, x1T, passes, 32, out_fn)
    mod1 = phase_mod(kb, c1["w_ada"], c1["b_adaT"], c1["cT"])
    lng1, lnb1 = load_ln(kb, c1["ln_gT"], c1["ln_bT"])
    scr = l1_scratch(kb)
    l1["x1T"] = x2T.ap[:, 0:S]
    l1["ctx1T"] = x2T.ap[:, S:S + L]
    l1["x_reads"] = [x2T]
    phase_l1_proj(kb, l1, mod1, scr)
    phase_l1_attn(kb, l1, scr)
    phase_l1_out(kb, l1, mod1, lng1, lnb1, scr)
    ov = out.ap.rearrange("(kc p) t -> p kc t", p=128)
    passes = [list(range(8 * i, 8 * i + 8)) for i in range(4)]
    phase_moe(kb, m1, mod1, lng1, lnb1, scr["x1b"], passes, 99, lambda tile: (ov[:, :, tile * 128:(tile + 1) * 128], out))
    kb.P.barrier()
    return kb


def fused_inputs(inp):
    maps = []
    rpb = np.asarray(inp["b_rpb"][0], np.float32)
    tabs = {r: _bias_tables(rpb, r) for r in range(4)}
    masks = {r: _mask_a(r) for r in range(4)}
    moe0 = {k + "0": v for k, v in _moe_inputs(inp, 0).items()}
    moe1 = {k + "1": v for k, v in _moe_inputs(inp, 1).items()}
    ropes0 = {}
    xts = {}
    for r in range(4):
        et = _ext_tokens(r)
        cos, sin = _rope_np(et, 64)
        ropes0[r] = np.stack([np.concatenate([cos.T, cos.T], 0), np.concatenate([sin.T, sin.T], 0)], axis=1)
        for b in range(2):
            xts[(b, r)] = np.ascontiguousarray(inp["x"][b][et].T)
    for core in range(8):
        b, r = core // 4, core % 4
        order = [r] + [q for q in range(4) if q != r]
        perm = _own_first(r)
        cos64, sin64 = _rope_np(perm, 64)
        cos32, sin32 = _rope_np(perm, 32)
        rope64 = np.stack([np.concatenate([cos64.T, cos64.T], 0), np.concatenate([sin64.T, sin64.T], 0)], axis=1)
        z = np.zeros((64, S), np.float32)
        z2 = np.zeros((32, S), np.float32)
        rope32 = np.stack([np.concatenate([z, cos32.T, z2], 0), np.concatenate([z, sin32.T, z2], 0)], axis=1)
        m = {k + "0": v for k, v in _common_inputs(inp, 0, b).items()}
        m.update({k + "1": v for k, v in _common_inputs(inp, 1, b).items()})
        m.update(moe0)
        m.update(moe1)
        m.update({
            "xT": np.stack([xts[(b, q)] for q in order], 0),
            "biasB": np.stack([tabs[q] for q in order], 0),
            "maskA": np.stack([masks[q] for q in order], 0),
            "ropeT": np.stack([ropes0[q] for q in order], 0),
            "ctxT": np.ascontiguousarray(inp["ctx"][b].T),
            "ab_w_in": np.ascontiguousarray(inp["ab_w_in"][0]), "ab_w_out": np.ascontiguousarray(inp["ab_w_out"][0]),
            "a_sink": np.ascontiguousarray(inp["a_sink"][0].reshape(1, 8)),
            "cd_w_in": np.ascontiguousarray(inp["cd_w_in"][0]), "cd_w_out": np.ascontiguousarray(inp["cd_w_out"][0]),
            "c_w_uq": np.ascontiguousarray(inp["c_w_uq"][0]), "c_w_ukv": np.ascontiguousarray(inp["c_w_ukv"][0]),
            "c_q_normT": _fm(inp["c_q_norm"][0], 3), "c_kv_normT": _fm(inp["c_kv_norm"][0], 2),
            "d_lambda": np.ascontiguousarray(inp["d_lambda"][0]), "d_sublnT": _fm(inp["d_subln"][0], 1),
            "rope64": np.ascontiguousarray(rope64), "rope32": np.ascontiguousarray(rope32),
        })
        maps.append(m)
    return maps


def kernel_unfused(**inp):
    return _kernel2(**inp)


def kernel(**inp):
    inp = {k: np.asarray(v) for k, v in inp.items()}
    if "kf" not in _CACHE:
        _CACHE["kf"] = build_fused()
    kf = _CACHE["kf"]
    res = run_bass_kernel_spmd(kf.nc, fused_inputs(inp), core_ids=list(range(8)))
    out = np.empty((2, S, D), np.float32)
    for core in range(8):
        b, r = core // 4, core % 4
        out[b, r * 4096:(r + 1) * 4096] = res.results[core]["outT"].T
    return out
```

```python
import math
from contextlib import ExitStack
import numpy as np
import concourse.bass as bass
import concourse.mybir as mybir
from concourse.bass_utils import run_bass_kernel_spmd

F32 = mybir.dt.float32
BF16 = mybir.dt.bfloat16
ALU = mybir.AluOpType
AF = mybir.ActivationFunctionType
AX = mybir.AxisListType

D = 1024
S = 16384
L = 256
DEPTH = 2
ALPHA = (2 * DEPTH) ** 0.25
LN_EPS = 1e-5 / (ALPHA * ALPHA)
NEG = -30000.0
LAM_INIT = 0.8 - 0.6 * math.exp(-0.3 * 1)
NEXT = 4608


class H:
    __slots__ = ("w", "r")

    def __init__(self):
        self.w = None
        self.r = []


class T:
    def __init__(self, ap):
        self.ap = ap
        self.h = H()


class Prog:
    NDMA = 8

    def __init__(self, nc):
        self.nc = nc
        self.eng = {"pe": nc.tensor, "act": nc.scalar, "dve": nc.vector,
                    "pool": nc.gpsimd, "sp": nc.sync}
        self.sem = {}
        self.cnt = {}
        for k in ("pe", "act", "dve", "pool"):
            self.sem[k] = nc.alloc_semaphore("s_" + k)
            self.cnt[k] = 0
        self.seen = {k: {} for k in self.eng}
        self.dma_i = {}
        self.dma_sems = {}
        for q in ("sp", "pool", "act"):
            self.dma_i[q] = 0
            self.dma_sems[q] = []
            for i in range(self.NDMA):
                key = ("dma", q, i)
                self.sem[key] = nc.alloc_semaphore("d_%s_%d" % (q, i))
                self.cnt[key] = 0
                self.dma_sems[q].append(key)
        self.n_ins = 0

    def _wait(self, e, key, val):
        if self.seen[e].get(key, 0) < val:
            self.eng[e].wait_ge(self.sem[key], val)
            self.seen[e][key] = val

    def _deps(self, e, reads, writes, skip_own_waw=False):
        deps = {}
        for h in reads:
            if h.w is not None:
                k, v = h.w
                if deps.get(k, 0) < v:
                    deps[k] = v
        for h in writes:
            if h.w is not None:
                k, v = h.w
                if not (skip_own_waw and k == e):
                    if deps.get(k, 0) < v:
                        deps[k] = v
            for (k, v) in h.r:
                if deps.get(k, 0) < v:
                    deps[k] = v
        for k, v in deps.items():
            self._wait(e, k, v)

    def _mark(self, tok, reads, writes):
        for h in reads:
            if len(h.r) > 16:
                d = {}
                for (k, v) in h.r:
                    if d.get(k, 0) < v:
                        d[k] = v
                h.r = list(d.items())
            h.r.append(tok)
        for h in writes:
            h.w = tok
            h.r = []

    def op(self, e, fn, reads=(), writes=(), inc=True, acc=False):
        self._deps(e, reads, writes, skip_own_waw=acc)
        ins = fn(self.eng[e])
        self.n_ins += 1
        if inc:
            self.cnt[e] += 1
            ins.then_inc(self.sem[e], 1)
            tok = (e, self.cnt[e])
        else:
            tok = (e, self.cnt[e] + 1)
        self._mark(tok, reads, writes)
        return tok

    def dma(self, q, out, in_, reads=(), writes=()):
        self._deps(q, reads, writes)
        i = self.dma_i[q]
        self.dma_i[q] += 1
        key = self.dma_sems[q][i % self.NDMA]
        if self.cnt[key] > 0:
            self._wait(q, key, self.cnt[key])
        self.cnt[key] += 16
        self.eng[q].dma_start(out=out, in_=in_).then_inc(self.sem[key], 16)
        self.n_ins += 1
        tok = (key, self.cnt[key])
        self._mark(tok, reads, writes)
        return tok

    def barrier(self):
        for e in self.eng:
            for k, v in self.cnt.items():
                if v > 0:
                    self._wait(e, k, v)


class KB:
    def __init__(self):
        self.nc = bass.Bass("TRN2", target_bir_lowering=False)
        self.P = Prog(self.nc)
        nc = self.nc
        psall = nc.alloc_psum_tensor("psall", [128, 4096], F32).ap()
        self.psall = psall
        self.ps = [T(psall[:, i * 512:(i + 1) * 512]) for i in range(8)]
        self.stack = None
        self.nm = 0
        self.ones_f = self.gsb([128, 128], F32)
        self.ones_b = self.gsb([128, 128], BF16)
        self.op("dve", lambda e: e.memset(self.ones_f.ap, 1.0), [], [self.ones_f])
        self.op("dve", lambda e: e.memset(self.ones_b.ap, 1.0), [], [self.ones_b])

    def name(self, p="t"):
        self.nm += 1
        return "%s%d" % (p, self.nm)

    def gsb(self, shape, dt):
        return T(self.nc.alloc_sbuf_tensor(self.name("g"), list(shape), dt).ap())

    def sb(self, shape, dt):
        t = self.stack.enter_context(self.nc.sbuf_tensor(self.name("s"), list(shape), dt))
        return T(t.ap())

    def din(self, name, shape, dt=F32):
        return self.nc.dram_tensor(name, list(shape), dt, kind="ExternalInput").ap()

    def dout(self, name, shape, dt=F32):
        return self.nc.dram_tensor(name, list(shape), dt, kind="ExternalOutput").ap()

    def dscr(self, name, shape, dt):
        return T(self.nc.dram_tensor(name, list(shape), dt, kind="Internal").ap())

    def op(self, e, fn, reads, writes, inc=True, acc=False):
        return self.P.op(e, fn, [t.h for t in reads], [t.h for t in writes], inc=inc, acc=acc)

    def dma(self, q, out, in_, reads, writes):
        return self.P.dma(q, out, in_, [t.h for t in reads], [t.h for t in writes])

    def mm(self, o, o_ap, l, l_ap, r, r_ap, start=True, stop=True, acc=None, skip=False, inc=None):
        if acc is None:
            acc = not start
        if inc is None:
            inc = stop
        self.op("pe", lambda e: e.matmul(o_ap, lhsT=l_ap, rhs=r_ap, start=start, stop=stop, skip_group_check=skip),
                [l, r], [o], inc=inc, acc=acc)

    def tt(self, eng, o, o_ap, a, a_ap, b, b_ap, op):
        self.op(eng, lambda e: e.tensor_tensor(out=o_ap, in0=a_ap, in1=b_ap, op=op), [a, b], [o])

    def stt(self, eng, o, o_ap, a, a_ap, sc, b, b_ap, op0, op1, extra=()):
        self.op(eng, lambda e: e.scalar_tensor_tensor(out=o_ap, in0=a_ap, scalar=sc, in1=b_ap, op0=op0, op1=op1),
                [a, b] + list(extra), [o])

    def ts(self, eng, o, o_ap, a, a_ap, s1, s2, op0, op1=None, extra=()):
        if op1 is None:
            self.op(eng, lambda e: e.tensor_scalar(out=o_ap, in0=a_ap, scalar1=s1, scalar2=None, op0=op0),
                    [a] + list(extra), [o])
        else:
            self.op(eng, lambda e: e.tensor_scalar(out=o_ap, in0=a_ap, scalar1=s1, scalar2=s2, op0=op0, op1=op1),
                    [a] + list(extra), [o])

    def act(self, o, o_ap, a, a_ap, func, scale=1.0, bias=None, extra=()):
        if bias is None:
            self.op("act", lambda e: e.activation(out=o_ap, in_=a_ap, func=func, scale=scale),
                    [a] + list(extra), [o])
        else:
            self.op("act", lambda e: e.activation(out=o_ap, in_=a_ap, func=func, scale=scale, bias=bias),
                    [a] + list(extra), [o])

    def cp(self, eng, o, o_ap, a, a_ap):
        if eng == "act":
            self.op("act", lambda e: e.copy(out=o_ap, in_=a_ap), [a], [o])
        else:
            self.op(eng, lambda e: e.tensor_copy(out=o_ap, in_=a_ap), [a], [o])

    def memset(self, eng, o, o_ap, v):
        self.op(eng, lambda e: e.memset(o_ap, v), [], [o])

    def rstd(self, o, o_ap, a, a_ap, scale, eps, epsT):
        self.act(o, o_ap, a, a_ap, AF.Ln, scale=scale, bias=epsT.ap[0:o_ap.shape[0] + o_ap.base_partition(), 0:1][o_ap.base_partition():, :], extra=[epsT])
        self.act(o, o_ap, o, o_ap, AF.Exp, scale=-0.5)


def phase_mod(kb, w_ada, b_adaT, cT):
    modT = kb.gsb([128, 48, 2], F32)
    with ExitStack() as st:
        kb.stack = st
        cs = kb.sb([128, 8, 2], F32)
        kb.dma("sp", cs.ap, cT, [], [cs])
        kb.act(cs, cs.ap, cs, cs.ap, AF.Silu)
        bT = kb.sb([128, 48], F32)
        kb.dma("sp", bT.ap, b_adaT, [], [bT])
        wv = w_ada.rearrange("(kc p) f -> p kc f", p=128)
        bufs = [kb.sb([128, 8, 512], F32) for _ in range(2)]
        ps = kb.ps[0]
        for pc in range(12):
            wb = bufs[pc % 2]
            kb.dma("sp", wb.ap, wv[:, :, pc * 512:(pc + 1) * 512], [], [wb])
            for fc in range(4):
                ch = pc * 4 + fc
                for kc in range(8):
                    kb.mm(ps, ps.ap[:, 2 * ch:2 * ch + 2], wb, wb.ap[:, kc, fc * 128:(fc + 1) * 128],
                          cs, cs.ap[:, kc, :], start=(kc == 0), stop=(kc == 7), acc=(not (pc == 0 and fc == 0 and kc == 0)))
        pv = ps.ap[:, 0:96].rearrange("p (c j) -> p c j", j=2)
        kb.tt("dve", modT, modT.ap, ps, pv, bT, bT.ap.unsqueeze(2).to_broadcast([128, 48, 2]), ALU.add)
        for w in (1, 4):
            kb.ts("dve", modT, modT.ap[:, w * 8:(w + 1) * 8, :], modT, modT.ap[:, w * 8:(w + 1) * 8, :], 1.0, None, ALU.add)
        for w in (2, 5):
            kb.ts("dve", modT, modT.ap[:, w * 8:(w + 1) * 8, :], modT, modT.ap[:, w * 8:(w + 1) * 8, :], 1.0 / ALPHA, None, ALU.mult)
        kb.P.barrier()
    return modT


def load_ln(kb, ln_gT, ln_bT):
    g = kb.gsb([128, 2, 8], F32)
    b = kb.gsb([128, 2, 8], F32)
    kb.dma("sp", g.ap, ln_gT, [], [g])
    kb.dma("sp", b.ap, ln_bT, [], [b])
    return g, b


class LNCtx:
    def __init__(self, kb):
        self.z = kb.sb([128, 8, 128], F32)
        self.zsq = kb.sb([128, 8, 128], F32)
        self.xo = [kb.sb([128, 8, 128], F32) for _ in range(2)]
        self.m = kb.sb([128, 128], F32)
        self.msq = kb.sb([128, 128], F32)
        self.var = kb.sb([128, 128], F32)
        self.eps = kb.sb([128, 1], F32)
        kb.memset("dve", self.eps, self.eps.ap, LN_EPS)
        self.i = 0


def ln_epilogue(kb, lc, y_fn, xres_ap, xres_reads, gmod_ap, modT, lng_ap, lnb_ap, lnp, out_ap, out_T, stat_ps):
    z, zsq = lc.z, lc.zsq
    kb.dma("sp", z.ap, xres_ap, xres_reads, [z])
    for dc in range(8):
        yt, yap = y_fn(dc)
        kb.stt("dve", z, z.ap[:, dc, :], yt, yap, gmod_ap[:, dc, :], z, z.ap[:, dc, :], ALU.mult, ALU.add, extra=[modT])
    kb.tt("pool", zsq, zsq.ap, z, z.ap, z, z.ap, ALU.mult)
    for dc in range(8):
        kb.mm(stat_ps, stat_ps.ap[:, 0:128], kb.ones_f, kb.ones_f.ap, z, z.ap[:, dc, :], start=(dc == 0), stop=(dc == 7))
    for dc in range(8):
        kb.mm(stat_ps, stat_ps.ap[:, 128:256], kb.ones_f, kb.ones_f.ap, zsq, zsq.ap[:, dc, :], start=(dc == 0), stop=(dc == 7), acc=True)
    m, msq, var = lc.m, lc.msq, lc.var
    kb.act(m, m.ap, stat_ps, stat_ps.ap[:, 0:128], AF.Copy, scale=1.0 / D)
    kb.tt("dve", msq, msq.ap, m, m.ap, m, m.ap, ALU.mult)
    kb.stt("dve", var, var.ap, stat_ps, stat_ps.ap[:, 128:256], 1.0 / D, msq, msq.ap, ALU.mult, ALU.subtract)
    kb.act(var, var.ap, var, var.ap, AF.Ln, scale=1.0, bias=lc.eps.ap[:, 0:1], extra=[lc.eps])
    kb.act(var, var.ap, var, var.ap, AF.Exp, scale=-0.5)
    xo = lc.xo[lc.i % 2]
    lc.i += 1
    kb.tt("dve", z, z.ap, z, z.ap, m, m.ap.unsqueeze(1).to_broadcast([128, 8, 128]), ALU.subtract)
    kb.tt("pool", z, z.ap, z, z.ap, var, var.ap.unsqueeze(1).to_broadcast([128, 8, 128]), ALU.mult)
    kb.tt("dve", z, z.ap, z, z.ap, lnp[0], lng_ap.unsqueeze(2).to_broadcast([128, 8, 128]), ALU.mult)
    kb.tt("pool", xo, xo.ap, z, z.ap, lnp[1], lnb_ap.unsqueeze(2).to_broadcast([128, 8, 128]), ALU.add)
    kb.dma("pool", out_ap, xo.ap, [xo], [out_T])


class AttCtx:
    def __init__(self, kb, s_banks, o_banks, bc_bank):
        self.sb_ = [kb.sb([128, 512], F32) for _ in range(2)]
        self.E = [kb.sb([128, 512], BF16) for _ in range(3)]
        self.zr = kb.sb([128, 512], F32)
        self.bcs = kb.sb([64, 512], F32)
        self.s_banks = s_banks
        self.o_banks = o_banks
        self.bc = bc_bank
        self.si = 0
        self.ei = 0
        self.oi = 0
        self.bi = 0


def attn_unit(kb, ac, tiles, scale, out_T, out_ap, ncols=512, sink=None):
    ob = ac.o_banks[ac.oi % len(ac.o_banks)]
    ac.oi += 1
    nt = len(tiles)
    for ti, tl in enumerate(tiles):
        sbk = ac.s_banks[ac.si % len(ac.s_banks)]
        ac.si += 1
        first = True
        for (kT, kap, qT, qap, c0, n) in tl["qk"]:
            kb.mm(sbk, sbk.ap[:, c0:c0 + n], kT, kap, qT, qap, start=True, stop=True, acc=(not first))
            first = False
        E = ac.E[ac.ei % 3]
        ac.ei += 1
        if tl["bias"] is not None:
            bT, bap = tl["bias"]
            sbuf = ac.sb_[ac.bi % 2]
            ac.bi += 1
            kb.stt("dve", sbuf, sbuf.ap[:, 0:ncols], sbk, sbk.ap[:, 0:ncols], scale, bT, bap, ALU.mult, ALU.add)
            kb.act(E, E.ap[:, 0:ncols], sbuf, sbuf.ap[:, 0:ncols], AF.Exp)
        else:
            kb.act(E, E.ap[:, 0:ncols], sbk, sbk.ap[:, 0:ncols], AF.Exp, scale=scale)
        last = (ti == nt - 1) and sink is None
        for pi, (vT, vap, c0, n) in enumerate(tl["pv"]):
            kb.mm(ob, ob.ap[0:65, c0:c0 + n], vT, vap, E, E.ap[:, c0:c0 + n], start=(ti == 0 and pi == 0), stop=last,
                  acc=not (ti == 0 and pi == 0), skip=True, inc=last)
    if sink is not None:
        e64, srow_T, srow_ap = sink
        kb.mm(ob, ob.ap[0:65, 0:ncols], e64, e64.ap, srow_T, srow_ap, start=False, stop=True, acc=True)
    zr = ac.zr
    kb.op("dve", lambda e: e.reciprocal(out=zr.ap[64:65, 0:ncols], in_=ob.ap[64:65, 0:ncols]), [ob], [zr])
    bc = ac.bc
    kb.mm(bc, bc.ap[0:64, 0:ncols], kb.ones_f, kb.ones_f.ap[64:65, 0:64], zr, zr.ap[64:65, 0:ncols])
    kb.cp("act", ac.bcs, ac.bcs.ap[:, 0:ncols], bc, bc.ap[0:64, 0:ncols])
    kb.tt("dve", out_T, out_ap, ob, ob.ap[0:64, 0:ncols], ac.bcs, ac.bcs.ap[:, 0:ncols], ALU.mult)


def phase_l0_attn(kb, d, modT, lng, lnb, x1T, with_ctx=True):
    P = kb.P
    with ExitStack() as st:
        kb.stack = st
        wA = kb.sb([128, 8, 640], BF16)
        wArot = kb.sb([128, 8, 640], BF16)
        wR = kb.sb([128, 8, 1664], BF16)
        wout = kb.sb([64, 16, 1024], BF16)
        win = d["ab_w_in"].rearrange("(kc p) c -> p kc c", p=128)
        for slot, hd in enumerate([0, 4, 1, 5, 2, 6, 3, 7]):
            kb.dma("pool", wA.ap[:, :, slot * 64:(slot + 1) * 64], win[:, :, hd * 64:(hd + 1) * 64], [], [wA])
        kb.dma("pool", wA.ap[:, :, 512:640], win[:, :, 512:640], [], [wA])
        kb.dma("pool", wR.ap[:, :, 0:1024], win[:, :, 768:1792], [], [wR])
        kb.dma("pool", wR.ap[:, :, 1024:1152], win[:, :, 640:768], [], [wR])
        kb.dma("pool", wR.ap[:, :, 1152:1664], win[:, :, 1792:2304], [], [wR])
        kb.dma("pool", wout.ap, d["ab_w_out"].rearrange("(h p) c -> p h c", p=64), [], [wout])
        src = wA.ap.rearrange("p k (g two s) -> p (k g) two s", two=2, s=16)
        dst = wArot.ap.rearrange("p k (g two s) -> p (k g) two s", two=2, s=16)
        kb.ts("dve", wArot, dst[:, :, 0, :], wA, src[:, :, 1, :], -1.0, None, ALU.mult)
        kb.cp("dve", wArot, dst[:, :, 1, :], wA, src[:, :, 0, :])
        biasG = kb.sb([128, 5, 8, 128], BF16)
        biasE = kb.sb([128, 5, 8, 128], BF16)
        kb.dma("pool", biasG.ap, d["biasB"][:, 2], [], [biasG])
        maskA = kb.sb([128, 4, 128], BF16)
        kb.dma("pool", maskA.ap, d["maskA"], [], [maskA])
        sinkrow = kb.sb([1, 8, 128], F32)
        sk = kb.sb([1, 8], F32)
        kb.dma("sp", sk.ap, d["a_sink"], [], [sk])
        kb.act(sk, sk.ap, sk, sk.ap, AF.Exp)
        kb.cp("dve", sinkrow, sinkrow.ap, sk, sk.ap.unsqueeze(2).to_broadcast([1, 8, 128]))
        e64 = kb.sb([1, 65], F32)
        kb.memset("dve", e64, e64.ap, 0.0)
        kb.memset("dve", e64, e64.ap[:, 64:65], 1.0)
        def mkslot():
            s = dict(q=kb.sb([128, 8, 128], BF16), k=kb.sb([128, 5, 128], BF16), v=kb.sb([128, 10, 66], BF16))
            kb.memset("pool", s["v"], s["v"].ap[:, :, 64:65], 1.0)
            return s
        ring = [mkslot() for _ in range(6)]
        cslots = [mkslot() for _ in range(2)]
        xb = [kb.sb([128, 8, 128], F32) for _ in range(2)]
        hTs = [kb.sb([128, 8, 128], BF16) for _ in range(2)]
        tmpf = kb.sb([128, 8, 128], F32)
        ropeT = [kb.sb([128, 2, 128], F32) for _ in range(2)]
        r1 = kb.sb([128, 4, 128], F32)
        r2 = kb.sb([128, 4, 128], F32)
        oT = kb.sb([64, 16, 128], BF16)
        lc = LNCtx(kb)
        ac = AttCtx(kb, [kb.ps[3], kb.ps[4]], [kb.ps[5], kb.ps[6]], kb.ps[7])
        ps0, ps1, ps2 = kb.ps[0], kb.ps[1], kb.ps[2]
        xTv = d["xT"].rearrange("(kc p) t -> p kc t", p=128)
        cTv = d["ctxT"].rearrange("(kc p) t -> p kc t", p=128)
        x1v = x1T.ap.rearrange("(kc p) t -> p kc t", p=128)
        cnt = [0]
        import os
        LIM = int(os.environ.get("LIM", "99"))

        def project(src_ap, col, slot, rope_t):
            i = cnt[0]
            cnt[0] += 1
            xt = xb[i % 2]
            hT = hTs[i % 2]
            kb.dma("sp", xt.ap, src_ap, [], [xt])
            s_b = modT.ap[:, 8:16, col:col + 1].to_broadcast([128, 8, 128])
            sh_b = modT.ap[:, 0:8, col:col + 1].to_broadcast([128, 8, 128])
            kb.tt("dve", tmpf, tmpf.ap, xt, xt.ap, modT, s_b, ALU.mult)
            kb.tt("pool", hT, hT.ap, tmpf, tmpf.ap, modT, sh_b, ALU.add)
            rope = rope_t is not None
            if rope:
                rt = ropeT[i % 2]
                kb.dma("sp", rt.ap, d["ropeT"][:, :, rope_t * 128:(rope_t + 1) * 128], [], [rt])
            for c in range(4):
                for kc in range(8):
                    kb.mm(ps0, ps0.ap[:, c * 128:(c + 1) * 128], wA, wA.ap[:, kc, c * 128:(c + 1) * 128], hT, hT.ap[:, kc, :],
                          start=(kc == 0), stop=(kc == 7), acc=not (c == 0 and kc == 0))
            if rope:
                for c in range(4):
                    for kc in range(8):
                        kb.mm(ps1, ps1.ap[:, c * 128:(c + 1) * 128], wArot, wArot.ap[:, kc, c * 128:(c + 1) * 128], hT, hT.ap[:, kc, :],
                              start=(kc == 0), stop=(kc == 7), acc=not (c == 0 and kc == 0))
            for kc in range(8):
                kb.mm(ps2, ps2.ap[:, 0:128], wA, wA.ap[:, kc, 512:640], hT, hT.ap[:, kc, :], start=(kc == 0), stop=(kc == 7), acc=(kc != 0))
            if rope:
                for kc in range(8):
                    kb.mm(ps2, ps2.ap[:, 128:256], wArot, wArot.ap[:, kc, 512:640], hT, hT.ap[:, kc, :], start=(kc == 0), stop=(kc == 7), acc=True)
            for kc in range(8):
                kb.mm(ps2, ps2.ap[:, 256:384], hT, hT.ap[:, kc, :], wR, wR.ap[:, kc, 1024:1152], start=(kc == 0), stop=(kc == 7), acc=True)
            q, k, v = slot["q"], slot["k"], slot["v"]
            if rope:
                cosb = rt.ap[:, 0:1, :].to_broadcast([128, 4, 128])
                sinb = rt.ap[:, 1:2, :].to_broadcast([128, 4, 128])
                p0v = ps0.ap.rearrange("p (c t) -> p c t", t=128)
                p1v = ps1.ap.rearrange("p (c t) -> p c t", t=128)
                kb.tt("dve", r1, r1.ap, ps0, p0v, rt, cosb, ALU.mult)
                kb.tt("dve", r2, r2.ap, ps1, p1v, rt, sinb, ALU.mult)
                kb.tt("pool", q, q.ap[:, 0:4, :], r1, r1.ap, r2, r2.ap, ALU.add)
                kb.tt("dve", r1, r1.ap[:, 0, :], ps2, ps2.ap[:, 0:128], rt, rt.ap[:, 0, :], ALU.mult)
                kb.tt("dve", r2, r2.ap[:, 0, :], ps2, ps2.ap[:, 128:256], rt, rt.ap[:, 1, :], ALU.mult)
                kb.tt("pool", k, k.ap[:, 0, :], r1, r1.ap[:, 0, :], r2, r2.ap[:, 0, :], ALU.add)
            else:
                kb.cp("act", q, q.ap[:, 0:4, :], ps0, ps0.ap.rearrange("p (c t) -> p c t", t=128))
                kb.cp("act", k, k.ap[:, 0, :], ps2, ps2.ap[:, 0:128])
            kb.cp("act", v, v.ap[:, 0:2, 0:64], ps2, ps2.ap[:, 256:384].rearrange("p (h e) -> p h e", e=64))
            for c in range(4):
                for kc in range(8):
                    kb.mm(ps0, ps0.ap[:, c * 128:(c + 1) * 128], wR, wR.ap[:, kc, c * 128:(c + 1) * 128], hT, hT.ap[:, kc, :],
                          start=(kc == 0), stop=(kc == 7), acc=not (c == 0 and kc == 0))
            kb.cp("act", q, q.ap[:, 4:8, :], ps0, ps0.ap.rearrange("p (c t) -> p c t", t=128))
            for c in range(4):
                for kc in range(8):
                    kb.mm(ps1, ps1.ap[:, c * 128:(c + 1) * 128], wR, wR.ap[:, kc, 512 + c * 128:512 + (c + 1) * 128], hT, hT.ap[:, kc, :],
                          start=(kc == 0), stop=(kc == 7), acc=not (c == 0 and kc == 0))
            kb.cp("act", k, k.ap[:, 1:5, :], ps1, ps1.ap.rearrange("p (c t) -> p c t", t=128))
            for kc in range(8):
                kb.mm(ps2, ps2.ap, hT, hT.ap[:, kc, :], wR, wR.ap[:, kc, 1152:1664], start=(kc == 0), stop=(kc == 7), acc=(kc != 0))
            kb.cp("act", v, v.ap[:, 2:10, 0:64], ps2, ps2.ap.rearrange("p (h e) -> p h e", e=64))

        def attend(qslot, loc_tiles, bias_tab, mask_prev, mask_next, xres_ap, col, out_col):
            q = qslot["q"]
            for g in range(2):
                pb = 64 * g
                tiles = []
                for cs_ in cslots:
                    tiles.append(dict(qk=[(cs_["k"], cs_["k"].ap[pb:pb + 64, 0, :], q, q.ap[pb:pb + 64, 0:4, :], 0, 512)],
                                      bias=None, pv=[(cs_["v"], cs_["v"].ap[:, g, 0:65], 0, 512)]))
                if loc_tiles is not None:
                    for idx, mk in ((1, mask_prev), (2, None), (3, mask_next)):
                        sl = loc_tiles[idx]
                        bias = None
                        if mk is not None:
                            bias = (maskA, maskA.ap[:, mk:mk + 1, :].to_broadcast([128, 4, 128]))
                        tiles.append(dict(qk=[(sl["k"], sl["k"].ap[pb:pb + 64, 0, :], q, q.ap[pb:pb + 64, 0:4, :], 0, 512)],
                                          bias=bias, pv=[(sl["v"], sl["v"].ap[:, g, 0:65], 0, 512)]))
                for tl in tiles:
                    if tl["bias"] is not None:
                        tl["bias"] = (tl["bias"][0], tl["bias"][1])
                attn_unit_l0(kb, ac, tiles, 0.125, oT, oT.ap[:, 4 * g:4 * g + 4, :],
                             sink=(e64, sinkrow, sinkrow.ap[0:1, 4 * g:4 * g + 4, :]))
            if LIM == 4:
                return
            for u in range(2):
                tiles = []
                srcs = [(cs_, None) for cs_ in cslots]
                if loc_tiles is not None:
                    srcs += [(loc_tiles[t], t) for t in range(5)]
                for (sl, t) in srcs:
                    qk = []
                    pv = []
                    for hh in range(4):
                        h = 4 * u + hh
                        pb = 64 * (h % 2)
                        qk.append((sl["k"], sl["k"].ap[pb:pb + 64, 1 + h // 2, :], q, q.ap[pb:pb + 64, 4 + h // 2, :], hh * 128, 128))
                        pv.append((sl["v"], sl["v"].ap[:, 2 + h, 0:65], hh * 128, 128))
                    bias = None
                    if t is not None:
                        bias = (bias_tab, bias_tab.ap[:, t, 4 * u:4 * u + 4, :].rearrange("p (a two) q -> p two a q", two=2))
                    tiles.append(dict(qk=qk, bias=bias, pv=pv))
                attn_unit_b(kb, ac, tiles, 0.125, oT, oT.ap[:, 8 + 4 * u:8 + 4 * u + 4, :])
            if LIM == 5:
                return
            for dc in range(8):
                pb_ = ps0 if dc < 4 else ps1
                c0 = (dc % 4) * 128
                for h in range(16):
                    kb.mm(pb_, pb_.ap[:, c0:c0 + 128], wout, wout.ap[:, h, dc * 128:(dc + 1) * 128], oT, oT.ap[:, h, :],
                          start=(h == 0), stop=(h == 15), acc=not (dc % 4 == 0 and h == 0))

            if LIM == 6:
                return

            def y_fn(dc):
                pb_ = ps0 if dc < 4 else ps1
                return pb_, pb_.ap[:, (dc % 4) * 128:(dc % 4 + 1) * 128]
            ln_epilogue(kb, lc, y_fn, xres_ap, [], modT.ap[:, 16:24, col:col + 1], modT,
                        lng.ap[:, 0, :], lnb.ap[:, 0, :], (lng, lnb), x1v[:, :, out_col:out_col + 128], x1T, kb.ps[7])

        def attn_unit_l0(kb_, ac_, tiles, scale, out_T, out_ap3, sink):
            for tl in tiles:
                if tl["bias"] is not None:
                    tl["bias3"] = True
            attn_unit3(kb_, ac_, tiles, scale, out_T, out_ap3, sink)

        import os
        LIM = int(os.environ.get("LIM", "99"))
        if LIM == 0:
            kb.P.barrier()
            return
        for c in range(2):
            project(cTv[:, :, c * 128:(c + 1) * 128], 1, cslots[c], None)
        if LIM == 1:
            kb.P.barrier()
            return
        for Tt in range(4):
            project(xTv[:, :, Tt * 128:(Tt + 1) * 128], 0, ring[Tt % 6], Tt)
        if LIM == 2:
            kb.P.barrier()
            return
        for Tt in range(4, 36):
            if LIM in (3, 4, 5, 6) and Tt == 5:
                kb.P.barrier()
                return
            project(xTv[:, :, Tt * 128:(Tt + 1) * 128], 0, ring[Tt % 6], Tt)
            j = Tt - 4
            if j in (0, 1, 30, 31):
                var = {0: 0, 1: 1, 30: 3, 31: 4}[j]
                kb.dma("pool", biasE.ap, d["biasB"][:, var], [], [biasE])
                btab = biasE
            else:
                btab = biasG
            loc = [ring[(j + t) % 6] for t in range(5)]
            attend(ring[(j + 2) % 6], loc, btab, 0 if j == 0 else 1, 3 if j == 31 else 2,
                   xTv[:, :, (j + 2) * 128:(j + 3) * 128], 0, j * 128)
        if with_ctx:
            for c in range(2):
                attend(cslots[c], None, None, None, None, cTv[:, :, c * 128:(c + 1) * 128], 1, 4096 + c * 128)
        kb.P.barrier()


def attn_unit3(kb, ac, tiles, scale, out_T, out_ap3, sink):
    ob = ac.o_banks[ac.oi % len(ac.o_banks)]
    ac.oi += 1
    nt = len(tiles)
    for ti, tl in enumerate(tiles):
        sbk = ac.s_banks[ac.si % len(ac.s_banks)]
        ac.si += 1
        first = True
        for (kT, kap, qT, qap, c0, n) in tl["qk"]:
            kb.mm(sbk, sbk.ap[:, c0:c0 + n], kT, kap, qT, qap, start=True, stop=True, acc=(not first))
            first = False
        E = ac.E[ac.ei % 3]
        ac.ei += 1
        if tl["bias"] is not None:
            bT, bap = tl["bias"]
            sbuf = ac.sb_[ac.bi % 2]
            ac.bi += 1
            kb.stt("dve", sbuf, sbuf.ap.rearrange("p (c t) -> p c t", t=128), sbk, sbk.ap.rearrange("p (c t) -> p c t", t=128),
                   scale, bT, bap, ALU.mult, ALU.add)
            kb.act(E, E.ap, sbuf, sbuf.ap, AF.Exp)
        else:
            kb.act(E, E.ap, sbk, sbk.ap, AF.Exp, scale=scale)
        last = (ti == nt - 1) and sink is None
        for pi, (vT, vap, c0, n) in enumerate(tl["pv"]):
            kb.mm(ob, ob.ap[0:65, c0:c0 + n], vT, vap, E, E.ap[:, c0:c0 + n], start=(ti == 0 and pi == 0), stop=last,
                  acc=not (ti == 0 and pi == 0), skip=True, inc=last)
    if sink is not None:
        e64, srow_T, srow_ap = sink
        kb.mm(ob, ob.ap[0:65, :], e64, e64.ap, srow_T, srow_ap, start=False, stop=True, acc=True, skip=True)
    zr = ac.zr
    kb.op("dve", lambda e: e.reciprocal(out=zr.ap[64:65, :], in_=ob.ap[64:65, :]), [ob], [zr])
    bc = ac.bc
    kb.mm(bc, bc.ap[0:64, :], kb.ones_f, kb.ones_f.ap[64:65, 0:64], zr, zr.ap[64:65, :])
    kb.cp("act", ac.bcs, ac.bcs.ap, bc, bc.ap[0:64, :])
    kb.tt("dve", out_T, out_ap3, ob, ob.ap[0:64, :].rearrange("p (c t) -> p c t", t=128),
          ac.bcs, ac.bcs.ap.rearrange("p (c t) -> p c t", t=128), ALU.mult)


def phase_moe(kb, d, modT, lng, lnb, xin, passes, ctx_tile0, out_fn):
    BIG = 1.0e30
    with ExitStack() as st:
        kb.stack = st
        PT = max(len(p) for p in passes)
        wr = kb.sb([128, 8, 36], F32)
        kb.dma("sp", wr.ap[:, :, 0:4], d["w_group"].rearrange("(kc p) g -> p kc g", p=128), [], [wr])
        kb.dma("sp", wr.ap[:, :, 4:36], d["w_er"].rearrange("(kc p) g -> p kc g", p=128), [], [wr])
        br = kb.sb([36, 1], F32)
        kb.dma("sp", br.ap[0:4, :], d["b_group"], [], [br])
        kb.dma("sp", br.ap[4:36, :], d["b_er"], [], [br])
        ident = kb.sb([128, 128], F32)
        kb.dma("sp", ident.ap, d["ident"], [], [ident])
        sel = kb.sb([32, 32, 128], BF16)
        kb.dma("pool", sel.ap, d["sel"], [], [sel])
        acc = kb.sb([128, 8, PT * 128], F32)
        hT = kb.sb([128, 8, PT * 128], BF16)
        WdT = kb.sb([32, PT * 128], BF16)
        wgu = [kb.sb([128, 8, 1024], BF16) for _ in range(2)]
        wdn = [kb.sb([128, 4, 1024], BF16) for _ in range(2)]
        actT = [kb.sb([128, 4, 512], BF16) for _ in range(2)]
        sg = [kb.sb([128, 512], F32) for _ in range(2)]
        tmp = [kb.sb([128, 512], F32) for _ in range(2)]
        wbs = [kb.sb([128, 512], F32) for _ in range(2)]
        xt = [kb.sb([128, 8, 128], F32) for _ in range(2)]
        hf = kb.sb([128, 8, 128], F32)
        lT = kb.sb([36, 128], F32)
        Lt = kb.sb([128, 36], F32)
        sm = kb.sb([128, 16], F32)
        gm = kb.sb([128, 4], F32)
        pen = kb.sb([128, 4], F32)
        elm = kb.sb([128, 32], F32)
        elm2 = kb.sb([128, 32], F32)
        m1 = kb.sb([128, 32], F32)
        ew = kb.sb([128, 32], F32)
        Wd = kb.sb([128, 32], F32)
        lc = LNCtx(kb)
        xv = xin.ap.rearrange("(kc p) t -> p kc t", p=128)
        wguv = d["w_gate_up"]
        wdnv = d["w_down"]
        ps = kb.ps
        ecount = 0
        xi = 0
        for tiles in passes:
            for li, tile in enumerate(tiles):
                col = 1 if tile >= ctx_tile0 else 0
                x_ = xt[xi % 2]
                xi += 1
                kb.dma("sp", x_.ap, xv[:, :, tile * 128:(tile + 1) * 128], [xin], [x_])
                s_b = modT.ap[:, 32:40, col:col + 1].to_broadcast([128, 8, 128])
                sh_b = modT.ap[:, 24:32, col:col + 1].to_broadcast([128, 8, 128])
                kb.tt("dve", hf, hf.ap, x_, x_.ap, modT, s_b, ALU.mult)
                kb.tt("pool", hf, hf.ap, hf, hf.ap, modT, sh_b, ALU.add)
                kb.cp("act", hT, hT.ap[:, :, li * 128:(li + 1) * 128], hf, hf.ap)
                p7 = ps[7]
                for kc in range(8):
                    kb.mm(p7, p7.ap[0:36, 0:128], wr, wr.ap[:, kc, :], hf, hf.ap[:, kc, :], start=(kc == 0), stop=(kc == 7))
                kb.act(lT, lT.ap, p7, p7.ap[0:36, 0:128], AF.Identity, scale=1.0, bias=br.ap[:, 0:1], extra=[br])
                kb.mm(p7, p7.ap[:, 128:164], lT, lT.ap, ident, ident.ap[0:36, 0:36], acc=True)
                kb.cp("dve", Lt, Lt.ap, p7, p7.ap[:, 128:164])
                gl = Lt.ap[:, 0:4]
                el = Lt.ap[:, 4:36]
                kb.op("dve", lambda e: e.reduce_max(out=sm.ap[:, 0:1], in_=gl, axis=AX.X), [Lt], [sm])
                kb.ts("dve", sm, sm.ap[:, 1:2], sm, sm.ap[:, 0:1], -1.0, None, ALU.mult)
                kb.op("act", lambda e: e.activation(out=gm.ap, in_=gl, func=AF.Exp, bias=sm.ap[:, 1:2], scale=1.0, accum_out=sm.ap[:, 2:3]),
                      [Lt, sm], [gm, sm])
                kb.op("dve", lambda e: e.reciprocal(out=sm.ap[:, 3:4], in_=sm.ap[:, 2:3]), [sm], [sm])
                kb.ts("dve", gm, gm.ap, Lt, gl, sm.ap[:, 0:1], None, ALU.is_ge, extra=[sm])
                kb.ts("dve", pen, pen.ap, gm, gm.ap, BIG, -BIG, ALU.mult, ALU.add)
                kb.tt("dve", elm, elm.ap.rearrange("p (g e) -> p g e", e=8), Lt, el.rearrange("p (g e) -> p g e", e=8),
                      pen, pen.ap.unsqueeze(2).to_broadcast([128, 4, 8]), ALU.add)
                kb.op("dve", lambda e: e.reduce_max(out=sm.ap[:, 4:5], in_=elm.ap, axis=AX.X), [elm], [sm])
                kb.ts("dve", m1, m1.ap, elm, elm.ap, sm.ap[:, 4:5], None, ALU.is_ge, extra=[sm])
                kb.stt("dve", elm2, elm2.ap, m1, m1.ap, -BIG, elm, elm.ap, ALU.mult, ALU.add)
                kb.op("dve", lambda e: e.reduce_max(out=sm.ap[:, 6:7], in_=elm2.ap, axis=AX.X), [elm2], [sm])
                kb.ts("dve", m1, m1.ap, elm, elm.ap, sm.ap[:, 6:7], None, ALU.is_ge, extra=[sm])
                kb.ts("dve", sm, sm.ap[:, 5:6], sm, sm.ap[:, 4:5], -1.0, None, ALU.mult)
                kb.act(ew, ew.ap, elm, elm.ap, AF.Exp, scale=1.0, bias=sm.ap[:, 5:6], extra=[sm])
                kb.act(sm, sm.ap[:, 7:8], sm, sm.ap[:, 6:7], AF.Exp, scale=1.0, bias=sm.ap[:, 5:6])
                kb.ts("dve", sm, sm.ap[:, 7:8], sm, sm.ap[:, 7:8], 1.0, None, ALU.add)
                kb.op("dve", lambda e: e.reciprocal(out=sm.ap[:, 7:8], in_=sm.ap[:, 7:8]), [sm], [sm])
                kb.tt("dve", sm, sm.ap[:, 8:9], sm, sm.ap[:, 7:8], sm, sm.ap[:, 3:4], ALU.mult)
                kb.stt("dve", Wd, Wd.ap, ew, ew.ap, sm.ap[:, 8:9], m1, m1.ap, ALU.mult, ALU.mult, extra=[sm])
                kb.mm(p7, p7.ap[0:32, 256:384], Wd, Wd.ap, ident, ident.ap, acc=True)
                kb.cp("act", WdT, WdT.ap[:, li * 128:(li + 1) * 128], p7, p7.ap[0:32, 256:384])
            nt = len(tiles)
            groups = []
            c = 0
            while c < nt:
                n = min(4, nt - c)
                groups.append((c * 128, n * 128))
                c += n
            gi = 0
            for e in range(32):
                wg = wgu[ecount % 2]
                wd = wdn[ecount % 2]
                ecount += 1
                gv = wguv[e].rearrange("(kc p) f -> p kc f", p=128)
                kb.dma("pool", wg.ap[:, 0:4, :], gv[:, 0:4, :], [], [wg])
                kb.dma("pool", wg.ap[:, 4:8, :], gv[:, 4:8, :], [], [wg])
                kb.dma("pool", wd.ap, wdnv[e].rearrange("(kc p) f -> p kc f", p=128), [], [wd])
                for (c0, n) in groups:
                    aT = actT[gi % 2]
                    wb_ = wbs[gi % 2]
                    gi += 1
                    kb.mm(ps[4], ps[4].ap[:, 0:n], sel, sel.ap[:, e, :], WdT, WdT.ap[:, c0:c0 + n])
                    kb.cp("act", wb_, wb_.ap[:, 0:n], ps[4], ps[4].ap[:, 0:n])
                    for j in range(4):
                        G = ps[j % 2]
                        U = ps[2 + j % 2]
                        for kc in range(8):
                            kb.mm(G, G.ap[:, 0:n], wg, wg.ap[:, kc, j * 128:(j + 1) * 128], hT, hT.ap[:, kc, c0:c0 + n],
                                  start=(kc == 0), stop=(kc == 7))
                        for kc in range(8):
                            kb.mm(U, U.ap[:, 0:n], wg, wg.ap[:, kc, 512 + j * 128:512 + (j + 1) * 128], hT, hT.ap[:, kc, c0:c0 + n],
                                  start=(kc == 0), stop=(kc == 7))
                        s_ = sg[j % 2]
                        t_ = tmp[j % 2]
                        kb.act(s_, s_.ap[:, 0:n], G, G.ap[:, 0:n], AF.Silu)
                        kb.tt("dve", t_, t_.ap[:, 0:n], U, U.ap[:, 0:n], s_, s_.ap[:, 0:n], ALU.mult)
                        kb.tt("pool", aT, aT.ap[:, j, 0:n], t_, t_.ap[:, 0:n], wb_, wb_.ap[:, 0:n], ALU.mult)
                    for dc in range(8):
                        Y = ps[5 + dc % 2]
                        for j in range(4):
                            kb.mm(Y, Y.ap[:, 0:n], wd, wd.ap[:, j, dc * 128:(dc + 1) * 128], aT, aT.ap[:, j, 0:n],
                                  start=(j == 0), stop=(j == 3))
                        if e == 0:
                            kb.cp("dve", acc, acc.ap[:, dc, c0:c0 + n], Y, Y.ap[:, 0:n])
                        else:
                            kb.tt("dve", acc, acc.ap[:, dc, c0:c0 + n], Y, Y.ap[:, 0:n], acc, acc.ap[:, dc, c0:c0 + n], ALU.add)
            for li, tile in enumerate(tiles):
                col = 1 if tile >= ctx_tile0 else 0
                oap, oT_ = out_fn(tile)

                def y_fn(dc, li=li):
                    return acc, acc.ap[:, dc, li * 128:(li + 1) * 128]
                ln_epilogue(kb, lc, y_fn, xv[:, :, tile * 128:(tile + 1) * 128], [xin], modT.ap[:, 40:48, col:col + 1], modT,
                            lng.ap[:, 1, :], lnb.ap[:, 1, :], (lng, lnb), oap, oT_, ps[7])
        kb.P.barrier()


def _rope_np(pos, dim):
    pos = np.asarray(pos)
    pos_r = (pos // 64).astype(np.float32)
    pos_c = (pos % 64).astype(np.float32)
    quarter = dim // 4
    inv = np.power(np.float32(10000.0), -(np.arange(quarter, dtype=np.float32) / np.float32(quarter))).astype(np.float32)
    ang_r = pos_r[:, None] * inv
    ang_c = pos_c[:, None] * inv
    ang = np.concatenate([ang_r, ang_r, ang_c, ang_c], -1).astype(np.float32)
    return np.cos(ang).astype(np.float32), np.sin(ang).astype(np.float32)


def _ext_tokens(r):
    base = r * 4096 - 256 + np.arange(NEXT)
    if r == 0:
        base[0:256] = 256 + np.arange(256)
    if r == 3:
        base[4352:4608] = 248 * 64 + np.arange(256)
    return base


def _bias_tables(rpb, r):
    out = np.full((128, 5, 5, 8, 128), NEG, np.float32)
    kk = np.arange(128)
    kr, kc = kk // 64, kk % 64
    qr, qc = kk // 64, kk % 64
    cs = np.clip(qc - 8, 0, 48)
    validc = (kc[:, None] >= cs[None, :]) & (kc[:, None] < cs[None, :] + 16)
    dc = np.clip(kc[:, None] - qc[None, :], -15, 15) + 15
    for vi, j in enumerate([0, 1, 2, 30, 31]):
        for t in range(5):
            ext_row = 2 * j + 2 * t + kr
            lr = 2 * j + qr
            xw = ext_row[:, None] - lr[None, :]
            validr = (xw >= 0) & (xw <= 7)
            if r == 0:
                act = np.where(ext_row < 4, ext_row + 4, ext_row - 4)
            elif r == 3:
                act = np.where(ext_row >= 68, 248 + (ext_row - 68), 188 + ext_row)
            else:
                act = r * 64 - 4 + ext_row
            rq = r * 64 + lr
            dr = act[:, None] - rq[None, :] + 7
            ws = np.clip(rq - 4, 0, 248)
            inwin = (act[:, None] >= ws[None, :]) & (act[:, None] < ws[None, :] + 8)
            assert np.array_equal(validr & inwin, validr), (r, j, t)
            valid = validr & validc
            drc = np.clip(dr, 0, 14)
            vals = rpb[:, drc, dc]
            out[:, vi, t, :, :] = np.where(valid[:, None, :], vals.transpose(1, 0, 2), np.float32(NEG))
    return out


def _mask_a(r):
    kk = np.arange(128)
    prev = np.where(kk[:, None] >= kk[None, :], 0.0, NEG).astype(np.float32)
    nxt = np.where(kk[:, None] <= kk[None, :], 0.0, NEG).astype(np.float32)
    allneg = np.full((128, 128), NEG, np.float32)
    m = np.stack([allneg if r == 0 else prev, prev, nxt, allneg if r == 3 else nxt], axis=1)
    return np.ascontiguousarray(m)


def _fm(v, nch):
    return np.ascontiguousarray(np.asarray(v, np.float32).reshape(nch, 128).T)


def _moe_inputs(inp, l):
    sel = np.zeros((32, 32, 128), np.float32)
    for e in range(32):
        sel[e, e, :] = 1.0
    return {
        "w_group": np.ascontiguousarray(inp["w_group"][l]),
        "b_group": np.ascontiguousarray(inp["b_group"][l].reshape(4, 1)),
        "w_er": np.ascontiguousarray(inp["w_exp_router"][l]),
        "b_er": np.ascontiguousarray(inp["b_exp_router"][l].reshape(32, 1)),
        "w_gate_up": np.ascontiguousarray(inp["w_gate_up"][l]),
        "w_down": np.ascontiguousarray(inp["w_down"][l]),
        "ident": np.eye(128, dtype=np.float32),
        "sel": sel,
    }


def _common_inputs(inp, l, b):
    cT = np.stack([_fm(inp["c"][b], 8), _fm(inp["c_ctx"], 8)], axis=2)
    lg = np.stack([_fm(inp["ln_g"][l, 0], 8), _fm(inp["ln_g"][l, 1], 8)], axis=1)
    lb = np.stack([_fm(inp["ln_b"][l, 0], 8), _fm(inp["ln_b"][l, 1], 8)], axis=1)
    return {
        "cT": np.ascontiguousarray(cT),
        "w_ada": np.ascontiguousarray(inp["w_ada"][l]),
        "b_adaT": _fm(inp["b_ada"][l], 48),
        "ln_gT": np.ascontiguousarray(lg),
        "ln_bT": np.ascontiguousarray(lb),
    }


def _declare_moe(kb, sfx=""):
    return {
        "w_group": kb.din("w_group" + sfx, [1024, 4]), "b_group": kb.din("b_group" + sfx, [4, 1]),
        "w_er": kb.din("w_er" + sfx, [1024, 32]), "b_er": kb.din("b_er" + sfx, [32, 1]),
        "w_gate_up": kb.din("w_gate_up" + sfx, [32, 1024, 1024]), "w_down": kb.din("w_down" + sfx, [32, 512, 1024]),
        "ident": kb.din("ident" + sfx, [128, 128]), "sel": kb.din("sel" + sfx, [32, 32, 128]),
    }


def _declare_common(kb, sfx=""):
    return {
        "cT": kb.din("cT" + sfx, [128, 8, 2]), "w_ada": kb.din("w_ada" + sfx, [1024, 6144]),
        "b_adaT": kb.din("b_adaT" + sfx, [128, 48]), "ln_gT": kb.din("ln_gT" + sfx, [128, 2, 8]), "ln_bT": kb.din("ln_bT" + sfx, [128, 2, 8]),
    }


def build_launch1(debug=False, stop=None):
    kb = KB()
    d = _declare_common(kb)
    d.update(_declare_moe(kb))
    d.update({
        "xT": kb.din("xT", [1024, NEXT]), "ctxT": kb.din("ctxT", [1024, 256]),
        "ab_w_in": kb.din("ab_w_in", [1024, 2304]), "ab_w_out": kb.din("ab_w_out", [1024, 1024]),
        "a_sink": kb.din("a_sink", [1, 8]), "biasB": kb.din("biasB", [128, 5, 5, 8, 128]),
        "maskA": kb.din("maskA", [128, 4, 128]), "ropeT": kb.din("ropeT", [128, 2, NEXT]),
    })
    out = T(kb.dout("x2T", [1024, 4352]))
    if debug:
        x1T = T(kb.dout("x1T", [1024, 4352]))
    else:
        x1T = kb.dscr("x1T", [1024, 4352], F32)
    modT = phase_mod(kb, d["w_ada"], d["b_adaT"], d["cT"])
    lng, lnb = load_ln(kb, d["ln_gT"], d["ln_bT"])
    if stop == "mod":
        dbg = T(kb.dout("modT", [128, 48, 2]))
        kb.dma("sp", dbg.ap, modT.ap, [modT], [dbg])
        kb.P.barrier()
        return kb
    phase_l0_attn(kb, d, modT, lng, lnb, x1T)
    if stop == "l0":
        kb.P.barrier()
        return kb
    ov = out.ap.rearrange("(kc p) t -> p kc t", p=128)
    passes = [list(range(0, 10)), list(range(10, 18)), list(range(18, 26)), list(range(26, 34))]
    phase_moe(kb, d, modT, lng, lnb, x1T, passes, 32, lambda tile: (ov[:, :, tile * 128:(tile + 1) * 128], out))
    kb.P.barrier()
    return kb


def launch1_inputs(inp):
    maps = []
    rpb = np.asarray(inp["b_rpb"][0], np.float32)
    moe = _moe_inputs(inp, 0)
    tabs = {r: _bias_tables(rpb, r) for r in range(4)}
    for core in range(8):
        b, r = core // 4, core % 4
        et = _ext_tokens(r)
        cos, sin = _rope_np(et, 64)
        rope = np.stack([np.concatenate([cos.T, cos.T], 0), np.concatenate([sin.T, sin.T], 0)], axis=1)
        m = _common_inputs(inp, 0, b)
        m.update(moe)
        m.update({
            "xT": np.ascontiguousarray(inp["x"][b][et].T),
            "ctxT": np.ascontiguousarray(inp["ctx"][b].T),
            "ab_w_in": np.ascontiguousarray(inp["ab_w_in"][0]),
            "ab_w_out": np.ascontiguousarray(inp["ab_w_out"][0]),
            "a_sink": np.ascontiguousarray(inp["a_sink"][0].reshape(1, 8)),
            "biasB": tabs[r], "maskA": _mask_a(r), "ropeT": np.ascontiguousarray(rope),
        })
        maps.append(m)
    return maps


def attn_unit_b(kb, ac, tiles, scale, out_T, out_ap3):
    ob = ac.o_banks[ac.oi % len(ac.o_banks)]
    ac.oi += 1
    X, Y = ac.s_banks[0], ac.s_banks[1]
    bi0 = kb.ps.index(X)
    assert kb.ps.index(Y) == bi0 + 1
    pview = kb.psall[:, bi0 * 512:(bi0 + 2) * 512].rearrange("p (b c) -> p b c", b=2)[:, :, 0:256]
    nt = len(tiles)
    for ti, tl in enumerate(tiles):
        for hh, (kT, kap, qT, qap, c0, n) in enumerate(tl["qk"]):
            bk = X if hh % 2 == 0 else Y
            cc = (hh // 2) * 128
            kb.mm(bk, bk.ap[:, cc:cc + 128], kT, kap, qT, qap, start=True, stop=True, acc=(hh >= 2))
        E = ac.E[ac.ei % 3]
        ac.ei += 1
        ev = E.ap.rearrange("p (b c) -> p b c", b=2)
        if tl["bias"] is not None:
            bT, bap = tl["bias"]
            sbuf = ac.sb_[ac.bi % 2]
            ac.bi += 1
            for b_, bk_ in enumerate((X, Y)):
                kb.stt("dve", sbuf, sbuf.ap[:, b_ * 256:(b_ + 1) * 256].rearrange("p (a q) -> p a q", a=2),
                       bk_, bk_.ap[:, 0:256].rearrange("p (a q) -> p a q", a=2), scale, bT, bap[:, b_, :, :], ALU.mult, ALU.add)
            kb.act(E, E.ap, sbuf, sbuf.ap, AF.Exp)
        else:
            kb.op("act", lambda e: e.activation(out=ev, in_=pview, func=AF.Exp, scale=scale), [X, Y], [E])
        last = (ti == nt - 1)
        for hh, (vT, vap, c0, n) in enumerate(tl["pv"]):
            ec = (hh % 2) * 256 + (hh // 2) * 128
            kb.mm(ob, ob.ap[0:65, hh * 128:(hh + 1) * 128], vT, vap, E, E.ap[:, ec:ec + 128], start=(ti == 0 and hh == 0), stop=last,
                  acc=not (ti == 0 and hh == 0), skip=True, inc=last)
    zr = ac.zr
    kb.op("dve", lambda e: e.reciprocal(out=zr.ap[64:65, :], in_=ob.ap[64:65, :]), [ob], [zr])
    bc = ac.bc
    kb.mm(bc, bc.ap[0:64, :], kb.ones_f, kb.ones_f.ap[64:65, 0:64], zr, zr.ap[64:65, :])
    kb.cp("act", ac.bcs, ac.bcs.ap, bc, bc.ap[0:64, :])
    kb.tt("dve", out_T, out_ap3, ob, ob.ap[0:64, :].rearrange("p (c t) -> p c t", t=128),
          ac.bcs, ac.bcs.ap.rearrange("p (c t) -> p c t", t=128), ALU.mult)


NK = 16640
NKT = NK // 128


class Banks:
    def __init__(self, kb, idx):
        self.kb = kb
        self.idx = list(idx)
        self.i = 0

    def get(self):
        b = self.kb.ps[self.idx[self.i % len(self.idx)]]
        self.i += 1
        return b


def l1_scratch(kb):
    return dict(
        kTm=kb.dscr("kTm", [8, 96, NK], BF16), Vm=kb.dscr("Vm", [8, 128, NKT, 66], BF16),
        kTd=kb.dscr("kTd", [4, 128, NK], BF16), Vd=kb.dscr("Vd", [4, 128, NKT, 128], BF16),
        qTm=kb.dscr("qTm", [8, 96, 4096], BF16), qTd=kb.dscr("qTd", [4, 128, 4096], BF16),
        oTs=kb.dscr("oTs", [12, 128, 4096], BF16), x1b=kb.dscr("x1b", [1024, 4096], F32))


def phase_l1_proj(kb, d, modT, scr):
    with ExitStack() as st:
        kb.stack = st
        win = d["cd_w_in"].rearrange("(kc p) c -> p kc c", p=128)
        wq_c = kb.sb([128, 8, 384], BF16)
        wkv = kb.sb([128, 8, 256], BF16)
        wkr = kb.sb([128, 8, 96], BF16)
        wkr_r = kb.sb([128, 8, 96], BF16)
        wdq = kb.sb([128, 8, 512], BF16)
        wdq_r = kb.sb([128, 8, 512], BF16)
        wdk = kb.sb([128, 8, 512], BF16)
        wdk_r = kb.sb([128, 8, 512], BF16)
        wdv = kb.sb([128, 8, 512], BF16)
        kb.dma("pool", wq_c.ap, win[:, :, 0:384], [], [wq_c])
        kb.dma("pool", wkv.ap, win[:, :, 384:640], [], [wkv])
        kb.memset("dve", wkr, wkr.ap, 0.0)
        kb.memset("dve", wkr_r, wkr_r.ap, 0.0)
        kb.dma("pool", wkr.ap[:, :, 64:96], win[:, :, 640:672], [], [wkr])
        kb.dma("pool", wdq.ap, win[:, :, 672:1184], [], [wdq])
        kb.dma("pool", wdk.ap, win[:, :, 1184:1696], [], [wdk])
        kb.dma("pool", wdv.ap, win[:, :, 1696:2208], [], [wdv])

        def mkrot(dst, src, dap, sap, s_):
            sv = sap.rearrange("p k (g two s) -> p k g two s", two=2, s=s_)
            dv = dap.rearrange("p k (g two s) -> p k g two s", two=2, s=s_)
            for kc in range(sap.shape[1]):
                kb.ts("dve", dst, dv[:, kc, :, 0, :], src, sv[:, kc, :, 1, :], -1.0, None, ALU.mult)
                kb.cp("dve", dst, dv[:, kc, :, 1, :], src, sv[:, kc, :, 0, :])
        mkrot(wkr_r, wkr, wkr_r.ap[:, :, 64:96], wkr.ap[:, :, 64:96], 8)
        mkrot(wdq_r, wdq, wdq_r.ap, wdq.ap, 16)
        mkrot(wdk_r, wdk, wdk_r.ap, wdk.ap, 16)
        wuq = kb.sb([128, 3, 768], BF16)
        wuq_r = kb.sb([128, 3, 768], BF16)
        kb.dma("pool", wuq.ap, d["c_w_uq"].rearrange("(kc p) c -> p kc c", p=128), [], [wuq])
        kb.memset("dve", wuq_r, wuq_r.ap, 0.0)
        for h in range(8):
            mkrot(wuq_r, wuq, wuq_r.ap[:, :, h * 96 + 64:h * 96 + 96], wuq.ap[:, :, h * 96 + 64:h * 96 + 96], 8)
        wukv = kb.sb([128, 2, 1024], BF16)
        kb.dma("pool", wukv.ap, d["c_w_ukv"].rearrange("(kc p) c -> p kc c", p=128), [], [wukv])
        qg = kb.sb([128, 3], F32)
        kvg = kb.sb([128, 2], F32)
        kb.dma("sp", qg.ap, d["c_q_normT"], [], [qg])
        kb.dma("sp", kvg.ap, d["c_kv_normT"], [], [kvg])
        eps6 = kb.sb([128, 1], F32)
        kb.memset("dve", eps6, eps6.ap, 1e-6)

        xb = [kb.sb([128, 8, 512], F32) for _ in range(2)]
        hTs = [kb.sb([128, 8, 512], BF16) for _ in range(2)]
        r64 = [kb.sb([128, 2, 512], F32) for _ in range(2)]
        r32 = [kb.sb([128, 2, 512], F32) for _ in range(2)]
        sq = kb.sb([128, 3, 512], BF16)
        rs = kb.sb([128, 512], F32)
        ckvn = kb.sb([128, 2, 512], BF16)
        cqn = kb.sb([128, 3, 512], BF16)
        kn = kb.sb([64, 8, 512], BF16)
        krs = kb.sb([96, 512], BF16)
        t1 = kb.sb([128, 2, 512], F32)
        t2 = kb.sb([128, 2, 512], F32)
        dks = [kb.sb([128, 4, 512], BF16) for _ in range(2)]
        vms = kb.sb([128, 8, 4, 66], BF16)
        kb.memset("pool", vms, vms.ap[:, :, :, 64:66], 1.0)
        vds = kb.sb([128, 4, 4, 128], BF16)
        qs = [kb.sb([96, 512], BF16) for _ in range(2)]
        bk = Banks(kb, range(8))
        xv = d["x1T"].rearrange("(kc p) t -> p kc t", p=128)
        cv = d["ctx1T"].rearrange("(kc p) t -> p kc t", p=128)
        xrd = d.get("x_reads", [])
        di = [0]

        def rms(chunks, nch, gT, dstT, n):
            for c, (pt, pap) in enumerate(chunks):
                kb.act(sq, sq.ap[:, c, 0:n], pt, pap, AF.Square)
            ssb = bk.get()
            for c in range(nch):
                kb.mm(ssb, ssb.ap[:, 0:n], kb.ones_b, kb.ones_b.ap, sq, sq.ap[:, c, 0:n], start=(c == 0), stop=(c == nch - 1))
            kb.act(rs, rs.ap[:, 0:n], ssb, ssb.ap[:, 0:n], AF.Ln, scale=1.0 / (128 * nch), bias=eps6.ap[:, 0:1], extra=[eps6])
            kb.act(rs, rs.ap[:, 0:n], rs, rs.ap[:, 0:n], AF.Exp, scale=-0.5)
            for c, (pt, pap) in enumerate(chunks):
                kb.stt("dve", dstT, dstT.ap[:, c, 0:n], pt, pap, gT.ap[:, c:c + 1], rs, rs.ap[:, 0:n], ALU.mult, ALU.mult, extra=[gT])

        def proj_fm(w, c0, hT, n, M=128):
            b = bk.get()
            for kc in range(8):
                kb.mm(b, b.ap[0:M, 0:n], w, w.ap[:, kc, c0:c0 + M], hT, hT.ap[:, kc, 0:n], start=(kc == 0), stop=(kc == 7))
            return b

        def rope_pair(dstT, dst_ap, p1, p2, tab, rows, n, eng2="pool"):
            lo, hi = rows
            kb.tt("dve", t1, t1.ap[lo:hi, 0, 0:n], p1, p1.ap[lo:hi, 0:n], tab, tab.ap[lo:hi, 0, 0:n], ALU.mult)
            kb.tt("dve", t2, t2.ap[lo:hi, 0, 0:n], p2, p2.ap[lo:hi, 0:n], tab, tab.ap[lo:hi, 1, 0:n], ALU.mult)
            kb.tt(eng2, dstT, dst_ap, t1, t1.ap[lo:hi, 0, 0:n], t2, t2.ap[lo:hi, 0, 0:n], ALU.add)

        for g in range(33):
            ctx = (g == 32)
            own = (g < 8)
            n = 256 if ctx else 512
            col = 1 if ctx else 0
            k0 = g * 512
            i = di[0]
            di[0] += 1
            xt, hT = xb[i % 2], hTs[i % 2]
            src = cv if ctx else xv[:, :, k0:k0 + 512]
            kb.dma("sp", xt.ap[:, :, 0:n], src, xrd, [xt])
            s_b = modT.ap[:, 8:16, col:col + 1].to_broadcast([128, 8, n])
            sh_b = modT.ap[:, 0:8, col:col + 1].to_broadcast([128, 8, n])
            kb.tt("dve", xt, xt.ap[:, :, 0:n], xt, xt.ap[:, :, 0:n], modT, s_b, ALU.mult)
            kb.tt("pool", hT, hT.ap[:, :, 0:n], xt, xt.ap[:, :, 0:n], modT, sh_b, ALU.add)
            if not ctx:
                a64, a32 = r64[i % 2], r32[i % 2]
                kb.dma("sp", a64.ap, d["rope64"][:, :, k0:k0 + 512], [], [a64])
                kb.dma("sp", a32.ap, d["rope32"][:, :, k0:k0 + 512], [], [a32])
            pc = [proj_fm(wkv, c * 128, hT, n) for c in range(2)]
            rms([(p, p.ap[:, 0:n]) for p in pc], 2, kvg, ckvn, n)
            for h in range(8):
                b = bk.get()
                for c in range(2):
                    kb.mm(b, b.ap[0:64, 0:n], wukv, wukv.ap[:, c, h * 128:h * 128 + 64], ckvn, ckvn.ap[:, c, 0:n], start=(c == 0), stop=(c == 1))
                kb.cp("act", kn, kn.ap[:, h, 0:n], b, b.ap[0:64, 0:n])
            kb.dma("pool", scr["kTm"].ap[:, 0:64, k0:k0 + n].rearrange("h p n -> p h n"), kn.ap[:, :, 0:n], [kn], [scr["kTm"]])
            p1 = proj_fm(wkr, 0, hT, n, M=96)
            if not ctx:
                p2 = proj_fm(wkr_r, 0, hT, n, M=96)
                rope_pair(krs, krs.ap[64:96, 0:n], p1, p2, a32, (64, 96), n)
            else:
                kb.cp("act", krs, krs.ap[64:96, 0:n], p1, p1.ap[64:96, 0:n])
            for h in range(8):
                kb.dma("pool", scr["kTm"].ap[h, 64:96, k0:k0 + n], krs.ap[64:96, 0:n], [krs], [scr["kTm"]])
            nt = n // 128
            for tt in range(nt):
                b = bk.get()
                for c in range(2):
                    kb.mm(b, b.ap, ckvn, ckvn.ap[:, c, tt * 128:(tt + 1) * 128],
                          wukv, wukv.ap[:, c, :].rearrange("p (h x) -> p h x", x=128)[:, :, 64:128], start=(c == 0), stop=(c == 1))
                kb.cp("act", vms, vms.ap[:, :, tt, 0:64], b, b.ap.rearrange("p (h e) -> p h e", e=64))
            kb.dma("pool", scr["Vm"].ap[:, :, g * 4:g * 4 + nt, :].rearrange("h p t e -> p h t e"), vms.ap[:, :, 0:nt, :], [vms], [scr["Vm"]])
            dk_ = dks[i % 2]
            for half in range(2):
                pl = [proj_fm(wdk, (2 * half + c) * 128, hT, n) for c in range(2)]
                if not ctx:
                    pr = [proj_fm(wdk_r, (2 * half + c) * 128, hT, n) for c in range(2)]
                    for c in range(2):
                        rope_pair(dk_, dk_.ap[:, 2 * half + c, 0:n], pl[c], pr[c], a64, (0, 128), n)
                else:
                    for c in range(2):
                        kb.cp("act", dk_, dk_.ap[:, 2 * half + c, 0:n], pl[c], pl[c].ap[:, 0:n])
            kb.dma("pool", scr["kTd"].ap[:, :, k0:k0 + n].rearrange("h p n -> p h n"), dk_.ap[:, :, 0:n], [dk_], [scr["kTd"]])
            for tt in range(nt):
                b = bk.get()
                for kc in range(8):
                    kb.mm(b, b.ap, hT, hT.ap[:, kc, tt * 128:(tt + 1) * 128], wdv, wdv.ap[:, kc, :], start=(kc == 0), stop=(kc == 7))
                kb.cp("act", vds, vds.ap[:, :, tt, :], b, b.ap.rearrange("p (h e) -> p h e", e=128))
            kb.dma("pool", scr["Vd"].ap[:, :, g * 4:g * 4 + nt, :].rearrange("h p t e -> p h t e"), vds.ap[:, :, 0:nt, :], [vds], [scr["Vd"]])
            if own:
                pq = [proj_fm(wq_c, c * 128, hT, n) for c in range(3)]
                rms([(p, p.ap[:, 0:n]) for p in pq], 3, qg, cqn, n)
                for h in range(8):
                    q_ = qs[h % 2]
                    b1 = bk.get()
                    b2 = bk.get()
                    for c in range(3):
                        kb.mm(b1, b1.ap[0:96, 0:n], wuq, wuq.ap[:, c, h * 96:(h + 1) * 96], cqn, cqn.ap[:, c, 0:n], start=(c == 0), stop=(c == 2))
                    for c in range(3):
                        kb.mm(b2, b2.ap[0:96, 0:n], wuq_r, wuq_r.ap[:, c, h * 96:(h + 1) * 96], cqn, cqn.ap[:, c, 0:n], start=(c == 0), stop=(c == 2))
                    kb.cp("act", q_, q_.ap[0:64, 0:n], b1, b1.ap[0:64, 0:n])
                    rope_pair(q_, q_.ap[64:96, 0:n], b1, b2, a32, (64, 96), n)
                    kb.dma("pool", scr["qTm"].ap[h, :, k0:k0 + n], q_.ap[:, 0:n], [q_], [scr["qTm"]])
                dq_ = dks[(i + 1) % 2]
                for half in range(2):
                    pl = [proj_fm(wdq, (2 * half + c) * 128, hT, n) for c in range(2)]
                    pr = [proj_fm(wdq_r, (2 * half + c) * 128, hT, n) for c in range(2)]
                    for c in range(2):
                        rope_pair(dq_, dq_.ap[:, 2 * half + c, 0:n], pl[c], pr[c], a64, (0, 128), n)
                kb.dma("pool", scr["qTd"].ap[:, :, k0:k0 + n].rearrange("h p n -> p h n"), dq_.ap[:, :, 0:n], [dq_], [scr["qTd"]])
        kb.P.barrier()


def phase_l1_attn(kb, d, scr):
    with ExitStack() as st:
        kb.stack = st
        lp = kb.sb([128, 256], F32)
        kb.dma("sp", lp.ap, d["d_lambda"].rearrange("a b -> (a b)").partition_broadcast(128), [], [lp])
        pr_ = kb.sb([128, 128], F32)
        lpv = lp.ap.rearrange("p (a b) -> p a b", b=64)
        kb.tt("dve", pr_, pr_.ap.rearrange("p (a b) -> p a b", b=64), lp, lpv[:, 0:4:2, :], lp, lpv[:, 1:4:2, :], ALU.mult)
        ssum = kb.sb([128, 4], F32)
        kb.op("dve", lambda e: e.reduce_sum(out=ssum.ap[:, 0:2], in_=pr_.ap.rearrange("p (a b) -> p a b", b=64), axis=AX.X), [pr_], [ssum])
        kb.act(ssum, ssum.ap[:, 0:2], ssum, ssum.ap[:, 0:2], AF.Exp)
        kb.tt("dve", ssum, ssum.ap[:, 2:3], ssum, ssum.ap[:, 1:2], ssum, ssum.ap[:, 0:1], ALU.subtract)
        kb.ts("dve", ssum, ssum.ap[:, 2:3], ssum, ssum.ap[:, 2:3], -LAM_INIT, None, ALU.add)
        subl = kb.sb([128, 1], F32)
        kb.dma("sp", subl.ap, d["d_sublnT"], [], [subl])
        kb.ts("dve", subl, subl.ap, subl, subl.ap, 1.0 - LAM_INIT, None, ALU.mult)
        eps6 = kb.sb([128, 1], F32)
        kb.memset("dve", eps6, eps6.ap, 1e-6)

        E = [kb.sb([128, 2, 512], BF16) for _ in range(3)]
        kT = kb.sb([128, NK], BF16)
        V = kb.sb([128, NKT, 128], BF16)
        qT = kb.sb([128, 4096], BF16)
        zr = kb.sb([128, 512], F32)
        bcs = kb.sb([128, 512], F32)
        ta = kb.sb([128, 512], F32)
        tb = kb.sb([128, 512], F32)
        osb = [kb.sb([128, 512], BF16) for _ in range(2)]
        ei = 0
        oi = 0
        sp_i = 0
        ps = kb.ps

        def load(dst, dst_ap_fn, src_T, src_ap_fn, total, nsplit, reads):
            step = total // nsplit
            for s_ in range(nsplit):
                kb.dma("sp", dst_ap_fn(s_ * step, (s_ + 1) * step), src_ap_fn(s_ * step, (s_ + 1) * step), reads, [dst])

        sc_m = 96 ** -0.5
        Vm_v = V.ap.rearrange("p t e -> p (t e)")[:, 0:NKT * 66].rearrange("p (t e) -> p t e", e=66)
        for h in range(8):
            load(kT, lambda a, b: kT.ap[0:96, a:b], scr["kTm"], lambda a, b: scr["kTm"].ap[h, :, a:b], NK, 4, [scr["kTm"]])
            load(V, lambda a, b: Vm_v[:, a:b, :], scr["Vm"], lambda a, b: scr["Vm"].ap[h, :, a:b, :], NKT, 2, [scr["Vm"]])
            kb.dma("sp", qT.ap[0:96, :], scr["qTm"].ap[h], [scr["qTm"]], [qT])
            for qb in range(8):
                ob = ps[4 + oi % 2]
                oi += 1
                qap = qT.ap[0:96, qb * 512:(qb + 1) * 512]

                def s_step(kp):
                    pb_ = 2 * (kp % 2)
                    X, Y = ps[pb_], ps[pb_ + 1]
                    kb.mm(X, X.ap, kT, kT.ap[0:96, (2 * kp) * 128:(2 * kp + 1) * 128], qT, qap)
                    kb.mm(Y, Y.ap, kT, kT.ap[0:96, (2 * kp + 1) * 128:(2 * kp + 2) * 128], qT, qap)
                    return pb_, X, Y
                nxt = s_step(0)
                for kp in range(NKT // 2):
                    pb_, X, Y = nxt
                    if kp + 1 < NKT // 2:
                        nxt = s_step(kp + 1)
                    E_ = E[ei % 3]
                    ei += 1
                    kb.op("act", lambda e: e.activation(out=E_.ap.rearrange("p a b -> p (a b)"), in_=kb.psall[:, pb_ * 512:(pb_ + 2) * 512],
                                                        func=AF.Exp, scale=sc_m), [X, Y], [E_])
                    for a_ in range(2):
                        first = (kp == 0 and a_ == 0)
                        last = (kp == NKT // 2 - 1 and a_ == 1)
                        kb.mm(ob, ob.ap[0:65, :], V, Vm_v[:, 2 * kp + a_, 0:65], E_, E_.ap[:, a_, :], start=first, stop=last, acc=not first)
                kb.op("dve", lambda e: e.reciprocal(out=zr.ap[64:65, :], in_=ob.ap[64:65, :]), [ob], [zr])
                bc = ps[6]
                kb.mm(bc, bc.ap[0:64, :], kb.ones_f, kb.ones_f.ap[64:65, 0:64], zr, zr.ap[64:65, :])
                kb.cp("act", bcs, bcs.ap[0:64, :], bc, bc.ap[0:64, :])
                o_ = osb[oi % 2]
                kb.tt("dve", o_, o_.ap[0:64, :], ob, ob.ap[0:64, :], bcs, bcs.ap[0:64, :], ALU.mult)
                kb.dma("pool", scr["oTs"].ap[h, 0:64, qb * 512:(qb + 1) * 512], o_.ap[0:64, :], [o_], [scr["oTs"]])
        for h in range(4):
            load(kT, lambda a, b: kT.ap[:, a:b], scr["kTd"], lambda a, b: scr["kTd"].ap[h, :, a:b], NK, 4, [scr["kTd"]])
            load(V, lambda a, b: V.ap[:, a:b, :], scr["Vd"], lambda a, b: scr["Vd"].ap[h, :, a:b, :], NKT, 2, [scr["Vd"]])
            kb.dma("sp", qT.ap, scr["qTd"].ap[h], [scr["qTd"]], [qT])
            for qb in range(8):
                o0, o1, Z0, Z1 = ps[4], ps[5], ps[6], ps[7]
                qsl = slice(qb * 512, (qb + 1) * 512)

                def s_step(kt):
                    pb_ = 2 * (kt % 2)
                    X, Y = ps[pb_], ps[pb_ + 1]
                    ksl = slice(kt * 128, (kt + 1) * 128)
                    kb.mm(X, X.ap, kT, kT.ap[0:64, ksl], qT, qT.ap[0:64, qsl])
                    kb.mm(Y, Y.ap, kT, kT.ap[64:128, ksl], qT, qT.ap[64:128, qsl])
                    return pb_, X, Y
                nxt = s_step(0)
                for kt in range(NKT):
                    pb_, X, Y = nxt
                    if kt + 1 < NKT:
                        nxt = s_step(kt + 1)
                    E_ = E[ei % 3]
                    ei += 1
                    kb.op("act", lambda e: e.activation(out=E_.ap.rearrange("p a b -> p (a b)"), in_=kb.psall[:, pb_ * 512:(pb_ + 2) * 512],
                                                        func=AF.Exp, scale=0.125), [X, Y], [E_])
                    first = (kt == 0)
                    last = (kt == NKT - 1)
                    for a_, (o_b, z_b) in enumerate(((o0, Z0), (o1, Z1))):
                        kb.mm(o_b, o_b.ap, V, V.ap[:, kt, :], E_, E_.ap[:, a_, :], start=first, stop=last, acc=not first)
                        kb.mm(z_b, z_b.ap, kb.ones_b, kb.ones_b.ap, E_, E_.ap[:, a_, :], start=first, stop=last, acc=not first)
                kb.op("dve", lambda e: e.reciprocal(out=zr.ap, in_=Z0.ap), [Z0], [zr])
                kb.tt("dve", ta, ta.ap, o0, o0.ap, zr, zr.ap, ALU.mult)
                kb.op("dve", lambda e: e.reciprocal(out=zr.ap, in_=Z1.ap), [Z1], [zr])
                kb.tt("dve", tb, tb.ap, o1, o1.ap, zr, zr.ap, ALU.mult)
                kb.stt("dve", ta, ta.ap, tb, tb.ap, ssum.ap[:, 2:3], ta, ta.ap, ALU.mult, ALU.add, extra=[ssum])
                kb.tt("pool", tb, tb.ap, ta, ta.ap, ta, ta.ap, ALU.mult)
                sb_ = ps[0]
                kb.mm(sb_, sb_.ap, kb.ones_f, kb.ones_f.ap, tb, tb.ap)
                kb.act(bcs, bcs.ap, sb_, sb_.ap, AF.Ln, scale=1.0 / 128, bias=eps6.ap[:, 0:1], extra=[eps6])
                kb.act(bcs, bcs.ap, bcs, bcs.ap, AF.Exp, scale=-0.5)
                o_ = osb[oi % 2]
                oi += 1
                kb.stt("dve", o_, o_.ap, ta, ta.ap, subl.ap[:, 0:1], bcs, bcs.ap, ALU.mult, ALU.mult, extra=[subl])
                kb.dma("pool", scr["oTs"].ap[8 + h, :, qb * 512:(qb + 1) * 512], o_.ap, [o_], [scr["oTs"]])
        kb.P.barrier()


def phase_l1_out(kb, d, modT, lng, lnb, scr):
    with ExitStack() as st:
        kb.stack = st
        wC = kb.sb([64, 8, 1024], BF16)
        wD = kb.sb([128, 4, 1024], BF16)
        kb.dma("pool", wC.ap, d["cd_w_out"][0:512, :].rearrange("(h p) c -> p h c", p=64), [], [wC])
        kb.dma("pool", wD.ap, d["cd_w_out"][512:1024, :].rearrange("(h p) c -> p h c", p=128), [], [wD])
        oTb = [kb.sb([128, 12, 128], BF16) for _ in range(2)]
        lc = LNCtx(kb)
        xv = d["x1T"].rearrange("(kc p) t -> p kc t", p=128)
        ov = scr["x1b"].ap.rearrange("(kc p) t -> p kc t", p=128)
        ps = kb.ps
        for tile in range(32):
            oT = oTb[tile % 2]
            cs_ = slice(tile * 128, (tile + 1) * 128)
            kb.dma("sp", oT.ap[0:64, 0:8, :], scr["oTs"].ap[0:8, 0:64, cs_].rearrange("h p n -> p h n"), [scr["oTs"]], [oT])
            kb.dma("sp", oT.ap[:, 8:12, :], scr["oTs"].ap[8:12, :, cs_].rearrange("h p n -> p h n"), [scr["oTs"]], [oT])
            y0, y1 = ps[(2 * tile) % 4], ps[(2 * tile) % 4 + 1]
            for dc in range(8):
                pb_ = y0 if dc < 4 else y1
                c0 = (dc % 4) * 128
                for h in range(12):
                    if h < 8:
                        l_, lap, rap = wC, wC.ap[:, h, dc * 128:(dc + 1) * 128], oT.ap[0:64, h, :]
                    else:
                        l_, lap, rap = wD, wD.ap[:, h - 8, dc * 128:(dc + 1) * 128], oT.ap[:, h, :]
                    kb.mm(pb_, pb_.ap[:, c0:c0 + 128], l_, lap, oT, rap, start=(h == 0), stop=(h == 11), acc=not (dc % 4 == 0 and h == 0))

            def y_fn(dc, y0=y0, y1=y1):
                pb_ = y0 if dc < 4 else y1
                return pb_, pb_.ap[:, (dc % 4) * 128:(dc % 4 + 1) * 128]
            ln_epilogue(kb, lc, y_fn, xv[:, :, cs_], d.get("x_reads", []), modT.ap[:, 16:24, 0:1], modT, lng.ap[:, 0, :], lnb.ap[:, 0, :], (lng, lnb),
                        ov[:, :, cs_], scr["x1b"], ps[7])
        kb.P.barrier()


def build_launch2(debug=False, stop=None):
    kb = KB()
    d = _declare_common(kb)
    d.update(_declare_moe(kb))
    d.update({
        "x1T": kb.din("x1T", [1024, 16384]), "ctx1T": kb.din("ctx1T", [1024, 256]),
        "cd_w_in": kb.din("cd_w_in", [1024, 2208]), "cd_w_out": kb.din("cd_w_out", [1024, 1024]),
        "c_w_uq": kb.din("c_w_uq", [384, 768]), "c_w_ukv": kb.din("c_w_ukv", [256, 1024]),
        "c_q_normT": kb.din("c_q_normT", [128, 3]), "c_kv_normT": kb.din("c_kv_normT", [128, 2]),
        "d_lambda": kb.din("d_lambda", [4, 64]), "d_sublnT": kb.din("d_sublnT", [128, 1]),
        "rope64": kb.din("rope64", [128, 2, 16384]), "rope32": kb.din("rope32", [128, 2, 16384]),
    })
    out = T(kb.dout("outT", [1024, 4096]))
    scr = l1_scratch(kb)
    if debug:
        scr["x1b"] = T(kb.dout("x1b_dbg", [1024, 4096]))
    modT = phase_mod(kb, d["w_ada"], d["b_adaT"], d["cT"])
    lng, lnb = load_ln(kb, d["ln_gT"], d["ln_bT"])
    phase_l1_proj(kb, d, modT, scr)
    phase_l1_attn(kb, d, scr)
    phase_l1_out(kb, d, modT, lng, lnb, scr)
    if stop == "attn":
        kb.P.barrier()
        return kb
    ov = out.ap.rearrange("(kc p) t -> p kc t", p=128)
    passes = [list(range(8 * i, 8 * i + 8)) for i in range(4)]
    phase_moe(kb, d, modT, lng, lnb, scr["x1b"], passes, 99, lambda tile: (ov[:, :, tile * 128:(tile + 1) * 128], out))
    kb.P.barrier()
    return kb


def _own_first(r):
    idx = np.arange(S)
    own = idx[r * 4096:(r + 1) * 4096]
    rest = np.concatenate([idx[:r * 4096], idx[(r + 1) * 4096:]])
    return np.concatenate([own, rest])


def launch2_inputs(inp, x1, ctx1):
    maps = []
    moe = _moe_inputs(inp, 1)
    for core in range(8):
        b, r = core // 4, core % 4
        perm = _own_first(r)
        cos64, sin64 = _rope_np(perm, 64)
        cos32, sin32 = _rope_np(perm, 32)
        rope64 = np.stack([np.concatenate([cos64.T, cos64.T], 0), np.concatenate([sin64.T, sin64.T], 0)], axis=1)
        z = np.zeros((64, S), np.float32)
        z2 = np.zeros((32, S), np.float32)
        rope32 = np.stack([np.concatenate([z, cos32.T, z2], 0), np.concatenate([z, sin32.T, z2], 0)], axis=1)
        m = _common_inputs(inp, 1, b)
        m.update(moe)
        m.update({
            "x1T": np.ascontiguousarray(x1[b][perm].T), "ctx1T": np.ascontiguousarray(ctx1[b].T),
            "cd_w_in": np.ascontiguousarray(inp["cd_w_in"][0]), "cd_w_out": np.ascontiguousarray(inp["cd_w_out"][0]),
            "c_w_uq": np.ascontiguousarray(inp["c_w_uq"][0]), "c_w_ukv": np.ascontiguousarray(inp["c_w_ukv"][0]),
            "c_q_normT": _fm(inp["c_q_norm"][0], 3), "c_kv_normT": _fm(inp["c_kv_norm"][0], 2),
            "d_lambda": np.ascontiguousarray(inp["d_lambda"][0]), "d_sublnT": _fm(inp["d_subln"][0], 1),
            "rope64": np.ascontiguousarray(rope64), "rope32": np.ascontiguousarray(rope32),
        })
        maps.append(m)
    return maps


_CACHE = {}


def _kernel2(**inp):
    inp = {k: np.asarray(v) for k, v in inp.items()}
    if "k1" not in _CACHE:
        _CACHE["k1"] = build_launch1()
        _CACHE["k2"] = build_launch2()
    k1, k2 = _CACHE["k1"], _CACHE["k2"]
    res1 = run_bass_kernel_spmd(k1.nc, launch1_inputs(inp), core_ids=list(range(8)))
    x1 = np.empty((2, S, D), np.float32)
    ctx1 = np.empty((2, L, D), np.float32)
    for core in range(8):
        b, r = core // 4, core % 4
        o = res1.results[core]["x2T"]
        x1[b, r * 4096:(r + 1) * 4096] = o[:, 0:4096].T
        if r == 0:
            ctx1[b] = o[:, 4096:4352].T
    res2 = run_bass_kernel_spmd(k2.nc, launch2_inputs(inp, x1, ctx1), core_ids=list(range(8)))
    out = np.empty((2, S, D), np.float32)
    for core in range(8):
        b, r = core // 4, core % 4
        out[b, r * 4096:(r + 1) * 4096] = res2.results[core]["outT"].T
    return out


def build_fused():
    kb = KB()
    c0 = _declare_common(kb, "0")
    c1 = _declare_common(kb, "1")
    m0 = _declare_moe(kb, "0")
    m1 = _declare_moe(kb, "1")
    xT = kb.din("xT", [4, 1024, NEXT])
    biasB = kb.din("biasB", [4, 128, 5, 5, 8, 128])
    maskA = kb.din("maskA", [4, 128, 4, 128])
    ropeT = kb.din("ropeT", [4, 128, 2, NEXT])
    l0 = {"ctxT": kb.din("ctxT", [1024, 256]), "ab_w_in": kb.din("ab_w_in", [1024, 2304]),
          "ab_w_out": kb.din("ab_w_out", [1024, 1024]), "a_sink": kb.din("a_sink", [1, 8])}
    l1 = {
        "cd_w_in": kb.din("cd_w_in", [1024, 2208]), "cd_w_out": kb.din("cd_w_out", [1024, 1024]),
        "c_w_uq": kb.din("c_w_uq", [384, 768]), "c_w_ukv": kb.din("c_w_ukv", [256, 1024]),
        "c_q_normT": kb.din("c_q_normT", [128, 3]), "c_kv_normT": kb.din("c_kv_normT", [128, 2]),
        "d_lambda": kb.din("d_lambda", [4, 64]), "d_sublnT": kb.din("d_sublnT", [128, 1]),
        "rope64": kb.din("rope64", [128, 2, 16384]), "rope32": kb.din("rope32", [128, 2, 16384]),
    }
    out = T(kb.dout("outT", [1024, 4096]))
    x1T = kb.dscr("x1T_s", [1024, 4352], F32)
    x2T = kb.dscr("x2T_s", [1024, S + L], F32)
    x2v = x2T.ap.rearrange("(kc p) t -> p kc t", p=128)
    mod0 = phase_mod(kb, c0["w_ada"], c0["b_adaT"], c0["cT"])
    lng0, lnb0 = load_ln(kb, c0["ln_gT"], c0["ln_bT"])
    for seg in range(4):
        dseg = dict(l0)
        dseg.update({"xT": xT[seg], "biasB": biasB[seg], "maskA": maskA[seg], "ropeT": ropeT[seg]})
        phase_l0_attn(kb, dseg, mod0, lng0, lnb0, x1T, with_ctx=(seg == 0))
        ntile = 34 if seg == 0 else 32
        passes = [list(range(0, 12)), list(range(12, 24)), list(range(24, 34))] if seg == 0 else \
            [list(range(0, 12)), list(range(12, 24)), list(range(24, 32))]

        def out_fn(tile, seg=seg):
            if tile < 32:
                c = seg * 4096 + tile * 128
            else:
                c = S + (tile - 32) * 128
            return x2v[:, :, c:c + 128], x2T
        phase_moe(kb, m0, mod0, lng0, lnb0, x1T, passes, 32, out_fn)
    mod1 = phase_mod(kb, c1["w_ada"], c1["b_adaT"], c1["cT"])
    lng1, lnb1 = load_ln(kb, c1["ln_gT"], c1["ln_bT"])
    scr = l1_scratch(kb)
    l1["x1T"] = x2T.ap[:, 0:S]
    l1["ctx1T"] = x2T.ap[:, S:S + L]
    l1["x_reads"] = [x2T]
    phase_l1_proj(kb, l1, mod1, scr)
    phase_l1_attn(kb, l1, scr)
    phase_l1_out(kb, l1, mod1, lng1, lnb1, scr)
    ov = out.ap.rearrange("(kc p) t -> p kc t", p=128)
    passes = [list(range(0, 12)), list(range(12, 24)), list(range(24, 32))]
    phase_moe(kb, m1, mod1, lng1, lnb1, scr["x1b"], passes, 99, lambda tile: (ov[:, :, tile * 128:(tile + 1) * 128], out))
    kb.P.barrier()
    return kb


def fused_inputs(inp):
    maps = []
    rpb = np.asarray(inp["b_rpb"][0], np.float32)
    tabs = {r: _bias_tables(rpb, r) for r in range(4)}
    masks = {r: _mask_a(r) for r in range(4)}
    moe0 = {k + "0": v for k, v in _moe_inputs(inp, 0).items()}
    moe1 = {k + "1": v for k, v in _moe_inputs(inp, 1).items()}
    ropes0 = {}
    xts = {}
    for r in range(4):
        et = _ext_tokens(r)
        cos, sin = _rope_np(et, 64)
        ropes0[r] = np.stack([np.concatenate([cos.T, cos.T], 0), np.concatenate([sin.T, sin.T], 0)], axis=1)
        for b in range(2):
            xts[(b, r)] = np.ascontiguousarray(inp["x"][b][et].T)
    for core in range(8):
        b, r = core // 4, core % 4
        order = [r] + [q for q in range(4) if q != r]
        perm = _own_first(r)
        cos64, sin64 = _rope_np(perm, 64)
        cos32, sin32 = _rope_np(perm, 32)
        rope64 = np.stack([np.concatenate([cos64.T, cos64.T], 0), np.concatenate([sin64.T, sin64.T], 0)], axis=1)
        z = np.zeros((64, S), np.float32)
        z2 = np.zeros((32, S), np.float32)
        rope32 = np.stack([np.concatenate([z, cos32.T, z2], 0), np.concatenate([z, sin32.T, z2], 0)], axis=1)
        m = {k + "0": v for k, v in _common_inputs(inp, 0, b).items()}
        m.update({k + "1": v for k, v in _common_inputs(inp, 1, b).items()})
        m.update(moe0)
        m.update(moe1)
        m.update({
            "xT": np.stack([xts[(b, q)] for q in order], 0),
            "biasB": np.stack([tabs[q] for q in order], 0),
            "maskA": np.stack([masks[q] for q in order], 0),
            "ropeT": np.stack([ropes0[q] for q in order], 0),
            "ctxT": np.ascontiguousarray(inp["ctx"][b].T),
            "ab_w_in": np.ascontiguousarray(inp["ab_w_in"][0]), "ab_w_out": np.ascontiguousarray(inp["ab_w_out"][0]),
            "a_sink": np.ascontiguousarray(inp["a_sink"][0].reshape(1, 8)),
            "cd_w_in": np.ascontiguousarray(inp["cd_w_in"][0]), "cd_w_out": np.ascontiguousarray(inp["cd_w_out"][0]),
            "c_w_uq": np.ascontiguousarray(inp["c_w_uq"][0]), "c_w_ukv": np.ascontiguousarray(inp["c_w_ukv"][0]),
            "c_q_normT": _fm(inp["c_q_norm"][0], 3), "c_kv_normT": _fm(inp["c_kv_norm"][0], 2),
            "d_lambda": np.ascontiguousarray(inp["d_lambda"][0]), "d_sublnT": _fm(inp["d_subln"][0], 1),
            "rope64": np.ascontiguousarray(rope64), "rope32": np.ascontiguousarray(rope32),
        })
        maps.append(m)
    return maps


def kernel_unfused(**inp):
    return _kernel2(**inp)


def kernel(**inp):
    inp = {k: np.asarray(v) for k, v in inp.items()}
    if "kf" not in _CACHE:
        _CACHE["kf"] = build_fused()
    kf = _CACHE["kf"]
    res = run_bass_kernel_spmd(kf.nc, fused_inputs(inp), core_ids=list(range(8)))
    out = np.empty((2, S, D), np.float32)
    for core in range(8):
        b, r = core // 4, core % 4
        out[b, r * 4096:(r + 1) * 4096] = res.results[core]["outT"].T
    return out
```

```python
import math
from contextlib import ExitStack
import numpy as np
import concourse.bass as bass
import concourse.mybir as mybir
from concourse.bass_utils import run_bass_kernel_spmd

F32 = mybir.dt.float32
BF16 = mybir.dt.bfloat16
ALU = mybir.AluOpType
AF = mybir.ActivationFunctionType
AX = mybir.AxisListType

D = 1024
S = 16384
L = 256
DEPTH = 2
ALPHA = (2 * DEPTH) ** 0.25
LN_EPS = 1e-5 / (ALPHA * ALPHA)
NEG = -30000.0
LAM_INIT = 0.8 - 0.6 * math.exp(-0.3 * 1)
NEXT = 4608


class H:
    __slots__ = ("w", "r")

    def __init__(self):
        self.w = None
        self.r = []


class T:
    def __init__(self, ap):
        self.ap = ap
        self.h = H()


class Prog:
    NDMA = 8

    def __init__(self, nc):
        self.nc = nc
        self.eng = {"pe": nc.tensor, "act": nc.scalar, "dve": nc.vector,
                    "pool": nc.gpsimd, "sp": nc.sync}
        self.sem = {}
        self.cnt = {}
        for k in ("pe", "act", "dve", "pool"):
            self.sem[k] = nc.alloc_semaphore("s_" + k)
            self.cnt[k] = 0
        self.seen = {k: {} for k in self.eng}
        self.dma_i = {}
        self.dma_sems = {}
        for q in ("sp", "pool", "act"):
            self.dma_i[q] = 0
            self.dma_sems[q] = []
            for i in range(self.NDMA):
                key = ("dma", q, i)
                self.sem[key] = nc.alloc_semaphore("d_%s_%d" % (q, i))
                self.cnt[key] = 0
                self.dma_sems[q].append(key)
        self.n_ins = 0

    def _wait(self, e, key, val):
        if self.seen[e].get(key, 0) < val:
            self.eng[e].wait_ge(self.sem[key], val)
            self.seen[e][key] = val

    def _deps(self, e, reads, writes, skip_own_waw=False):
        deps = {}
        for h in reads:
            if h.w is not None:
                k, v = h.w
                if deps.get(k, 0) < v:
                    deps[k] = v
        for h in writes:
            if h.w is not None:
                k, v = h.w
                if not (skip_own_waw and k == e):
                    if deps.get(k, 0) < v:
                        deps[k] = v
            for (k, v) in h.r:
                if deps.get(k, 0) < v:
                    deps[k] = v
        for k, v in deps.items():
            self._wait(e, k, v)

    def _mark(self, tok, reads, writes):
        for h in reads:
            if len(h.r) > 16:
                d = {}
                for (k, v) in h.r:
                    if d.get(k, 0) < v:
                        d[k] = v
                h.r = list(d.items())
            h.r.append(tok)
        for h in writes:
            h.w = tok
            h.r = []

    def op(self, e, fn, reads=(), writes=(), inc=True, acc=False):
        self._deps(e, reads, writes, skip_own_waw=acc)
        ins = fn(self.eng[e])
        self.n_ins += 1
        if inc:
            self.cnt[e] += 1
            ins.then_inc(self.sem[e], 1)
            tok = (e, self.cnt[e])
        else:
            tok = (e, self.cnt[e] + 1)
        self._mark(tok, reads, writes)
        return tok

    def dma(self, q, out, in_, reads=(), writes=()):
        self._deps(q, reads, writes)
        i = self.dma_i[q]
        self.dma_i[q] += 1
        key = self.dma_sems[q][i % self.NDMA]
        if self.cnt[key] > 0:
            self._wait(q, key, self.cnt[key])
        self.cnt[key] += 16
        self.eng[q].dma_start(out=out, in_=in_).then_inc(self.sem[key], 16)
        self.n_ins += 1
        tok = (key, self.cnt[key])
        self._mark(tok, reads, writes)
        return tok

    def barrier(self):
        for e in self.eng:
            for k, v in self.cnt.items():
                if v > 0:
                    self._wait(e, k, v)


class KB:
    def __init__(self):
        self.nc = bass.Bass("TRN2", target_bir_lowering=False)
        self.P = Prog(self.nc)
        nc = self.nc
        psall = nc.alloc_psum_tensor("psall", [128, 4096], F32).ap()
        self.psall = psall
        self.ps = [T(psall[:, i * 512:(i + 1) * 512]) for i in range(8)]
        self.stack = None
        self.nm = 0
        self.ones_f = self.gsb([128, 128], F32)
        self.ones_b = self.gsb([128, 128], BF16)
        self.op("dve", lambda e: e.memset(self.ones_f.ap, 1.0), [], [self.ones_f])
        self.op("dve", lambda e: e.memset(self.ones_b.ap, 1.0), [], [self.ones_b])

    def name(self, p="t"):
        self.nm += 1
        return "%s%d" % (p, self.nm)

    def gsb(self, shape, dt):
        return T(self.nc.alloc_sbuf_tensor(self.name("g"), list(shape), dt).ap())

    def sb(self, shape, dt):
        t = self.stack.enter_context(self.nc.sbuf_tensor(self.name("s"), list(shape), dt))
        return T(t.ap())

    def din(self, name, shape, dt=F32):
        return self.nc.dram_tensor(name, list(shape), dt, kind="ExternalInput").ap()

    def dout(self, name, shape, dt=F32):
        return self.nc.dram_tensor(name, list(shape), dt, kind="ExternalOutput").ap()

    def dscr(self, name, shape, dt):
        return T(self.nc.dram_tensor(name, list(shape), dt, kind="Internal").ap())

    def op(self, e, fn, reads, writes, inc=True, acc=False):
        return self.P.op(e, fn, [t.h for t in reads], [t.h for t in writes], inc=inc, acc=acc)

    def dma(self, q, out, in_, reads, writes):
        return self.P.dma(q, out, in_, [t.h for t in reads], [t.h for t in writes])

    def mm(self, o, o_ap, l, l_ap, r, r_ap, start=True, stop=True, acc=None, skip=False, inc=None):
        if acc is None:
            acc = not start
        if inc is None:
            inc = stop
        self.op("pe", lambda e: e.matmul(o_ap, lhsT=l_ap, rhs=r_ap, start=start, stop=stop, skip_group_check=skip),
                [l, r], [o], inc=inc, acc=acc)

    def tt(self, eng, o, o_ap, a, a_ap, b, b_ap, op):
        self.op(eng, lambda e: e.tensor_tensor(out=o_ap, in0=a_ap, in1=b_ap, op=op), [a, b], [o])

    def stt(self, eng, o, o_ap, a, a_ap, sc, b, b_ap, op0, op1, extra=()):
        self.op(eng, lambda e: e.scalar_tensor_tensor(out=o_ap, in0=a_ap, scalar=sc, in1=b_ap, op0=op0, op1=op1),
                [a, b] + list(extra), [o])

    def ts(self, eng, o, o_ap, a, a_ap, s1, s2, op0, op1=None, extra=()):
        if op1 is None:
            self.op(eng, lambda e: e.tensor_scalar(out=o_ap, in0=a_ap, scalar1=s1, scalar2=None, op0=op0),
                    [a] + list(extra), [o])
        else:
            self.op(eng, lambda e: e.tensor_scalar(out=o_ap, in0=a_ap, scalar1=s1, scalar2=s2, op0=op0, op1=op1),
                    [a] + list(extra), [o])

    def act(self, o, o_ap, a, a_ap, func, scale=1.0, bias=None, extra=()):
        if bias is None:
            self.op("act", lambda e: e.activation(out=o_ap, in_=a_ap, func=func, scale=scale),
                    [a] + list(extra), [o])
        else:
            self.op("act", lambda e: e.activation(out=o_ap, in_=a_ap, func=func, scale=scale, bias=bias),
                    [a] + list(extra), [o])

    def cp(self, eng, o, o_ap, a, a_ap):
        if eng == "act":
            self.op("act", lambda e: e.copy(out=o_ap, in_=a_ap), [a], [o])
        else:
            self.op(eng, lambda e: e.tensor_copy(out=o_ap, in_=a_ap), [a], [o])

    def memset(self, eng, o, o_ap, v):
        self.op(eng, lambda e: e.memset(o_ap, v), [], [o])

    def rstd(self, o, o_ap, a, a_ap, scale, eps, epsT):
        self.act(o, o_ap, a, a_ap, AF.Ln, scale=scale, bias=epsT.ap[0:o_ap.shape[0] + o_ap.base_partition(), 0:1][o_ap.base_partition():, :], extra=[epsT])
        self.act(o, o_ap, o, o_ap, AF.Exp, scale=-0.5)


def phase_mod(kb, w_ada, b_adaT, cT):
    modT = kb.gsb([128, 48, 2], F32)
    with ExitStack() as st:
        kb.stack = st
        cs = kb.sb([128, 8, 2], F32)
        kb.dma("sp", cs.ap, cT, [], [cs])
        kb.act(cs, cs.ap, cs, cs.ap, AF.Silu)
        bT = kb.sb([128, 48], F32)
        kb.dma("sp", bT.ap, b_adaT, [], [bT])
        wv = w_ada.rearrange("(kc p) f -> p kc f", p=128)
        bufs = [kb.sb([128, 8, 512], F32) for _ in range(2)]
        ps = kb.ps[0]
        for pc in range(12):
            wb = bufs[pc % 2]
            kb.dma("sp", wb.ap, wv[:, :, pc * 512:(pc + 1) * 512], [], [wb])
            for fc in range(4):
                ch = pc * 4 + fc
                for kc in range(8):
                    kb.mm(ps, ps.ap[:, 2 * ch:2 * ch + 2], wb, wb.ap[:, kc, fc * 128:(fc + 1) * 128],
                          cs, cs.ap[:, kc, :], start=(kc == 0), stop=(kc == 7), acc=(not (pc == 0 and fc == 0 and kc == 0)))
        pv = ps.ap[:, 0:96].rearrange("p (c j) -> p c j", j=2)
        kb.tt("dve", modT, modT.ap, ps, pv, bT, bT.ap.unsqueeze(2).to_broadcast([128, 48, 2]), ALU.add)
        for w in (1, 4):
            kb.ts("dve", modT, modT.ap[:, w * 8:(w + 1) * 8, :], modT, modT.ap[:, w * 8:(w + 1) * 8, :], 1.0, None, ALU.add)
        for w in (2, 5):
            kb.ts("dve", modT, modT.ap[:, w * 8:(w + 1) * 8, :], modT, modT.ap[:, w * 8:(w + 1) * 8, :], 1.0 / ALPHA, None, ALU.mult)
        kb.P.barrier()
    return modT


def load_ln(kb, ln_gT, ln_bT):
    g = kb.gsb([128, 2, 8], F32)
    b = kb.gsb([128, 2, 8], F32)
    kb.dma("sp", g.ap, ln_gT, [], [g])
    kb.dma("sp", b.ap, ln_bT, [], [b])
    return g, b


class LNCtx:
    def __init__(self, kb):
        self.z = kb.sb([128, 8, 128], F32)
        self.zsq = kb.sb([128, 8, 128], F32)
        self.xo = [kb.sb([128, 8, 128], F32) for _ in range(2)]
        self.m = kb.sb([128, 128], F32)
        self.msq = kb.sb([128, 128], F32)
        self.var = kb.sb([128, 128], F32)
        self.eps = kb.sb([128, 1], F32)
        kb.memset("dve", self.eps, self.eps.ap, LN_EPS)
        self.i = 0


def ln_epilogue(kb, lc, y_fn, xres_ap, xres_reads, gmod_ap, modT, lng_ap, lnb_ap, lnp, out_ap, out_T, stat_ps):
    z, zsq = lc.z, lc.zsq
    kb.dma("sp", z.ap, xres_ap, xres_reads, [z])
    for dc in range(8):
        yt, yap = y_fn(dc)
        kb.stt("dve", z, z.ap[:, dc, :], yt, yap, gmod_ap[:, dc, :], z, z.ap[:, dc, :], ALU.mult, ALU.add, extra=[modT])
    kb.tt("pool", zsq, zsq.ap, z, z.ap, z, z.ap, ALU.mult)
    for dc in range(8):
        kb.mm(stat_ps, stat_ps.ap[:, 0:128], kb.ones_f, kb.ones_f.ap, z, z.ap[:, dc, :], start=(dc == 0), stop=(dc == 7))
    for dc in range(8):
        kb.mm(stat_ps, stat_ps.ap[:, 128:256], kb.ones_f, kb.ones_f.ap, zsq, zsq.ap[:, dc, :], start=(dc == 0), stop=(dc == 7), acc=True)
    m, msq, var = lc.m, lc.msq, lc.var
    kb.act(m, m.ap, stat_ps, stat_ps.ap[:, 0:128], AF.Copy, scale=1.0 / D)
    kb.tt("dve", msq, msq.ap, m, m.ap, m, m.ap, ALU.mult)
    kb.stt("dve", var, var.ap, stat_ps, stat_ps.ap[:, 128:256], 1.0 / D, msq, msq.ap, ALU.mult, ALU.subtract)
    kb.act(var, var.ap, var, var.ap, AF.Ln, scale=1.0, bias=lc.eps.ap[:, 0:1], extra=[lc.eps])
    kb.act(var, var.ap, var, var.ap, AF.Exp, scale=-0.5)
    xo = lc.xo[lc.i % 2]
    lc.i += 1
    kb.tt("dve", z, z.ap, z, z.ap, m, m.ap.unsqueeze(1).to_broadcast([128, 8, 128]), ALU.subtract)
    kb.tt("pool", z, z.ap, z, z.ap, var, var.ap.unsqueeze(1).to_broadcast([128, 8, 128]), ALU.mult)
    kb.tt("dve", z, z.ap, z, z.ap, lnp[0], lng_ap.unsqueeze(2).to_broadcast([128, 8, 128]), ALU.mult)
    kb.tt("pool", xo, xo.ap, z, z.ap, lnp[1], lnb_ap.unsqueeze(2).to_broadcast([128, 8, 128]), ALU.add)
    kb.dma("pool", out_ap, xo.ap, [xo], [out_T])


class AttCtx:
    def __init__(self, kb, s_banks, o_banks, bc_bank):
        self.sb_ = [kb.sb([128, 512], F32) for _ in range(2)]
        self.E = [kb.sb([128, 512], BF16) for _ in range(3)]
        self.zr = kb.sb([128, 512], F32)
        self.bcs = kb.sb([64, 512], F32)
        self.s_banks = s_banks
        self.o_banks = o_banks
        self.bc = bc_bank
        self.si = 0
        self.ei = 0
        self.oi = 0
        self.bi = 0


def attn_unit(kb, ac, tiles, scale, out_T, out_ap, ncols=512, sink=None):
    ob = ac.o_banks[ac.oi % len(ac.o_banks)]
    ac.oi += 1
    nt = len(tiles)
    for ti, tl in enumerate(tiles):
        sbk = ac.s_banks[ac.si % len(ac.s_banks)]
        ac.si += 1
        first = True
        for (kT, kap, qT, qap, c0, n) in tl["qk"]:
            kb.mm(sbk, sbk.ap[:, c0:c0 + n], kT, kap, qT, qap, start=True, stop=True, acc=(not first))
            first = False
        E = ac.E[ac.ei % 3]
        ac.ei += 1
        if tl["bias"] is not None:
            bT, bap = tl["bias"]
            sbuf = ac.sb_[ac.bi % 2]
            ac.bi += 1
            kb.stt("dve", sbuf, sbuf.ap[:, 0:ncols], sbk, sbk.ap[:, 0:ncols], scale, bT, bap, ALU.mult, ALU.add)
            kb.act(E, E.ap[:, 0:ncols], sbuf, sbuf.ap[:, 0:ncols], AF.Exp)
        else:
            kb.act(E, E.ap[:, 0:ncols], sbk, sbk.ap[:, 0:ncols], AF.Exp, scale=scale)
        last = (ti == nt - 1) and sink is None
        for pi, (vT, vap, c0, n) in enumerate(tl["pv"]):
            kb.mm(ob, ob.ap[0:65, c0:c0 + n], vT, vap, E, E.ap[:, c0:c0 + n], start=(ti == 0 and pi == 0), stop=last,
                  acc=not (ti == 0 and pi == 0), skip=True, inc=last)
    if sink is not None:
        e64, srow_T, srow_ap = sink
        kb.mm(ob, ob.ap[0:65, 0:ncols], e64, e64.ap, srow_T, srow_ap, start=False, stop=True, acc=True)
    zr = ac.zr
    kb.op("dve", lambda e: e.reciprocal(out=zr.ap[64:65, 0:ncols], in_=ob.ap[64:65, 0:ncols]), [ob], [zr])
    bc = ac.bc
    kb.mm(bc, bc.ap[0:64, 0:ncols], kb.ones_f, kb.ones_f.ap[64:65, 0:64], zr, zr.ap[64:65, 0:ncols])
    kb.cp("act", ac.bcs, ac.bcs.ap[:, 0:ncols], bc, bc.ap[0:64, 0:ncols])
    kb.tt("dve", out_T, out_ap, ob, ob.ap[0:64, 0:ncols], ac.bcs, ac.bcs.ap[:, 0:ncols], ALU.mult)


def phase_l0_attn(kb, d, modT, lng, lnb, x1T, with_ctx=True):
    P = kb.P
    with ExitStack() as st:
        kb.stack = st
        wA = kb.sb([128, 8, 640], BF16)
        wArot = kb.sb([128, 8, 640], BF16)
        wR = kb.sb([128, 8, 1664], BF16)
        wout = kb.sb([64, 16, 1024], BF16)
        win = d["ab_w_in"].rearrange("(kc p) c -> p kc c", p=128)
        for slot, hd in enumerate([0, 4, 1, 5, 2, 6, 3, 7]):
            kb.dma("pool", wA.ap[:, :, slot * 64:(slot + 1) * 64], win[:, :, hd * 64:(hd + 1) * 64], [], [wA])
        kb.dma("pool", wA.ap[:, :, 512:640], win[:, :, 512:640], [], [wA])
        kb.dma("pool", wR.ap[:, :, 0:1024], win[:, :, 768:1792], [], [wR])
        kb.dma("pool", wR.ap[:, :, 1024:1152], win[:, :, 640:768], [], [wR])
        kb.dma("pool", wR.ap[:, :, 1152:1664], win[:, :, 1792:2304], [], [wR])
        kb.dma("pool", wout.ap, d["ab_w_out"].rearrange("(h p) c -> p h c", p=64), [], [wout])
        src = wA.ap.rearrange("p k (g two s) -> p (k g) two s", two=2, s=16)
        dst = wArot.ap.rearrange("p k (g two s) -> p (k g) two s", two=2, s=16)
        kb.ts("dve", wArot, dst[:, :, 0, :], wA, src[:, :, 1, :], -1.0, None, ALU.mult)
        kb.cp("dve", wArot, dst[:, :, 1, :], wA, src[:, :, 0, :])
        biasG = kb.sb([128, 5, 8, 128], BF16)
        biasE = kb.sb([128, 5, 8, 128], BF16)
        kb.dma("pool", biasG.ap, d["biasB"][:, 2], [], [biasG])
        maskA = kb.sb([128, 4, 128], BF16)
        kb.dma("pool", maskA.ap, d["maskA"], [], [maskA])
        sinkrow = kb.sb([1, 8, 128], F32)
        sk = kb.sb([1, 8], F32)
        kb.dma("sp", sk.ap, d["a_sink"], [], [sk])
        kb.act(sk, sk.ap, sk, sk.ap, AF.Exp)
        kb.cp("dve", sinkrow, sinkrow.ap, sk, sk.ap.unsqueeze(2).to_broadcast([1, 8, 128]))
        e64 = kb.sb([1, 65], F32)
        kb.memset("dve", e64, e64.ap, 0.0)
        kb.memset("dve", e64, e64.ap[:, 64:65], 1.0)
        def mkslot():
            s = dict(q=kb.sb([128, 8, 128], BF16), k=kb.sb([128, 5, 128], BF16), v=kb.sb([128, 10, 66], BF16))
            kb.memset("pool", s["v"], s["v"].ap[:, :, 64:65], 1.0)
            return s
        ring = [mkslot() for _ in range(6)]
        cslots = [mkslot() for _ in range(2)]
        xb = [kb.sb([128, 8, 128], F32) for _ in range(2)]
        hTs = [kb.sb([128, 8, 128], BF16) for _ in range(2)]
        tmpf = kb.sb([128, 8, 128], F32)
        ropeT = [kb.sb([128, 2, 128], F32) for _ in range(2)]
        r1 = kb.sb([128, 4, 128], F32)
        r2 = kb.sb([128, 4, 128], F32)
        oT = kb.sb([64, 16, 128], BF16)
        lc = LNCtx(kb)
        ac = AttCtx(kb, [kb.ps[3], kb.ps[4]], [kb.ps[5], kb.ps[6]], kb.ps[7])
        ps0, ps1, ps2 = kb.ps[0], kb.ps[1], kb.ps[2]
        xTv = d["xT"].rearrange("(kc p) t -> p kc t", p=128)
        cTv = d["ctxT"].rearrange("(kc p) t -> p kc t", p=128)
        x1v = x1T.ap.rearrange("(kc p) t -> p kc t", p=128)
        cnt = [0]
        import os
        LIM = int(os.environ.get("LIM", "99"))

        def project(src_ap, col, slot, rope_t):
            i = cnt[0]
            cnt[0] += 1
            xt = xb[i % 2]
            hT = hTs[i % 2]
            kb.dma("sp", xt.ap, src_ap, [], [xt])
            s_b = modT.ap[:, 8:16, col:col + 1].to_broadcast([128, 8, 128])
            sh_b = modT.ap[:, 0:8, col:col + 1].to_broadcast([128, 8, 128])
            kb.tt("dve", tmpf, tmpf.ap, xt, xt.ap, modT, s_b, ALU.mult)
            kb.tt("pool", hT, hT.ap, tmpf, tmpf.ap, modT, sh_b, ALU.add)
            rope = rope_t is not None
            if rope:
                rt = ropeT[i % 2]
                kb.dma("sp", rt.ap, d["ropeT"][:, :, rope_t * 128:(rope_t + 1) * 128], [], [rt])
            for c in range(4):
                for kc in range(8):
                    kb.mm(ps0, ps0.ap[:, c * 128:(c + 1) * 128], wA, wA.ap[:, kc, c * 128:(c + 1) * 128], hT, hT.ap[:, kc, :],
                          start=(kc == 0), stop=(kc == 7), acc=not (c == 0 and kc == 0))
            if rope:
                for c in range(4):
                    for kc in range(8):
                        kb.mm(ps1, ps1.ap[:, c * 128:(c + 1) * 128], wArot, wArot.ap[:, kc, c * 128:(c + 1) * 128], hT, hT.ap[:, kc, :],
                              start=(kc == 0), stop=(kc == 7), acc=not (c == 0 and kc == 0))
            for kc in range(8):
                kb.mm(ps2, ps2.ap[:, 0:128], wA, wA.ap[:, kc, 512:640], hT, hT.ap[:, kc, :], start=(kc == 0), stop=(kc == 7), acc=(kc != 0))
            if rope:
                for kc in range(8):
                    kb.mm(ps2, ps2.ap[:, 128:256], wArot, wArot.ap[:, kc, 512:640], hT, hT.ap[:, kc, :], start=(kc == 0), stop=(kc == 7), acc=True)
            for kc in range(8):
                kb.mm(ps2, ps2.ap[:, 256:384], hT, hT.ap[:, kc, :], wR, wR.ap[:, kc, 1024:1152], start=(kc == 0), stop=(kc == 7), acc=True)
            q, k, v = slot["q"], slot["k"], slot["v"]
            if rope:
                cosb = rt.ap[:, 0:1, :].to_broadcast([128, 4, 128])
                sinb = rt.ap[:, 1:2, :].to_broadcast([128, 4, 128])
                p0v = ps0.ap.rearrange("p (c t) -> p c t", t=128)
                p1v = ps1.ap.rearrange("p (c t) -> p c t", t=128)
                kb.tt("dve", r1, r1.ap, ps0, p0v, rt, cosb, ALU.mult)
                kb.tt("dve", r2, r2.ap, ps1, p1v, rt, sinb, ALU.mult)
                kb.tt("pool", q, q.ap[:, 0:4, :], r1, r1.ap, r2, r2.ap, ALU.add)
                kb.tt("dve", r1, r1.ap[:, 0, :], ps2, ps2.ap[:, 0:128], rt, rt.ap[:, 0, :], ALU.mult)
                kb.tt("dve", r2, r2.ap[:, 0, :], ps2, ps2.ap[:, 128:256], rt, rt.ap[:, 1, :], ALU.mult)
                kb.tt("pool", k, k.ap[:, 0, :], r1, r1.ap[:, 0, :], r2, r2.ap[:, 0, :], ALU.add)
            else:
                kb.cp("act", q, q.ap[:, 0:4, :], ps0, ps0.ap.rearrange("p (c t) -> p c t", t=128))
                kb.cp("act", k, k.ap[:, 0, :], ps2, ps2.ap[:, 0:128])
            kb.cp("act", v, v.ap[:, 0:2, 0:64], ps2, ps2.ap[:, 256:384].rearrange("p (h e) -> p h e", e=64))
            for c in range(4):
                for kc in range(8):
                    kb.mm(ps0, ps0.ap[:, c * 128:(c + 1) * 128], wR, wR.ap[:, kc, c * 128:(c + 1) * 128], hT, hT.ap[:, kc, :],
                          start=(kc == 0), stop=(kc == 7), acc=not (c == 0 and kc == 0))
            kb.cp("act", q, q.ap[:, 4:8, :], ps0, ps0.ap.rearrange("p (c t) -> p c t", t=128))
            for c in range(4):
                for kc in range(8):
                    kb.mm(ps1, ps1.ap[:, c * 128:(c + 1) * 128], wR, wR.ap[:, kc, 512 + c * 128:512 + (c + 1) * 128], hT, hT.ap[:, kc, :],
                          start=(kc == 0), stop=(kc == 7), acc=not (c == 0 and kc == 0))
            kb.cp("act", k, k.ap[:, 1:5, :], ps1, ps1.ap.rearrange("p (c t) -> p c t", t=128))
            for kc in range(8):
                kb.mm(ps2, ps2.ap, hT, hT.ap[:, kc, :], wR, wR.ap[:, kc, 1152:1664], start=(kc == 0), stop=(kc == 7), acc=(kc != 0))
            kb.cp("act", v, v.ap[:, 2:10, 0:64], ps2, ps2.ap.rearrange("p (h e) -> p h e", e=64))

        def attend(qslot, loc_tiles, bias_tab, mask_prev, mask_next, xres_ap, col, out_col):
            q = qslot["q"]
            for g in range(2):
                pb = 64 * g
                tiles = []
                for cs_ in cslots:
                    tiles.append(dict(qk=[(cs_["k"], cs_["k"].ap[pb:pb + 64, 0, :], q, q.ap[pb:pb + 64, 0:4, :], 0, 512)],
                                      bias=None, pv=[(cs_["v"], cs_["v"].ap[:, g, 0:65], 0, 512)]))
                if loc_tiles is not None:
                    for idx, mk in ((1, mask_prev), (2, None), (3, mask_next)):
                        sl = loc_tiles[idx]
                        bias = None
                        if mk is not None:
                            bias = (maskA, maskA.ap[:, mk:mk + 1, :].to_broadcast([128, 4, 128]))
                        tiles.append(dict(qk=[(sl["k"], sl["k"].ap[pb:pb + 64, 0, :], q, q.ap[pb:pb + 64, 0:4, :], 0, 512)],
                                          bias=bias, pv=[(sl["v"], sl["v"].ap[:, g, 0:65], 0, 512)]))
                for tl in tiles:
                    if tl["bias"] is not None:
                        tl["bias"] = (tl["bias"][0], tl["bias"][1])
                attn_unit_l0(kb, ac, tiles, 0.125, oT, oT.ap[:, 4 * g:4 * g + 4, :],
                             sink=(e64, sinkrow, sinkrow.ap[0:1, 4 * g:4 * g + 4, :]))
            if LIM == 4:
                return
            for u in range(2):
                tiles = []
                srcs = [(cs_, None) for cs_ in cslots]
                if loc_tiles is not None:
                    srcs += [(loc_tiles[t], t) for t in range(5)]
                for (sl, t) in srcs:
                    qk = []
                    pv = []
                    for hh in range(4):
                        h = 4 * u + hh
                        pb = 64 * (h % 2)
                        qk.append((sl["k"], sl["k"].ap[pb:pb + 64, 1 + h // 2, :], q, q.ap[pb:pb + 64, 4 + h // 2, :], hh * 128, 128))
                        pv.append((sl["v"], sl["v"].ap[:, 2 + h, 0:65], hh * 128, 128))
                    bias = None
                    if t is not None:
                        bias = (bias_tab, bias_tab.ap[:, t, 4 * u:4 * u + 4, :].rearrange("p (a two) q -> p two a q", two=2))
                    tiles.append(dict(qk=qk, bias=bias, pv=pv))
                attn_unit_b(kb, ac, tiles, 0.125, oT, oT.ap[:, 8 + 4 * u:8 + 4 * u + 4, :])
            if LIM == 5:
                return
            for dc in range(8):
                pb_ = ps0 if dc < 4 else ps1
                c0 = (dc % 4) * 128
                for h in range(16):
                    kb.mm(pb_, pb_.ap[:, c0:c0 + 128], wout, wout.ap[:, h, dc * 128:(dc + 1) * 128], oT, oT.ap[:, h, :],
                          start=(h == 0), stop=(h == 15), acc=not (dc % 4 == 0 and h == 0))

            if LIM == 6:
                return

            def y_fn(dc):
                pb_ = ps0 if dc < 4 else ps1
                return pb_, pb_.ap[:, (dc % 4) * 128:(dc % 4 + 1) * 128]
            ln_epilogue(kb, lc, y_fn, xres_ap, [], modT.ap[:, 16:24, col:col + 1], modT,
                        lng.ap[:, 0, :], lnb.ap[:, 0, :], (lng, lnb), x1v[:, :, out_col:out_col + 128], x1T, kb.ps[7])

        def attn_unit_l0(kb_, ac_, tiles, scale, out_T, out_ap3, sink):
            for tl in tiles:
                if tl["bias"] is not None:
                    tl["bias3"] = True
            attn_unit3(kb_, ac_, tiles, scale, out_T, out_ap3, sink)

        import os
        LIM = int(os.environ.get("LIM", "99"))
        if LIM == 0:
            kb.P.barrier()
            return
        for c in range(2):
            project(cTv[:, :, c * 128:(c + 1) * 128], 1, cslots[c], None)
        if LIM == 1:
            kb.P.barrier()
            return
        for Tt in range(4):
            project(xTv[:, :, Tt * 128:(Tt + 1) * 128], 0, ring[Tt % 6], Tt)
        if LIM == 2:
            kb.P.barrier()
            return
        for Tt in range(4, 36):
            if LIM in (3, 4, 5, 6) and Tt == 5:
                kb.P.barrier()
                return
            project(xTv[:, :, Tt * 128:(Tt + 1) * 128], 0, ring[Tt % 6], Tt)
            j = Tt - 4
            if j in (0, 1, 30, 31):
                var = {0: 0, 1: 1, 30: 3, 31: 4}[j]
                kb.dma("pool", biasE.ap, d["biasB"][:, var], [], [biasE])
                btab = biasE
            else:
                btab = biasG
            loc = [ring[(j + t) % 6] for t in range(5)]
            attend(ring[(j + 2) % 6], loc, btab, 0 if j == 0 else 1, 3 if j == 31 else 2,
                   xTv[:, :, (j + 2) * 128:(j + 3) * 128], 0, j * 128)
        if with_ctx:
            for c in range(2):
                attend(cslots[c], None, None, None, None, cTv[:, :, c * 128:(c + 1) * 128], 1, 4096 + c * 128)
        kb.P.barrier()


def attn_unit3(kb, ac, tiles, scale, out_T, out_ap3, sink):
    ob = ac.o_banks[ac.oi % len(ac.o_banks)]
    ac.oi += 1
    nt = len(tiles)
    for ti, tl in enumerate(tiles):
        sbk = ac.s_banks[ac.si % len(ac.s_banks)]
        ac.si += 1
        first = True
        for (kT, kap, qT, qap, c0, n) in tl["qk"]:
            kb.mm(sbk, sbk.ap[:, c0:c0 + n], kT, kap, qT, qap, start=True, stop=True, acc=(not first))
            first = False
        E = ac.E[ac.ei % 3]
        ac.ei += 1
        if tl["bias"] is not None:
            bT, bap = tl["bias"]
            sbuf = ac.sb_[ac.bi % 2]
            ac.bi += 1
            kb.stt("dve", sbuf, sbuf.ap.rearrange("p (c t) -> p c t", t=128), sbk, sbk.ap.rearrange("p (c t) -> p c t", t=128),
                   scale, bT, bap, ALU.mult, ALU.add)
            kb.act(E, E.ap, sbuf, sbuf.ap, AF.Exp)
        else:
            kb.act(E, E.ap, sbk, sbk.ap, AF.Exp, scale=scale)
        last = (ti == nt - 1) and sink is None
        for pi, (vT, vap, c0, n) in enumerate(tl["pv"]):
            kb.mm(ob, ob.ap[0:65, c0:c0 + n], vT, vap, E, E.ap[:, c0:c0 + n], start=(ti == 0 and pi == 0), stop=last,
                  acc=not (ti == 0 and pi == 0), skip=True, inc=last)
    if sink is not None:
        e64, srow_T, srow_ap = sink
        kb.mm(ob, ob.ap[0:65, :], e64, e64.ap, srow_T, srow_ap, start=False, stop=True, acc=True, skip=True)
    zr = ac.zr
    kb.op("dve", lambda e: e.reciprocal(out=zr.ap[64:65, :], in_=ob.ap[64:65, :]), [ob], [zr])
    bc = ac.bc
    kb.mm(bc, bc.ap[0:64, :], kb.ones_f, kb.ones_f.ap[64:65, 0:64], zr, zr.ap[64:65, :])
    kb.cp("act", ac.bcs, ac.bcs.ap, bc, bc.ap[0:64, :])
    kb.tt("dve", out_T, out_ap3, ob, ob.ap[0:64, :].rearrange("p (c t) -> p c t", t=128),
          ac.bcs, ac.bcs.ap.rearrange("p (c t) -> p c t", t=128), ALU.mult)


def phase_moe(kb, d, modT, lng, lnb, xin, passes, ctx_tile0, out_fn):
    BIG = 1.0e30
    with ExitStack() as st:
        kb.stack = st
        PT = max(len(p) for p in passes)
        wr = kb.sb([128, 8, 36], F32)
        kb.dma("sp", wr.ap[:, :, 0:4], d["w_group"].rearrange("(kc p) g -> p kc g", p=128), [], [wr])
        kb.dma("sp", wr.ap[:, :, 4:36], d["w_er"].rearrange("(kc p) g -> p kc g", p=128), [], [wr])
        br = kb.sb([36, 1], F32)
        kb.dma("sp", br.ap[0:4, :], d["b_group"], [], [br])
        kb.dma("sp", br.ap[4:36, :], d["b_er"], [], [br])
        ident = kb.sb([128, 128], F32)
        kb.dma("sp", ident.ap, d["ident"], [], [ident])
        sel = kb.sb([32, 32, 128], BF16)
        kb.dma("pool", sel.ap, d["sel"], [], [sel])
        acc = kb.sb([128, 8, PT * 128], F32)
        hT = kb.sb([128, 8, PT * 128], BF16)
        WdT = kb.sb([32, PT * 128], BF16)
        wgu = [kb.sb([128, 8, 1024], BF16) for _ in range(2)]
        wdn = [kb.sb([128, 4, 1024], BF16) for _ in range(2)]
        actT = [kb.sb([128, 4, 512], BF16) for _ in range(2)]
        sg = [kb.sb([128, 512], F32) for _ in range(2)]
        tmp = [kb.sb([128, 512], F32) for _ in range(2)]
        wbs = [kb.sb([128, 512], F32) for _ in range(2)]
        xt = [kb.sb([128, 8, 128], F32) for _ in range(2)]
        hf = kb.sb([128, 8, 128], F32)
        lT = kb.sb([36, 128], F32)
        Lt = kb.sb([128, 36], F32)
        sm = kb.sb([128, 16], F32)
        gm = kb.sb([128, 4], F32)
        pen = kb.sb([128, 4], F32)
        elm = kb.sb([128, 32], F32)
        elm2 = kb.sb([128, 32], F32)
        m1 = kb.sb([128, 32], F32)
        ew = kb.sb([128, 32], F32)
        Wd = kb.sb([128, 32], F32)
        lc = LNCtx(kb)
        xv = xin.ap.rearrange("(kc p) t -> p kc t", p=128)
        wguv = d["w_gate_up"]
        wdnv = d["w_down"]
        ps = kb.ps
        ecount = 0
        xi = 0
        for tiles in passes:
            for li, tile in enumerate(tiles):
                col = 1 if tile >= ctx_tile0 else 0
                x_ = xt[xi % 2]
                xi += 1
                kb.dma("sp", x_.ap, xv[:, :, tile * 128:(tile + 1) * 128], [xin], [x_])
                s_b = modT.ap[:, 32:40, col:col + 1].to_broadcast([128, 8, 128])
                sh_b = modT.ap[:, 24:32, col:col + 1].to_broadcast([128, 8, 128])
                kb.tt("dve", hf, hf.ap, x_, x_.ap, modT, s_b, ALU.mult)
                kb.tt("pool", hf, hf.ap, hf, hf.ap, modT, sh_b, ALU.add)
                kb.cp("act", hT, hT.ap[:, :, li * 128:(li + 1) * 128], hf, hf.ap)
                p7 = ps[7]
                for kc in range(8):
                    kb.mm(p7, p7.ap[0:36, 0:128], wr, wr.ap[:, kc, :], hf, hf.ap[:, kc, :], start=(kc == 0), stop=(kc == 7))
                kb.act(lT, lT.ap, p7, p7.ap[0:36, 0:128], AF.Identity, scale=1.0, bias=br.ap[:, 0:1], extra=[br])
                kb.mm(p7, p7.ap[:, 128:164], lT, lT.ap, ident, ident.ap[0:36, 0:36], acc=True)
                kb.cp("dve", Lt, Lt.ap, p7, p7.ap[:, 128:164])
                gl = Lt.ap[:, 0:4]
                el = Lt.ap[:, 4:36]
                kb.op("dve", lambda e: e.reduce_max(out=sm.ap[:, 0:1], in_=gl, axis=AX.X), [Lt], [sm])
                kb.ts("dve", sm, sm.ap[:, 1:2], sm, sm.ap[:, 0:1], -1.0, None, ALU.mult)
                kb.op("act", lambda e: e.activation(out=gm.ap, in_=gl, func=AF.Exp, bias=sm.ap[:, 1:2], scale=1.0, accum_out=sm.ap[:, 2:3]),
                      [Lt, sm], [gm, sm])
                kb.op("dve", lambda e: e.reciprocal(out=sm.ap[:, 3:4], in_=sm.ap[:, 2:3]), [sm], [sm])
                kb.ts("dve", gm, gm.ap, Lt, gl, sm.ap[:, 0:1], None, ALU.is_ge, extra=[sm])
                kb.ts("dve", pen, pen.ap, gm, gm.ap, BIG, -BIG, ALU.mult, ALU.add)
                kb.tt("dve", elm, elm.ap.rearrange("p (g e) -> p g e", e=8), Lt, el.rearrange("p (g e) -> p g e", e=8),
                      pen, pen.ap.unsqueeze(2).to_broadcast([128, 4, 8]), ALU.add)
                kb.op("dve", lambda e: e.reduce_max(out=sm.ap[:, 4:5], in_=elm.ap, axis=AX.X), [elm], [sm])
                kb.ts("dve", m1, m1.ap, elm, elm.ap, sm.ap[:, 4:5], None, ALU.is_ge, extra=[sm])
                kb.stt("dve", elm2, elm2.ap, m1, m1.ap, -BIG, elm, elm.ap, ALU.mult, ALU.add)
                kb.op("dve", lambda e: e.reduce_max(out=sm.ap[:, 6:7], in_=elm2.ap, axis=AX.X), [elm2], [sm])
                kb.ts("dve", m1, m1.ap, elm, elm.ap, sm.ap[:, 6:7], None, ALU.is_ge, extra=[sm])
                kb.ts("dve", sm, sm.ap[:, 5:6], sm, sm.ap[:, 4:5], -1.0, None, ALU.mult)
                kb.act(ew, ew.ap, elm, elm.ap, AF.Exp, scale=1.0, bias=sm.ap[:, 5:6], extra=[sm])
                kb.act(sm, sm.ap[:, 7:8], sm, sm.ap[:, 6:7], AF.Exp, scale=1.0, bias=sm.ap[:, 5:6])
                kb.ts("dve", sm, sm.ap[:, 7:8], sm, sm.ap[:, 7:8], 1.0, None, ALU.add)
                kb.op("dve", lambda e: e.reciprocal(out=sm.ap[:, 7:8], in_=sm.ap[:, 7:8]), [sm], [sm])
                kb.tt("dve", sm, sm.ap[:, 8:9], sm, sm.ap[:, 7:8], sm, sm.ap[:, 3:4], ALU.mult)
                kb.stt("dve", Wd, Wd.ap, ew, ew.ap, sm.ap[:, 8:9], m1, m1.ap, ALU.mult, ALU.mult, extra=[sm])
                kb.mm(p7, p7.ap[0:32, 256:384], Wd, Wd.ap, ident, ident.ap, acc=True)
                kb.cp("act", WdT, WdT.ap[:, li * 128:(li + 1) * 128], p7, p7.ap[0:32, 256:384])
            nt = len(tiles)
            groups = []
            c = 0
            while c < nt:
                n = min(4, nt - c)
                groups.append((c * 128, n * 128))
                c += n
            ng = len(groups)
            state = {}

            def load_w(e):
                nonlocal ecount
                wg = wgu[ecount % 2]
                wd = wdn[ecount % 2]
                ecount += 1
                gv = wguv[e].rearrange("(kc p) f -> p kc f", p=128)
                kb.dma("pool", wg.ap[:, 0:4, :], gv[:, 0:4, :], [], [wg])
                kb.dma("pool", wg.ap[:, 4:8, :], gv[:, 4:8, :], [], [wg])
                kb.dma("pool", wd.ap, wdnv[e].rearrange("(kc p) f -> p kc f", p=128), [], [wd])
                state[e] = (wg, wd)

            def emit_gu(e, gidx, gi):
                wg, wd = state[e]
                c0, n = groups[gidx]
                aT = actT[gi % 2]
                wb_ = wbs[gi % 2]
                kb.mm(ps[4], ps[4].ap[:, 0:n], sel, sel.ap[:, e, :], WdT, WdT.ap[:, c0:c0 + n])
                kb.cp("act", wb_, wb_.ap[:, 0:n], ps[4], ps[4].ap[:, 0:n])
                for j in range(4):
                    G = ps[j % 2]
                    U = ps[2 + j % 2]
                    for kc in range(8):
                        kb.mm(G, G.ap[:, 0:n], wg, wg.ap[:, kc, j * 128:(j + 1) * 128], hT, hT.ap[:, kc, c0:c0 + n],
                              start=(kc == 0), stop=(kc == 7))
                    for kc in range(8):
                        kb.mm(U, U.ap[:, 0:n], wg, wg.ap[:, kc, 512 + j * 128:512 + (j + 1) * 128], hT, hT.ap[:, kc, c0:c0 + n],
                              start=(kc == 0), stop=(kc == 7))
                    s_ = sg[j % 2]
                    t_ = tmp[j % 2]
                    kb.act(s_, s_.ap[:, 0:n], G, G.ap[:, 0:n], AF.Silu)
                    kb.tt("dve", t_, t_.ap[:, 0:n], U, U.ap[:, 0:n], s_, s_.ap[:, 0:n], ALU.mult)
                    kb.tt("pool", aT, aT.ap[:, j, 0:n], t_, t_.ap[:, 0:n], wb_, wb_.ap[:, 0:n], ALU.mult)
                return (e, c0, n, aT, wd)

            def emit_y(item):
                e, c0, n, aT, wd = item
                for dc in range(8):
                    Y = ps[5 + dc % 2]
                    for j in range(4):
                        kb.mm(Y, Y.ap[:, 0:n], wd, wd.ap[:, j, dc * 128:(dc + 1) * 128], aT, aT.ap[:, j, 0:n],
                              start=(j == 0), stop=(j == 3))
                    if e == 0:
                        kb.cp("dve", acc, acc.ap[:, dc, c0:c0 + n], Y, Y.ap[:, 0:n])
                    else:
                        kb.tt("dve", acc, acc.ap[:, dc, c0:c0 + n], Y, Y.ap[:, 0:n], acc, acc.ap[:, dc, c0:c0 + n], ALU.add)

            load_w(0)
            prev = None
            gi = 0
            for e in range(32):
                for gidx in range(ng):
                    cur = emit_gu(e, gidx, gi)
                    gi += 1
                    if prev is not None:
                        emit_y(prev)
                    if gidx == 0 and e + 1 < 32:
                        load_w(e + 1)
                    prev = cur
            emit_y(prev)
            for li, tile in enumerate(tiles):
                col = 1 if tile >= ctx_tile0 else 0
                oap, oT_ = out_fn(tile)

                def y_fn(dc, li=li):
                    return acc, acc.ap[:, dc, li * 128:(li + 1) * 128]
                ln_epilogue(kb, lc, y_fn, xv[:, :, tile * 128:(tile + 1) * 128], [xin], modT.ap[:, 40:48, col:col + 1], modT,
                            lng.ap[:, 1, :], lnb.ap[:, 1, :], (lng, lnb), oap, oT_, ps[7])
        kb.P.barrier()


def _rope_np(pos, dim):
    pos = np.asarray(pos)
    pos_r = (pos // 64).astype(np.float32)
    pos_c = (pos % 64).astype(np.float32)
    quarter = dim // 4
    inv = np.power(np.float32(10000.0), -(np.arange(quarter, dtype=np.float32) / np.float32(quarter))).astype(np.float32)
    ang_r = pos_r[:, None] * inv
    ang_c = pos_c[:, None] * inv
    ang = np.concatenate([ang_r, ang_r, ang_c, ang_c], -1).astype(np.float32)
    return np.cos(ang).astype(np.float32), np.sin(ang).astype(np.float32)


def _ext_tokens(r):
    base = r * 4096 - 256 + np.arange(NEXT)
    if r == 0:
        base[0:256] = 256 + np.arange(256)
    if r == 3:
        base[4352:4608] = 248 * 64 + np.arange(256)
    return base


def _bias_tables(rpb, r):
    out = np.full((128, 5, 5, 8, 128), NEG, np.float32)
    kk = np.arange(128)
    kr, kc = kk // 64, kk % 64
    qr, qc = kk // 64, kk % 64
    cs = np.clip(qc - 8, 0, 48)
    validc = (kc[:, None] >= cs[None, :]) & (kc[:, None] < cs[None, :] + 16)
    dc = np.clip(kc[:, None] - qc[None, :], -15, 15) + 15
    for vi, j in enumerate([0, 1, 2, 30, 31]):
        for t in range(5):
            ext_row = 2 * j + 2 * t + kr
            lr = 2 * j + qr
            xw = ext_row[:, None] - lr[None, :]
            validr = (xw >= 0) & (xw <= 7)
            if r == 0:
                act = np.where(ext_row < 4, ext_row + 4, ext_row - 4)
            elif r == 3:
                act = np.where(ext_row >= 68, 248 + (ext_row - 68), 188 + ext_row)
            else:
                act = r * 64 - 4 + ext_row
            rq = r * 64 + lr
            dr = act[:, None] - rq[None, :] + 7
            ws = np.clip(rq - 4, 0, 248)
            inwin = (act[:, None] >= ws[None, :]) & (act[:, None] < ws[None, :] + 8)
            assert np.array_equal(validr & inwin, validr), (r, j, t)
            valid = validr & validc
            drc = np.clip(dr, 0, 14)
            vals = rpb[:, drc, dc]
            out[:, vi, t, :, :] = np.where(valid[:, None, :], vals.transpose(1, 0, 2), np.float32(NEG))
    return out


def _mask_a(r):
    kk = np.arange(128)
    prev = np.where(kk[:, None] >= kk[None, :], 0.0, NEG).astype(np.float32)
    nxt = np.where(kk[:, None] <= kk[None, :], 0.0, NEG).astype(np.float32)
    allneg = np.full((128, 128), NEG, np.float32)
    m = np.stack([allneg if r == 0 else prev, prev, nxt, allneg if r == 3 else nxt], axis=1)
    return np.ascontiguousarray(m)


def _fm(v, nch):
    return np.ascontiguousarray(np.asarray(v, np.float32).reshape(nch, 128).T)


def _moe_inputs(inp, l):
    sel = np.zeros((32, 32, 128), np.float32)
    for e in range(32):
        sel[e, e, :] = 1.0
    return {
        "w_group": np.ascontiguousarray(inp["w_group"][l]),
        "b_group": np.ascontiguousarray(inp["b_group"][l].reshape(4, 1)),
        "w_er": np.ascontiguousarray(inp["w_exp_router"][l]),
        "b_er": np.ascontiguousarray(inp["b_exp_router"][l].reshape(32, 1)),
        "w_gate_up": np.ascontiguousarray(inp["w_gate_up"][l]),
        "w_down": np.ascontiguousarray(inp["w_down"][l]),
        "ident": np.eye(128, dtype=np.float32),
        "sel": sel,
    }


def _common_inputs(inp, l, b):
    cT = np.stack([_fm(inp["c"][b], 8), _fm(inp["c_ctx"], 8)], axis=2)
    lg = np.stack([_fm(inp["ln_g"][l, 0], 8), _fm(inp["ln_g"][l, 1], 8)], axis=1)
    lb = np.stack([_fm(inp["ln_b"][l, 0], 8), _fm(inp["ln_b"][l, 1], 8)], axis=1)
    return {
        "cT": np.ascontiguousarray(cT),
        "w_ada": np.ascontiguousarray(inp["w_ada"][l]),
        "b_adaT": _fm(inp["b_ada"][l], 48),
        "ln_gT": np.ascontiguousarray(lg),
        "ln_bT": np.ascontiguousarray(lb),
    }


def _declare_moe(kb, sfx=""):
    return {
        "w_group": kb.din("w_group" + sfx, [1024, 4]), "b_group": kb.din("b_group" + sfx, [4, 1]),
        "w_er": kb.din("w_er" + sfx, [1024, 32]), "b_er": kb.din("b_er" + sfx, [32, 1]),
        "w_gate_up": kb.din("w_gate_up" + sfx, [32, 1024, 1024]), "w_down": kb.din("w_down" + sfx, [32, 512, 1024]),
        "ident": kb.din("ident" + sfx, [128, 128]), "sel": kb.din("sel" + sfx, [32, 32, 128]),
    }


def _declare_common(kb, sfx=""):
    return {
        "cT": kb.din("cT" + sfx, [128, 8, 2]), "w_ada": kb.din("w_ada" + sfx, [1024, 6144]),
        "b_adaT": kb.din("b_adaT" + sfx, [128, 48]), "ln_gT": kb.din("ln_gT" + sfx, [128, 2, 8]), "ln_bT": kb.din("ln_bT" + sfx, [128, 2, 8]),
    }


def build_launch1(debug=False, stop=None):
    kb = KB()
    d = _declare_common(kb)
    d.update(_declare_moe(kb))
    d.update({
        "xT": kb.din("xT", [1024, NEXT]), "ctxT": kb.din("ctxT", [1024, 256]),
        "ab_w_in": kb.din("ab_w_in", [1024, 2304]), "ab_w_out": kb.din("ab_w_out", [1024, 1024]),
        "a_sink": kb.din("a_sink", [1, 8]), "biasB": kb.din("biasB", [128, 5, 5, 8, 128]),
        "maskA": kb.din("maskA", [128, 4, 128]), "ropeT": kb.din("ropeT", [128, 2, NEXT]),
    })
    out = T(kb.dout("x2T", [1024, 4352]))
    if debug:
        x1T = T(kb.dout("x1T", [1024, 4352]))
    else:
        x1T = kb.dscr("x1T", [1024, 4352], F32)
    modT = phase_mod(kb, d["w_ada"], d["b_adaT"], d["cT"])
    lng, lnb = load_ln(kb, d["ln_gT"], d["ln_bT"])
    if stop == "mod":
        dbg = T(kb.dout("modT", [128, 48, 2]))
        kb.dma("sp", dbg.ap, modT.ap, [modT], [dbg])
        kb.P.barrier()
        return kb
    phase_l0_attn(kb, d, modT, lng, lnb, x1T)
    if stop == "l0":
        kb.P.barrier()
        return kb
    ov = out.ap.rearrange("(kc p) t -> p kc t", p=128)
    passes = [list(range(0, 10)), list(range(10, 18)), list(range(18, 26)), list(range(26, 34))]
    phase_moe(kb, d, modT, lng, lnb, x1T, passes, 32, lambda tile: (ov[:, :, tile * 128:(tile + 1) * 128], out))
    kb.P.barrier()
    return kb


def launch1_inputs(inp):
    maps = []
    rpb = np.asarray(inp["b_rpb"][0], np.float32)
    moe = _moe_inputs(inp, 0)
    tabs = {r: _bias_tables(rpb, r) for r in range(4)}
    for core in range(8):
        b, r = core // 4, core % 4
        et = _ext_tokens(r)
        cos, sin = _rope_np(et, 64)
        rope = np.stack([np.concatenate([cos.T, cos.T], 0), np.concatenate([sin.T, sin.T], 0)], axis=1)
        m = _common_inputs(inp, 0, b)
        m.update(moe)
        m.update({
            "xT": np.ascontiguousarray(inp["x"][b][et].T),
            "ctxT": np.ascontiguousarray(inp["ctx"][b].T),
            "ab_w_in": np.ascontiguousarray(inp["ab_w_in"][0]),
            "ab_w_out": np.ascontiguousarray(inp["ab_w_out"][0]),
            "a_sink": np.ascontiguousarray(inp["a_sink"][0].reshape(1, 8)),
            "biasB": tabs[r], "maskA": _mask_a(r), "ropeT": np.ascontiguousarray(rope),
        })
        maps.append(m)
    return maps


def attn_unit_b(kb, ac, tiles, scale, out_T, out_ap3):
    ob = ac.o_banks[ac.oi % len(ac.o_banks)]
    ac.oi += 1
    X, Y = ac.s_banks[0], ac.s_banks[1]
    bi0 = kb.ps.index(X)
    assert kb.ps.index(Y) == bi0 + 1
    pview = kb.psall[:, bi0 * 512:(bi0 + 2) * 512].rearrange("p (b c) -> p b c", b=2)[:, :, 0:256]
    nt = len(tiles)
    for ti, tl in enumerate(tiles):
        for hh, (kT, kap, qT, qap, c0, n) in enumerate(tl["qk"]):
            bk = X if hh % 2 == 0 else Y
            cc = (hh // 2) * 128
            kb.mm(bk, bk.ap[:, cc:cc + 128], kT, kap, qT, qap, start=True, stop=True, acc=(hh >= 2))
        E = ac.E[ac.ei % 3]
        ac.ei += 1
        ev = E.ap.rearrange("p (b c) -> p b c", b=2)
        if tl["bias"] is not None:
            bT, bap = tl["bias"]
            sbuf = ac.sb_[ac.bi % 2]
            ac.bi += 1
            for b_, bk_ in enumerate((X, Y)):
                kb.stt("dve", sbuf, sbuf.ap[:, b_ * 256:(b_ + 1) * 256].rearrange("p (a q) -> p a q", a=2),
                       bk_, bk_.ap[:, 0:256].rearrange("p (a q) -> p a q", a=2), scale, bT, bap[:, b_, :, :], ALU.mult, ALU.add)
            kb.act(E, E.ap, sbuf, sbuf.ap, AF.Exp)
        else:
            kb.op("act", lambda e: e.activation(out=ev, in_=pview, func=AF.Exp, scale=scale), [X, Y], [E])
        last = (ti == nt - 1)
        for hh, (vT, vap, c0, n) in enumerate(tl["pv"]):
            ec = (hh % 2) * 256 + (hh // 2) * 128
            kb.mm(ob, ob.ap[0:65, hh * 128:(hh + 1) * 128], vT, vap, E, E.ap[:, ec:ec + 128], start=(ti == 0 and hh == 0), stop=last,
                  acc=not (ti == 0 and hh == 0), skip=True, inc=last)
    zr = ac.zr
    kb.op("dve", lambda e: e.reciprocal(out=zr.ap[64:65, :], in_=ob.ap[64:65, :]), [ob], [zr])
    bc = ac.bc
    kb.mm(bc, bc.ap[0:64, :], kb.ones_f, kb.ones_f.ap[64:65, 0:64], zr, zr.ap[64:65, :])
    kb.cp("act", ac.bcs, ac.bcs.ap, bc, bc.ap[0:64, :])
    kb.tt("dve", out_T, out_ap3, ob, ob.ap[0:64, :].rearrange("p (c t) -> p c t", t=128),
          ac.bcs, ac.bcs.ap.rearrange("p (c t) -> p c t", t=128), ALU.mult)


NK = 16640
NKT = NK // 128


class Banks:
    def __init__(self, kb, idx):
        self.kb = kb
        self.idx = list(idx)
        self.i = 0

    def get(self):
        b = self.kb.ps[self.idx[self.i % len(self.idx)]]
        self.i += 1
        return b


def l1_scratch(kb):
    return dict(
        kTm=kb.dscr("kTm", [8, 96, NK], BF16), Vm=kb.dscr("Vm", [8, 128, NKT, 66], BF16),
        kTd=kb.dscr("kTd", [4, 128, NK], BF16), Vd=kb.dscr("Vd", [4, 128, NKT, 128], BF16),
        qTm=kb.dscr("qTm", [8, 96, 4096], BF16), qTd=kb.dscr("qTd", [4, 128, 4096], BF16),
        oTs=kb.dscr("oTs", [12, 128, 4096], BF16), x1b=kb.dscr("x1b", [1024, 4096], F32))


def phase_l1_proj(kb, d, modT, scr):
    with ExitStack() as st:
        kb.stack = st
        win = d["cd_w_in"].rearrange("(kc p) c -> p kc c", p=128)
        wq_c = kb.sb([128, 8, 384], BF16)
        wkv = kb.sb([128, 8, 256], BF16)
        wkr = kb.sb([128, 8, 96], BF16)
        wkr_r = kb.sb([128, 8, 96], BF16)
        wdq = kb.sb([128, 8, 512], BF16)
        wdq_r = kb.sb([128, 8, 512], BF16)
        wdk = kb.sb([128, 8, 512], BF16)
        wdk_r = kb.sb([128, 8, 512], BF16)
        wdv = kb.sb([128, 8, 512], BF16)
        kb.dma("pool", wq_c.ap, win[:, :, 0:384], [], [wq_c])
        kb.dma("pool", wkv.ap, win[:, :, 384:640], [], [wkv])
        kb.memset("dve", wkr, wkr.ap, 0.0)
        kb.memset("dve", wkr_r, wkr_r.ap, 0.0)
        kb.dma("pool", wkr.ap[:, :, 64:96], win[:, :, 640:672], [], [wkr])
        kb.dma("pool", wdq.ap, win[:, :, 672:1184], [], [wdq])
        kb.dma("pool", wdk.ap, win[:, :, 1184:1696], [], [wdk])
        kb.dma("pool", wdv.ap, win[:, :, 1696:2208], [], [wdv])

        def mkrot(dst, src, dap, sap, s_):
            sv = sap.rearrange("p k (g two s) -> p k g two s", two=2, s=s_)
            dv = dap.rearrange("p k (g two s) -> p k g two s", two=2, s=s_)
            for kc in range(sap.shape[1]):
                kb.ts("dve", dst, dv[:, kc, :, 0, :], src, sv[:, kc, :, 1, :], -1.0, None, ALU.mult)
                kb.cp("dve", dst, dv[:, kc, :, 1, :], src, sv[:, kc, :, 0, :])
        mkrot(wkr_r, wkr, wkr_r.ap[:, :, 64:96], wkr.ap[:, :, 64:96], 8)
        mkrot(wdq_r, wdq, wdq_r.ap, wdq.ap, 16)
        mkrot(wdk_r, wdk, wdk_r.ap, wdk.ap, 16)
        wuq = kb.sb([128, 3, 768], BF16)
        wuq_r = kb.sb([128, 3, 768], BF16)
        kb.dma("pool", wuq.ap, d["c_w_uq"].rearrange("(kc p) c -> p kc c", p=128), [], [wuq])
        kb.memset("dve", wuq_r, wuq_r.ap, 0.0)
        for h in range(8):
            mkrot(wuq_r, wuq, wuq_r.ap[:, :, h * 96 + 64:h * 96 + 96], wuq.ap[:, :, h * 96 + 64:h * 96 + 96], 8)
        wukv = kb.sb([128, 2, 1024], BF16)
        kb.dma("pool", wukv.ap, d["c_w_ukv"].rearrange("(kc p) c -> p kc c", p=128), [], [wukv])
        qg = kb.sb([128, 3], F32)
        kvg = kb.sb([128, 2], F32)
        kb.dma("sp", qg.ap, d["c_q_normT"], [], [qg])
        kb.dma("sp", kvg.ap, d["c_kv_normT"], [], [kvg])
        eps6 = kb.sb([128, 1], F32)
        kb.memset("dve", eps6, eps6.ap, 1e-6)

        xb = [kb.sb([128, 8, 512], F32) for _ in range(2)]
        hTs = [kb.sb([128, 8, 512], BF16) for _ in range(2)]
        r64 = [kb.sb([128, 2, 512], F32) for _ in range(2)]
        r32 = [kb.sb([128, 2, 512], F32) for _ in range(2)]
        sq = kb.sb([128, 3, 512], BF16)
        rs = kb.sb([128, 512], F32)
        ckvn = kb.sb([128, 2, 512], BF16)
        cqn = kb.sb([128, 3, 512], BF16)
        kn = kb.sb([64, 8, 512], BF16)
        krs = kb.sb([96, 512], BF16)
        t1 = kb.sb([128, 2, 512], F32)
        t2 = kb.sb([128, 2, 512], F32)
        dks = [kb.sb([128, 4, 512], BF16) for _ in range(2)]
        vms = kb.sb([128, 8, 4, 66], BF16)
        kb.memset("pool", vms, vms.ap[:, :, :, 64:66], 1.0)
        vds = kb.sb([128, 4, 4, 128], BF16)
        qs = [kb.sb([96, 512], BF16) for _ in range(2)]
        bk = Banks(kb, range(8))
        xv = d["x1T"].rearrange("(kc p) t -> p kc t", p=128)
        cv = d["ctx1T"].rearrange("(kc p) t -> p kc t", p=128)
        xrd = d.get("x_reads", [])
        di = [0]

        def rms(chunks, nch, gT, dstT, n):
            for c, (pt, pap) in enumerate(chunks):
                kb.act(sq, sq.ap[:, c, 0:n], pt, pap, AF.Square)
            ssb = bk.get()
            for c in range(nch):
                kb.mm(ssb, ssb.ap[:, 0:n], kb.ones_b, kb.ones_b.ap, sq, sq.ap[:, c, 0:n], start=(c == 0), stop=(c == nch - 1))
            kb.act(rs, rs.ap[:, 0:n], ssb, ssb.ap[:, 0:n], AF.Ln, scale=1.0 / (128 * nch), bias=eps6.ap[:, 0:1], extra=[eps6])
            kb.act(rs, rs.ap[:, 0:n], rs, rs.ap[:, 0:n], AF.Exp, scale=-0.5)
            for c, (pt, pap) in enumerate(chunks):
                kb.stt("dve", dstT, dstT.ap[:, c, 0:n], pt, pap, gT.ap[:, c:c + 1], rs, rs.ap[:, 0:n], ALU.mult, ALU.mult, extra=[gT])

        def proj_fm(w, c0, hT, n, M=128):
            b = bk.get()
            for kc in range(8):
                kb.mm(b, b.ap[0:M, 0:n], w, w.ap[:, kc, c0:c0 + M], hT, hT.ap[:, kc, 0:n], start=(kc == 0), stop=(kc == 7))
            return b

        def rope_pair(dstT, dst_ap, p1, p2, tab, rows, n, eng2="pool"):
            lo, hi = rows
            kb.tt("dve", t1, t1.ap[lo:hi, 0, 0:n], p1, p1.ap[lo:hi, 0:n], tab, tab.ap[lo:hi, 0, 0:n], ALU.mult)
            kb.tt("dve", t2, t2.ap[lo:hi, 0, 0:n], p2, p2.ap[lo:hi, 0:n], tab, tab.ap[lo:hi, 1, 0:n], ALU.mult)
            kb.tt(eng2, dstT, dst_ap, t1, t1.ap[lo:hi, 0, 0:n], t2, t2.ap[lo:hi, 0, 0:n], ALU.add)

        for g in range(33):
            ctx = (g == 32)
            own = (g < 8)
            n = 256 if ctx else 512
            col = 1 if ctx else 0
            k0 = g * 512
            i = di[0]
            di[0] += 1
            xt, hT = xb[i % 2], hTs[i % 2]
            src = cv if ctx else xv[:, :, k0:k0 + 512]
            kb.dma("sp", xt.ap[:, :, 0:n], src, xrd, [xt])
            s_b = modT.ap[:, 8:16, col:col + 1].to_broadcast([128, 8, n])
            sh_b = modT.ap[:, 0:8, col:col + 1].to_broadcast([128, 8, n])
            kb.tt("dve", xt, xt.ap[:, :, 0:n], xt, xt.ap[:, :, 0:n], modT, s_b, ALU.mult)
            kb.tt("pool", hT, hT.ap[:, :, 0:n], xt, xt.ap[:, :, 0:n], modT, sh_b, ALU.add)
            if not ctx:
                a64, a32 = r64[i % 2], r32[i % 2]
                kb.dma("sp", a64.ap, d["rope64"][:, :, k0:k0 + 512], [], [a64])
                kb.dma("sp", a32.ap, d["rope32"][:, :, k0:k0 + 512], [], [a32])
            pc = [proj_fm(wkv, c * 128, hT, n) for c in range(2)]
            rms([(p, p.ap[:, 0:n]) for p in pc], 2, kvg, ckvn, n)
            for h in range(8):
                b = bk.get()
                for c in range(2):
                    kb.mm(b, b.ap[0:64, 0:n], wukv, wukv.ap[:, c, h * 128:h * 128 + 64], ckvn, ckvn.ap[:, c, 0:n], start=(c == 0), stop=(c == 1))
                kb.cp("act", kn, kn.ap[:, h, 0:n], b, b.ap[0:64, 0:n])
            kb.dma("pool", scr["kTm"].ap[:, 0:64, k0:k0 + n].rearrange("h p n -> p h n"), kn.ap[:, :, 0:n], [kn], [scr["kTm"]])
            p1 = proj_fm(wkr, 0, hT, n, M=96)
            if not ctx:
                p2 = proj_fm(wkr_r, 0, hT, n, M=96)
                rope_pair(krs, krs.ap[64:96, 0:n], p1, p2, a32, (64, 96), n)
            else:
                kb.cp("act", krs, krs.ap[64:96, 0:n], p1, p1.ap[64:96, 0:n])
            for h in range(8):
                kb.dma("pool", scr["kTm"].ap[h, 64:96, k0:k0 + n], krs.ap[64:96, 0:n], [krs], [scr["kTm"]])
            nt = n // 128
            for tt in range(nt):
                b = bk.get()
                for c in range(2):
                    kb.mm(b, b.ap, ckvn, ckvn.ap[:, c, tt * 128:(tt + 1) * 128],
                          wukv, wukv.ap[:, c, :].rearrange("p (h x) -> p h x", x=128)[:, :, 64:128], start=(c == 0), stop=(c == 1))
                kb.cp("act", vms, vms.ap[:, :, tt, 0:64], b, b.ap.rearrange("p (h e) -> p h e", e=64))
            kb.dma("pool", scr["Vm"].ap[:, :, g * 4:g * 4 + nt, :].rearrange("h p t e -> p h t e"), vms.ap[:, :, 0:nt, :], [vms], [scr["Vm"]])
            dk_ = dks[i % 2]
            for half in range(2):
                pl = [proj_fm(wdk, (2 * half + c) * 128, hT, n) for c in range(2)]
                if not ctx:
                    pr = [proj_fm(wdk_r, (2 * half + c) * 128, hT, n) for c in range(2)]
                    for c in range(2):
                        rope_pair(dk_, dk_.ap[:, 2 * half + c, 0:n], pl[c], pr[c], a64, (0, 128), n)
                else:
                    for c in range(2):
                        kb.cp("act", dk_, dk_.ap[:, 2 * half + c, 0:n], pl[c], pl[c].ap[:, 0:n])
            kb.dma("pool", scr["kTd"].ap[:, :, k0:k0 + n].rearrange("h p n -> p h n"), dk_.ap[:, :, 0:n], [dk_], [scr["kTd"]])
            for tt in range(nt):
                b = bk.get()
                for kc in range(8):
                    kb.mm(b, b.ap, hT, hT.ap[:, kc, tt * 128:(tt + 1) * 128], wdv, wdv.ap[:, kc, :], start=(kc == 0), stop=(kc == 7))
                kb.cp("act", vds, vds.ap[:, :, tt, :], b, b.ap.rearrange("p (h e) -> p h e", e=128))
            kb.dma("pool", scr["Vd"].ap[:, :, g * 4:g * 4 + nt, :].rearrange("h p t e -> p h t e"), vds.ap[:, :, 0:nt, :], [vds], [scr["Vd"]])
            if own:
                pq = [proj_fm(wq_c, c * 128, hT, n) for c in range(3)]
                rms([(p, p.ap[:, 0:n]) for p in pq], 3, qg, cqn, n)
                for h in range(8):
                    q_ = qs[h % 2]
                    b1 = bk.get()
                    b2 = bk.get()
                    for c in range(3):
                        kb.mm(b1, b1.ap[0:96, 0:n], wuq, wuq.ap[:, c, h * 96:(h + 1) * 96], cqn, cqn.ap[:, c, 0:n], start=(c == 0), stop=(c == 2))
                    for c in range(3):
                        kb.mm(b2, b2.ap[0:96, 0:n], wuq_r, wuq_r.ap[:, c, h * 96:(h + 1) * 96], cqn, cqn.ap[:, c, 0:n], start=(c == 0), stop=(c == 2))
                    kb.cp("act", q_, q_.ap[0:64, 0:n], b1, b1.ap[0:64, 0:n])
                    rope_pair(q_, q_.ap[64:96, 0:n], b1, b2, a32, (64, 96), n)
                    kb.dma("pool", scr["qTm"].ap[h, :, k0:k0 + n], q_.ap[:, 0:n], [q_], [scr["qTm"]])
                dq_ = dks[(i + 1) % 2]
                for half in range(2):
                    pl = [proj_fm(wdq, (2 * half + c) * 128, hT, n) for c in range(2)]
                    pr = [proj_fm(wdq_r, (2 * half + c) * 128, hT, n) for c in range(2)]
                    for c in range(2):
                        rope_pair(dq_, dq_.ap[:, 2 * half + c, 0:n], pl[c], pr[c], a64, (0, 128), n)
                kb.dma("pool", scr["qTd"].ap[:, :, k0:k0 + n].rearrange("h p n -> p h n"), dq_.ap[:, :, 0:n], [dq_], [scr["qTd"]])
        kb.P.barrier()


def phase_l1_attn(kb, d, scr):
    with ExitStack() as st:
        kb.stack = st
        lp = kb.sb([128, 256], F32)
        kb.dma("sp", lp.ap, d["d_lambda"].rearrange("a b -> (a b)").partition_broadcast(128), [], [lp])
        pr_ = kb.sb([128, 128], F32)
        lpv = lp.ap.rearrange("p (a b) -> p a b", b=64)
        kb.tt("dve", pr_, pr_.ap.rearrange("p (a b) -> p a b", b=64), lp, lpv[:, 0:4:2, :], lp, lpv[:, 1:4:2, :], ALU.mult)
        ssum = kb.sb([128, 4], F32)
        kb.op("dve", lambda e: e.reduce_sum(out=ssum.ap[:, 0:2], in_=pr_.ap.rearrange("p (a b) -> p a b", b=64), axis=AX.X), [pr_], [ssum])
        kb.act(ssum, ssum.ap[:, 0:2], ssum, ssum.ap[:, 0:2], AF.Exp)
        kb.tt("dve", ssum, ssum.ap[:, 2:3], ssum, ssum.ap[:, 1:2], ssum, ssum.ap[:, 0:1], ALU.subtract)
        kb.ts("dve", ssum, ssum.ap[:, 2:3], ssum, ssum.ap[:, 2:3], -LAM_INIT, None, ALU.add)
        subl = kb.sb([128, 1], F32)
        kb.dma("sp", subl.ap, d["d_sublnT"], [], [subl])
        kb.ts("dve", subl, subl.ap, subl, subl.ap, 1.0 - LAM_INIT, None, ALU.mult)
        eps6 = kb.sb([128, 1], F32)
        kb.memset("dve", eps6, eps6.ap, 1e-6)

        E = [kb.sb([128, 2, 512], BF16) for _ in range(3)]
        kT = kb.sb([128, NK], BF16)
        V = kb.sb([128, NKT, 128], BF16)
        qT = kb.sb([128, 4096], BF16)
        zr = kb.sb([128, 512], F32)
        bcs = kb.sb([128, 512], F32)
        ta = kb.sb([128, 512], F32)
        tb = kb.sb([128, 512], F32)
        osb = [kb.sb([128, 512], BF16) for _ in range(2)]
        ei = 0
        oi = 0
        sp_i = 0
        ps = kb.ps

        def load(dst, dst_ap_fn, src_T, src_ap_fn, total, nsplit, reads):
            step = total // nsplit
            for s_ in range(nsplit):
                kb.dma("sp", dst_ap_fn(s_ * step, (s_ + 1) * step), src_ap_fn(s_ * step, (s_ + 1) * step), reads, [dst])

        sc_m = 96 ** -0.5
        Vm_v = V.ap.rearrange("p t e -> p (t e)")[:, 0:NKT * 66].rearrange("p (t e) -> p t e", e=66)
        for h in range(8):
            load(kT, lambda a, b: kT.ap[0:96, a:b], scr["kTm"], lambda a, b: scr["kTm"].ap[h, :, a:b], NK, 4, [scr["kTm"]])
            load(V, lambda a, b: Vm_v[:, a:b, :], scr["Vm"], lambda a, b: scr["Vm"].ap[h, :, a:b, :], NKT, 2, [scr["Vm"]])
            kb.dma("sp", qT.ap[0:96, :], scr["qTm"].ap[h], [scr["qTm"]], [qT])
            for qb in range(8):
                ob = ps[4 + oi % 2]
                oi += 1
                qap = qT.ap[0:96, qb * 512:(qb + 1) * 512]

                def s_step(kp):
                    pb_ = 2 * (kp % 2)
                    X, Y = ps[pb_], ps[pb_ + 1]
                    kb.mm(X, X.ap, kT, kT.ap[0:96, (2 * kp) * 128:(2 * kp + 1) * 128], qT, qap)
                    kb.mm(Y, Y.ap, kT, kT.ap[0:96, (2 * kp + 1) * 128:(2 * kp + 2) * 128], qT, qap)
                    return pb_, X, Y
                nxt = s_step(0)
                for kp in range(NKT // 2):
                    pb_, X, Y = nxt
                    if kp + 1 < NKT // 2:
                        nxt = s_step(kp + 1)
                    E_ = E[ei % 3]
                    ei += 1
                    kb.op("act", lambda e: e.activation(out=E_.ap.rearrange("p a b -> p (a b)"), in_=kb.psall[:, pb_ * 512:(pb_ + 2) * 512],
                                                        func=AF.Exp, scale=sc_m), [X, Y], [E_])
                    for a_ in range(2):
                        first = (kp == 0 and a_ == 0)
                        last = (kp == NKT // 2 - 1 and a_ == 1)
                        kb.mm(ob, ob.ap[0:65, :], V, Vm_v[:, 2 * kp + a_, 0:65], E_, E_.ap[:, a_, :], start=first, stop=last, acc=not first)
                kb.op("dve", lambda e: e.reciprocal(out=zr.ap[64:65, :], in_=ob.ap[64:65, :]), [ob], [zr])
                bc = ps[6]
                kb.mm(bc, bc.ap[0:64, :], kb.ones_f, kb.ones_f.ap[64:65, 0:64], zr, zr.ap[64:65, :])
                kb.cp("act", bcs, bcs.ap[0:64, :], bc, bc.ap[0:64, :])
                o_ = osb[oi % 2]
                kb.tt("dve", o_, o_.ap[0:64, :], ob, ob.ap[0:64, :], bcs, bcs.ap[0:64, :], ALU.mult)
                kb.dma("pool", scr["oTs"].ap[h, 0:64, qb * 512:(qb + 1) * 512], o_.ap[0:64, :], [o_], [scr["oTs"]])
        for h in range(4):
            load(kT, lambda a, b: kT.ap[:, a:b], scr["kTd"], lambda a, b: scr["kTd"].ap[h, :, a:b], NK, 4, [scr["kTd"]])
            load(V, lambda a, b: V.ap[:, a:b, :], scr["Vd"], lambda a, b: scr["Vd"].ap[h, :, a:b, :], NKT, 2, [scr["Vd"]])
            kb.dma("sp", qT.ap, scr["qTd"].ap[h], [scr["qTd"]], [qT])
            for qb in range(8):
                o0, o1, Z0, Z1 = ps[4], ps[5], ps[6], ps[7]
                qsl = slice(qb * 512, (qb + 1) * 512)

                def s_step(kt):
                    pb_ = 2 * (kt % 2)
                    X, Y = ps[pb_], ps[pb_ + 1]
                    ksl = slice(kt * 128, (kt + 1) * 128)
                    kb.mm(X, X.ap, kT, kT.ap[0:64, ksl], qT, qT.ap[0:64, qsl])
                    kb.mm(Y, Y.ap, kT, kT.ap[64:128, ksl], qT, qT.ap[64:128, qsl])
                    return pb_, X, Y
                nxt = s_step(0)
                for kt in range(NKT):
                    pb_, X, Y = nxt
                    if kt + 1 < NKT:
                        nxt = s_step(kt + 1)
                    E_ = E[ei % 3]
                    ei += 1
                    kb.op("act", lambda e: e.activation(out=E_.ap.rearrange("p a b -> p (a b)"), in_=kb.psall[:, pb_ * 512:(pb_ + 2) * 512],
                                                        func=AF.Exp, scale=0.125), [X, Y], [E_])
                    first = (kt == 0)
                    last = (kt == NKT - 1)
                    for a_, (o_b, z_b) in enumerate(((o0, Z0), (o1, Z1))):
                        kb.mm(o_b, o_b.ap, V, V.ap[:, kt, :], E_, E_.ap[:, a_, :], start=first, stop=last, acc=not first)
                        kb.mm(z_b, z_b.ap, kb.ones_b, kb.ones_b.ap, E_, E_.ap[:, a_, :], start=first, stop=last, acc=not first)
                kb.op("dve", lambda e: e.reciprocal(out=zr.ap, in_=Z0.ap), [Z0], [zr])
                kb.tt("dve", ta, ta.ap, o0, o0.ap, zr, zr.ap, ALU.mult)
                kb.op("dve", lambda e: e.reciprocal(out=zr.ap, in_=Z1.ap), [Z1], [zr])
                kb.tt("dve", tb, tb.ap, o1, o1.ap, zr, zr.ap, ALU.mult)
                kb.stt("dve", ta, ta.ap, tb, tb.ap, ssum.ap[:, 2:3], ta, ta.ap, ALU.mult, ALU.add, extra=[ssum])
                kb.tt("pool", tb, tb.ap, ta, ta.ap, ta, ta.ap, ALU.mult)
                sb_ = ps[0]
                kb.mm(sb_, sb_.ap, kb.ones_f, kb.ones_f.ap, tb, tb.ap)
                kb.act(bcs, bcs.ap, sb_, sb_.ap, AF.Ln, scale=1.0 / 128, bias=eps6.ap[:, 0:1], extra=[eps6])
                kb.act(bcs, bcs.ap, bcs, bcs.ap, AF.Exp, scale=-0.5)
                o_ = osb[oi % 2]
                oi += 1
                kb.stt("dve", o_, o_.ap, ta, ta.ap, subl.ap[:, 0:1], bcs, bcs.ap, ALU.mult, ALU.mult, extra=[subl])
                kb.dma("pool", scr["oTs"].ap[8 + h, :, qb * 512:(qb + 1) * 512], o_.ap, [o_], [scr["oTs"]])
        kb.P.barrier()


def phase_l1_out(kb, d, modT, lng, lnb, scr):
    with ExitStack() as st:
        kb.stack = st
        wC = kb.sb([64, 8, 1024], BF16)
        wD = kb.sb([128, 4, 1024], BF16)
        kb.dma("pool", wC.ap, d["cd_w_out"][0:512, :].rearrange("(h p) c -> p h c", p=64), [], [wC])
        kb.dma("pool", wD.ap, d["cd_w_out"][512:1024, :].rearrange("(h p) c -> p h c", p=128), [], [wD])
        oTb = [kb.sb([128, 12, 128], BF16) for _ in range(2)]
        lc = LNCtx(kb)
        xv = d["x1T"].rearrange("(kc p) t -> p kc t", p=128)
        ov = scr["x1b"].ap.rearrange("(kc p) t -> p kc t", p=128)
        ps = kb.ps
        for tile in range(32):
            oT = oTb[tile % 2]
            cs_ = slice(tile * 128, (tile + 1) * 128)
            kb.dma("sp", oT.ap[0:64, 0:8, :], scr["oTs"].ap[0:8, 0:64, cs_].rearrange("h p n -> p h n"), [scr["oTs"]], [oT])
            kb.dma("sp", oT.ap[:, 8:12, :], scr["oTs"].ap[8:12, :, cs_].rearrange("h p n -> p h n"), [scr["oTs"]], [oT])
            y0, y1 = ps[(2 * tile) % 4], ps[(2 * tile) % 4 + 1]
            for dc in range(8):
                pb_ = y0 if dc < 4 else y1
                c0 = (dc % 4) * 128
                for h in range(12):
                    if h < 8:
                        l_, lap, rap = wC, wC.ap[:, h, dc * 128:(dc + 1) * 128], oT.ap[0:64, h, :]
                    else:
                        l_, lap, rap = wD, wD.ap[:, h - 8, dc * 128:(dc + 1) * 128], oT.ap[:, h, :]
                    kb.mm(pb_, pb_.ap[:, c0:c0 + 128], l_, lap, oT, rap, start=(h == 0), stop=(h == 11), acc=not (dc % 4 == 0 and h == 0))

            def y_fn(dc, y0=y0, y1=y1):
                pb_ = y0 if dc < 4 else y1
                return pb_, pb_.ap[:, (dc % 4) * 128:(dc % 4 + 1) * 128]
            ln_epilogue(kb, lc, y_fn, xv[:, :, cs_], d.get("x_reads", []), modT.ap[:, 16:24, 0:1], modT, lng.ap[:, 0, :], lnb.ap[:, 0, :], (lng, lnb),
                        ov[:, :, cs_], scr["x1b"], ps[7])
        kb.P.barrier()


def build_launch2(debug=False, stop=None):
    kb = KB()
    d = _declare_common(kb)
    d.update(_declare_moe(kb))
    d.update({
        "x1T": kb.din("x1T", [1024, 16384]), "ctx1T": kb.din("ctx1T", [1024, 256]),
        "cd_w_in": kb.din("cd_w_in", [1024, 2208]), "cd_w_out": kb.din("cd_w_out", [1024, 1024]),
        "c_w_uq": kb.din("c_w_uq", [384, 768]), "c_w_ukv": kb.din("c_w_ukv", [256, 1024]),
        "c_q_normT": kb.din("c_q_normT", [128, 3]), "c_kv_normT": kb.din("c_kv_normT", [128, 2]),
        "d_lambda": kb.din("d_lambda", [4, 64]), "d_sublnT": kb.din("d_sublnT", [128, 1]),
        "rope64": kb.din("rope64", [128, 2, 16384]), "rope32": kb.din("rope32", [128, 2, 16384]),
    })
    out = T(kb.dout("outT", [1024, 4096]))
    scr = l1_scratch(kb)
    if debug:
        scr["x1b"] = T(kb.dout("x1b_dbg", [1024, 4096]))
    modT = phase_mod(kb, d["w_ada"], d["b_adaT"], d["cT"])
    lng, lnb = load_ln(kb, d["ln_gT"], d["ln_bT"])
    phase_l1_proj(kb, d, modT, scr)
    phase_l1_attn(kb, d, scr)
    phase_l1_out(kb, d, modT, lng, lnb, scr)
    if stop == "attn":
        kb.P.barrier()
        return kb
    ov = out.ap.rearrange("(kc p) t -> p kc t", p=128)
    passes = [list(range(8 * i, 8 * i + 8)) for i in range(4)]
    phase_moe(kb, d, modT, lng, lnb, scr["x1b"], passes, 99, lambda tile: (ov[:, :, tile * 128:(tile + 1) * 128], out))
    kb.P.barrier()
    return kb


def _own_first(r):
    idx = np.arange(S)
    own = idx[r * 4096:(r + 1) * 4096]
    rest = np.concatenate([idx[:r * 4096], idx[(r + 1) * 4096:]])
    return np.concatenate([own, rest])


def launch2_inputs(inp, x1, ctx1):
    maps = []
    moe = _moe_inputs(inp, 1)
    for core in range(8):
        b, r = core // 4, core % 4
        perm = _own_first(r)
        cos64, sin64 = _rope_np(perm, 64)
        cos32, sin32 = _rope_np(perm, 32)
        rope64 = np.stack([np.concatenate([cos64.T, cos64.T], 0), np.concatenate([sin64.T, sin64.T], 0)], axis=1)
        z = np.zeros((64, S), np.float32)
        z2 = np.zeros((32, S), np.float32)
        rope32 = np.stack([np.concatenate([z, cos32.T, z2], 0), np.concatenate([z, sin32.T, z2], 0)], axis=1)
        m = _common_inputs(inp, 1, b)
        m.update(moe)
        m.update({
            "x1T": np.ascontiguousarray(x1[b][perm].T), "ctx1T": np.ascontiguousarray(ctx1[b].T),
            "cd_w_in": np.ascontiguousarray(inp["cd_w_in"][0]), "cd_w_out": np.ascontiguousarray(inp["cd_w_out"][0]),
            "c_w_uq": np.ascontiguousarray(inp["c_w_uq"][0]), "c_w_ukv": np.ascontiguousarray(inp["c_w_ukv"][0]),
            "c_q_normT": _fm(inp["c_q_norm"][0], 3), "c_kv_normT": _fm(inp["c_kv_norm"][0], 2),
            "d_lambda": np.ascontiguousarray(inp["d_lambda"][0]), "d_sublnT": _fm(inp["d_subln"][0], 1),
            "rope64": np.ascontiguousarray(rope64), "rope32": np.ascontiguousarray(rope32),
        })
        maps.append(m)
    return maps


_CACHE = {}


def _kernel2(**inp):
    inp = {k: np.asarray(v) for k, v in inp.items()}
    if "k1" not in _CACHE:
        _CACHE["k1"] = build_launch1()
        _CACHE["k2"] = build_launch2()
    k1, k2 = _CACHE["k1"], _CACHE["k2"]
    res1 = run_bass_kernel_spmd(k1.nc, launch1_inputs(inp), core_ids=list(range(8)))
    x1 = np.empty((2, S, D), np.float32)
    ctx1 = np.empty((2, L, D), np.float32)
    for core in range(8):
        b, r = core // 4, core % 4
        o = res1.results[core]["x2T"]
        x1[b, r * 4096:(r + 1) * 4096] = o[:, 0:4096].T
        if r == 0:
            ctx1[b] = o[:, 4096:4352].T
    res2 = run_bass_kernel_spmd(k2.nc, launch2_inputs(inp, x1, ctx1), core_ids=list(range(8)))
    out = np.empty((2, S, D), np.float32)
    for core in range(8):
        b, r = core // 4, core % 4
        out[b, r * 4096:(r + 1) * 4096] = res2.results[core]["outT"].T
    return out


def build_fused():
    kb = KB()
    c0 = _declare_common(kb, "0")
    c1 = _declare_common(kb, "1")
    m0 = _declare_moe(kb, "0")
    m1 = _declare_moe(kb, "1")
    xT = kb.din("xT", [4, 1024, NEXT])
    biasB = kb.din("biasB", [4, 128, 5, 5, 8, 128])
    maskA = kb.din("maskA", [4, 128, 4, 128])
    ropeT = kb.din("ropeT", [4, 128, 2, NEXT])
    l0 = {"ctxT": kb.din("ctxT", [1024, 256]), "ab_w_in": kb.din("ab_w_in", [1024, 2304]),
          "ab_w_out": kb.din("ab_w_out", [1024, 1024]), "a_sink": kb.din("a_sink", [1, 8])}
    l1 = {
        "cd_w_in": kb.din("cd_w_in", [1024, 2208]), "cd_w_out": kb.din("cd_w_out", [1024, 1024]),
        "c_w_uq": kb.din("c_w_uq", [384, 768]), "c_w_ukv": kb.din("c_w_ukv", [256, 1024]),
        "c_q_normT": kb.din("c_q_normT", [128, 3]), "c_kv_normT": kb.din("c_kv_normT", [128, 2]),
        "d_lambda": kb.din("d_lambda", [4, 64]), "d_sublnT": kb.din("d_sublnT", [128, 1]),
        "rope64": kb.din("rope64", [128, 2, 16384]), "rope32": kb.din("rope32", [128, 2, 16384]),
    }
    out = T(kb.dout("outT", [1024, 4096]))
    x1T = kb.dscr("x1T_s", [1024, 4352], F32)
    x2T = kb.dscr("x2T_s", [1024, S + L], F32)
    x2v = x2T.ap.rearrange("(kc p) t -> p kc t", p=128)
    mod0 = phase_mod(kb, c0["w_ada"], c0["b_adaT"], c0["cT"])
    lng0, lnb0 = load_ln(kb, c0["ln_gT"], c0["ln_bT"])
    for seg in range(4):
        dseg = dict(l0)
        dseg.update({"xT": xT[seg], "biasB": biasB[seg], "maskA": maskA[seg], "ropeT": ropeT[seg]})
        phase_l0_attn(kb, dseg, mod0, lng0, lnb0, x1T, with_ctx=(seg == 0))
        ntile = 34 if seg == 0 else 32
        passes = [list(range(0, 12)), list(range(12, 24)), list(range(24, 34))] if seg == 0 else \
            [list(range(0, 12)), list(range(12, 24)), list(range(24, 32))]

        def out_fn(tile, seg=seg):
            if tile < 32:
                c = seg * 4096 + tile * 128
            else:
                c = S + (tile - 32) * 128
            return x2v[:, :, c:c + 128], x2T
        phase_moe(kb, m0, mod0, lng0, lnb0, x1T, passes, 32, out_fn)
    mod1 = phase_mod(kb, c1["w_ada"], c1["b_adaT"], c1["cT"])
    lng1, lnb1 = load_ln(kb, c1["ln_gT"], c1["ln_bT"])
    scr = l1_scratch(kb)
    l1["x1T"] = x2T.ap[:, 0:S]
    l1["ctx1T"] = x2T.ap[:, S:S + L]
    l1["x_reads"] = [x2T]
    phase_l1_proj(kb, l1, mod1, scr)
    phase_l1_attn(kb, l1, scr)
    phase_l1_out(kb, l1, mod1, lng1, lnb1, scr)
    ov = out.ap.rearrange("(kc p) t -> p kc t", p=128)
    passes = [list(range(0, 12)), list(range(12, 24)), list(range(24, 32))]
    phase_moe(kb, m1, mod1, lng1, lnb1, scr["x1b"], passes, 99, lambda tile: (ov[:, :, tile * 128:(tile + 1) * 128], out))
    kb.P.barrier()
    return kb


def fused_inputs(inp):
    maps = []
    rpb = np.asarray(inp["b_rpb"][0], np.float32)
    tabs = {r: _bias_tables(rpb, r) for r in range(4)}
    masks = {r: _mask_a(r) for r in range(4)}
    moe0 = {k + "0": v for k, v in _moe_inputs(inp, 0).items()}
    moe1 = {k + "1": v for k, v in _moe_inputs(inp, 1).items()}
    ropes0 = {}
    xts = {}
    for r in range(4):
        et = _ext_tokens(r)
        cos, sin = _rope_np(et, 64)
        ropes0[r] = np.stack([np.concatenate([cos.T, cos.T], 0), np.concatenate([sin.T, sin.T], 0)], axis=1)
        for b in range(2):
            xts[(b, r)] = np.ascontiguousarray(inp["x"][b][et].T)
    for core in range(8):
        b, r = core // 4, core % 4
        order = [r] + [q for q in range(4) if q != r]
        perm = _own_first(r)
        cos64, sin64 = _rope_np(perm, 64)
        cos32, sin32 = _rope_np(perm, 32)
        rope64 = np.stack([np.concatenate([cos64.T, cos64.T], 0), np.concatenate([sin64.T, sin64.T], 0)], axis=1)
        z = np.zeros((64, S), np.float32)
        z2 = np.zeros((32, S), np.float32)
        rope32 = np.stack([np.concatenate([z, cos32.T, z2], 0), np.concatenate([z, sin32.T, z2], 0)], axis=1)
        m = {k + "0": v for k, v in _common_inputs(inp, 0, b).items()}
        m.update({k + "1": v for k, v in _common_inputs(inp, 1, b).items()})
        m.update(moe0)
        m.update(moe1)
        m.update({
            "xT": np.stack([xts[(b, q)] for q in order], 0),
            "biasB": np.stack([tabs[q] for q in order], 0),
            "maskA": np.stack([masks[q] for q in order], 0),
            "ropeT": np.stack([ropes0[q] for q in order], 0),
            "ctxT": np.ascontiguousarray(inp["ctx"][b].T),
            "ab_w_in": np.ascontiguousarray(inp["ab_w_in"][0]), "ab_w_out": np.ascontiguousarray(inp["ab_w_out"][0]),
            "a_sink": np.ascontiguousarray(inp["a_sink"][0].reshape(1, 8)),
            "cd_w_in": np.ascontiguousarray(inp["cd_w_in"][0]), "cd_w_out": np.ascontiguousarray(inp["cd_w_out"][0]),
            "c_w_uq": np.ascontiguousarray(inp["c_w_uq"][0]), "c_w_ukv": np.ascontiguousarray(inp["c_w_ukv"][0]),
            "c_q_normT": _fm(inp["c_q_norm"][0], 3), "c_kv_normT": _fm(inp["c_kv_norm"][0], 2),
            "d_lambda": np.ascontiguousarray(inp["d_lambda"][0]), "d_sublnT": _fm(inp["d_subln"][0], 1),
            "rope64": np.ascontiguousarray(rope64), "rope32": np.ascontiguousarray(rope32),
        })
        maps.append(m)
    return maps


def kernel_unfused(**inp):
    return _kernel2(**inp)


def kernel(**inp):
    inp = {k: np.asarray(v) for k, v in inp.items()}
    if "kf" not in _CACHE:
        _CACHE["kf"] = build_fused()
    kf = _CACHE["kf"]
    res = run_bass_kernel_spmd(kf.nc, fused_inputs(inp), core_ids=list(range(8)))
    out = np.empty((2, S, D), np.float32)
    for core in range(8):
        b, r = core // 4, core % 4
        out[b, r * 4096:(r + 1) * 4096] = res.results[core]["outT"].T
    return out
```

```python
import math
from contextlib import ExitStack
import numpy as np
import concourse.bass as bass
import concourse.mybir as mybir
from concourse.bass_utils import run_bass_kernel_spmd

F32 = mybir.dt.float32
BF16 = mybir.dt.bfloat16
ALU = mybir.AluOpType
AF = mybir.ActivationFunctionType
AX = mybir.AxisListType

D = 1024
S = 16384
L = 256
DEPTH = 2
ALPHA = (2 * DEPTH) ** 0.25
LN_EPS = 1e-5 / (ALPHA * ALPHA)
NEG = -30000.0
LAM_INIT = 0.8 - 0.6 * math.exp(-0.3 * 1)
NEXT = 4608


class H:
    __slots__ = ("w", "r")

    def __init__(self):
        self.w = None
        self.r = []


class T:
    def __init__(self, ap):
        self.ap = ap
        self.h = H()


class Prog:
    NDMA = 8

    def __init__(self, nc):
        self.nc = nc
        self.eng = {"pe": nc.tensor, "act": nc.scalar, "dve": nc.vector,
                    "pool": nc.gpsimd, "sp": nc.sync}
        self.sem = {}
        self.cnt = {}
        for k in ("pe", "act", "dve", "pool"):
            self.sem[k] = nc.alloc_semaphore("s_" + k)
            self.cnt[k] = 0
        self.seen = {k: {} for k in self.eng}
        self.dma_i = {}
        self.dma_sems = {}
        for q in ("sp", "pool", "act"):
            self.dma_i[q] = 0
            self.dma_sems[q] = []
            for i in range(self.NDMA):
                key = ("dma", q, i)
                self.sem[key] = nc.alloc_semaphore("d_%s_%d" % (q, i))
                self.cnt[key] = 0
                self.dma_sems[q].append(key)
        self.n_ins = 0

    def _wait(self, e, key, val):
        if self.seen[e].get(key, 0) < val:
            self.eng[e].wait_ge(self.sem[key], val)
            self.seen[e][key] = val

    def _deps(self, e, reads, writes, skip_own_waw=False):
        deps = {}
        for h in reads:
            if h.w is not None:
                k, v = h.w
                if deps.get(k, 0) < v:
                    deps[k] = v
        for h in writes:
            if h.w is not None:
                k, v = h.w
                if not (skip_own_waw and k == e):
                    if deps.get(k, 0) < v:
                        deps[k] = v
            for (k, v) in h.r:
                if deps.get(k, 0) < v:
                    deps[k] = v
        for k, v in deps.items():
            self._wait(e, k, v)

    def _mark(self, tok, reads, writes):
        for h in reads:
            if len(h.r) > 16:
                d = {}
                for (k, v) in h.r:
                    if d.get(k, 0) < v:
                        d[k] = v
                h.r = list(d.items())
            h.r.append(tok)
        for h in writes:
            h.w = tok
            h.r = []

    def op(self, e, fn, reads=(), writes=(), inc=True, acc=False):
        self._deps(e, reads, writes, skip_own_waw=acc)
        ins = fn(self.eng[e])
        self.n_ins += 1
        if inc:
            self.cnt[e] += 1
            ins.then_inc(self.sem[e], 1)
            tok = (e, self.cnt[e])
        else:
            tok = (e, self.cnt[e] + 1)
        self._mark(tok, reads, writes)
        return tok

    def dma(self, q, out, in_, reads=(), writes=()):
        self._deps(q, reads, writes)
        i = self.dma_i[q]
        self.dma_i[q] += 1
        key = self.dma_sems[q][i % self.NDMA]
        if self.cnt[key] > 0:
            self._wait(q, key, self.cnt[key])
        self.cnt[key] += 16
        self.eng[q].dma_start(out=out, in_=in_).then_inc(self.sem[key], 16)
        self.n_ins += 1
        tok = (key, self.cnt[key])
        self._mark(tok, reads, writes)
        return tok

    def barrier(self):
        for e in self.eng:
            for k, v in self.cnt.items():
                if v > 0:
                    self._wait(e, k, v)


class KB:
    def __init__(self):
        self.nc = bass.Bass("TRN2", target_bir_lowering=False)
        self.P = Prog(self.nc)
        nc = self.nc
        psall = nc.alloc_psum_tensor("psall", [128, 4096], F32).ap()
        self.psall = psall
        self.ps = [T(psall[:, i * 512:(i + 1) * 512]) for i in range(8)]
        self.stack = None
        self.nm = 0
        self.ones_f = self.gsb([128, 128], F32)
        self.ones_b = self.gsb([128, 128], BF16)
        self.op("dve", lambda e: e.memset(self.ones_f.ap, 1.0), [], [self.ones_f])
        self.op("dve", lambda e: e.memset(self.ones_b.ap, 1.0), [], [self.ones_b])

    def name(self, p="t"):
        self.nm += 1
        return "%s%d" % (p, self.nm)

    def gsb(self, shape, dt):
        return T(self.nc.alloc_sbuf_tensor(self.name("g"), list(shape), dt).ap())

    def sb(self, shape, dt):
        t = self.stack.enter_context(self.nc.sbuf_tensor(self.name("s"), list(shape), dt))
        return T(t.ap())

    def din(self, name, shape, dt=F32):
        return self.nc.dram_tensor(name, list(shape), dt, kind="ExternalInput").ap()

    def dout(self, name, shape, dt=F32):
        return self.nc.dram_tensor(name, list(shape), dt, kind="ExternalOutput").ap()

    def dscr(self, name, shape, dt):
        return T(self.nc.dram_tensor(name, list(shape), dt, kind="Internal").ap())

    def op(self, e, fn, reads, writes, inc=True, acc=False):
        return self.P.op(e, fn, [t.h for t in reads], [t.h for t in writes], inc=inc, acc=acc)

    def dma(self, q, out, in_, reads, writes):
        return self.P.dma(q, out, in_, [t.h for t in reads], [t.h for t in writes])

    def mm(self, o, o_ap, l, l_ap, r, r_ap, start=True, stop=True, acc=None, skip=False, inc=None):
        if acc is None:
            acc = not start
        if inc is None:
            inc = stop
        self.op("pe", lambda e: e.matmul(o_ap, lhsT=l_ap, rhs=r_ap, start=start, stop=stop, skip_group_check=skip),
                [l, r], [o], inc=inc, acc=acc)

    def tt(self, eng, o, o_ap, a, a_ap, b, b_ap, op):
        self.op(eng, lambda e: e.tensor_tensor(out=o_ap, in0=a_ap, in1=b_ap, op=op), [a, b], [o])

    def stt(self, eng, o, o_ap, a, a_ap, sc, b, b_ap, op0, op1, extra=()):
        self.op(eng, lambda e: e.scalar_tensor_tensor(out=o_ap, in0=a_ap, scalar=sc, in1=b_ap, op0=op0, op1=op1),
                [a, b] + list(extra), [o])

    def ts(self, eng, o, o_ap, a, a_ap, s1, s2, op0, op1=None, extra=()):
        if op1 is None:
            self.op(eng, lambda e: e.tensor_scalar(out=o_ap, in0=a_ap, scalar1=s1, scalar2=None, op0=op0),
                    [a] + list(extra), [o])
        else:
            self.op(eng, lambda e: e.tensor_scalar(out=o_ap, in0=a_ap, scalar1=s1, scalar2=s2, op0=op0, op1=op1),
                    [a] + list(extra), [o])

    def act(self, o, o_ap, a, a_ap, func, scale=1.0, bias=None, extra=()):
        if bias is None:
            self.op("act", lambda e: e.activation(out=o_ap, in_=a_ap, func=func, scale=scale),
                    [a] + list(extra), [o])
        else:
            self.op("act", lambda e: e.activation(out=o_ap, in_=a_ap, func=func, scale=scale, bias=bias),
                    [a] + list(extra), [o])

    def cp(self, eng, o, o_ap, a, a_ap):
        if eng == "act":
            self.op("act", lambda e: e.copy(out=o_ap, in_=a_ap), [a], [o])
        else:
            self.op(eng, lambda e: e.tensor_copy(out=o_ap, in_=a_ap), [a], [o])

    def memset(self, eng, o, o_ap, v):
        self.op(eng, lambda e: e.memset(o_ap, v), [], [o])

    def rstd(self, o, o_ap, a, a_ap, scale, eps, epsT):
        self.act(o, o_ap, a, a_ap, AF.Ln, scale=scale, bias=epsT.ap[0:o_ap.shape[0] + o_ap.base_partition(), 0:1][o_ap.base_partition():, :], extra=[epsT])
        self.act(o, o_ap, o, o_ap, AF.Exp, scale=-0.5)


def phase_mod(kb, w_ada, b_adaT, cT):
    modT = kb.gsb([128, 48, 2], F32)
    with ExitStack() as st:
        kb.stack = st
        cs = kb.sb([128, 8, 2], F32)
        kb.dma("sp", cs.ap, cT, [], [cs])
        kb.act(cs, cs.ap, cs, cs.ap, AF.Silu)
        bT = kb.sb([128, 48], F32)
        kb.dma("sp", bT.ap, b_adaT, [], [bT])
        wv = w_ada.rearrange("(kc p) f -> p kc f", p=128)
        bufs = [kb.sb([128, 8, 512], F32) for _ in range(2)]
        ps = kb.ps[0]
        for pc in range(12):
            wb = bufs[pc % 2]
            kb.dma("sp", wb.ap, wv[:, :, pc * 512:(pc + 1) * 512], [], [wb])
            for fc in range(4):
                ch = pc * 4 + fc
                for kc in range(8):
                    kb.mm(ps, ps.ap[:, 2 * ch:2 * ch + 2], wb, wb.ap[:, kc, fc * 128:(fc + 1) * 128],
                          cs, cs.ap[:, kc, :], start=(kc == 0), stop=(kc == 7), acc=(not (pc == 0 and fc == 0 and kc == 0)))
        pv = ps.ap[:, 0:96].rearrange("p (c j) -> p c j", j=2)
        kb.tt("dve", modT, modT.ap, ps, pv, bT, bT.ap.unsqueeze(2).to_broadcast([128, 48, 2]), ALU.add)
        for w in (1, 4):
            kb.ts("dve", modT, modT.ap[:, w * 8:(w + 1) * 8, :], modT, modT.ap[:, w * 8:(w + 1) * 8, :], 1.0, None, ALU.add)
        for w in (2, 5):
            kb.ts("dve", modT, modT.ap[:, w * 8:(w + 1) * 8, :], modT, modT.ap[:, w * 8:(w + 1) * 8, :], 1.0 / ALPHA, None, ALU.mult)
        kb.P.barrier()
    return modT


def load_ln(kb, ln_gT, ln_bT):
    g = kb.gsb([128, 2, 8], F32)
    b = kb.gsb([128, 2, 8], F32)
    kb.dma("sp", g.ap, ln_gT, [], [g])
    kb.dma("sp", b.ap, ln_bT, [], [b])
    return g, b


class LNCtx:
    def __init__(self, kb):
        self.z = kb.sb([128, 8, 128], F32)
        self.zsq = kb.sb([128, 8, 128], F32)
        self.xo = [kb.sb([128, 8, 128], F32) for _ in range(2)]
        self.m = kb.sb([128, 128], F32)
        self.msq = kb.sb([128, 128], F32)
        self.var = kb.sb([128, 128], F32)
        self.eps = kb.sb([128, 1], F32)
        kb.memset("dve", self.eps, self.eps.ap, LN_EPS)
        self.i = 0


def ln_epilogue(kb, lc, y_fn, xres_ap, xres_reads, gmod_ap, modT, lng_ap, lnb_ap, lnp, out_ap, out_T, stat_ps):
    z, zsq = lc.z, lc.zsq
    kb.dma("sp", z.ap, xres_ap, xres_reads, [z])
    for dc in range(8):
        yt, yap = y_fn(dc)
        kb.stt("dve", z, z.ap[:, dc, :], yt, yap, gmod_ap[:, dc, :], z, z.ap[:, dc, :], ALU.mult, ALU.add, extra=[modT])
    kb.tt("pool", zsq, zsq.ap, z, z.ap, z, z.ap, ALU.mult)
    for dc in range(8):
        kb.mm(stat_ps, stat_ps.ap[:, 0:128], kb.ones_f, kb.ones_f.ap, z, z.ap[:, dc, :], start=(dc == 0), stop=(dc == 7))
    for dc in range(8):
        kb.mm(stat_ps, stat_ps.ap[:, 128:256], kb.ones_f, kb.ones_f.ap, zsq, zsq.ap[:, dc, :], start=(dc == 0), stop=(dc == 7), acc=True)
    m, msq, var = lc.m, lc.msq, lc.var
    kb.act(m, m.ap, stat_ps, stat_ps.ap[:, 0:128], AF.Copy, scale=1.0 / D)
    kb.tt("dve", msq, msq.ap, m, m.ap, m, m.ap, ALU.mult)
    kb.stt("dve", var, var.ap, stat_ps, stat_ps.ap[:, 128:256], 1.0 / D, msq, msq.ap, ALU.mult, ALU.subtract)
    kb.act(var, var.ap, var, var.ap, AF.Ln, scale=1.0, bias=lc.eps.ap[:, 0:1], extra=[lc.eps])
    kb.act(var, var.ap, var, var.ap, AF.Exp, scale=-0.5)
    xo = lc.xo[lc.i % 2]
    lc.i += 1
    kb.tt("dve", z, z.ap, z, z.ap, m, m.ap.unsqueeze(1).to_broadcast([128, 8, 128]), ALU.subtract)
    kb.tt("pool", z, z.ap, z, z.ap, var, var.ap.unsqueeze(1).to_broadcast([128, 8, 128]), ALU.mult)
    kb.tt("dve", z, z.ap, z, z.ap, lnp[0], lng_ap.unsqueeze(2).to_broadcast([128, 8, 128]), ALU.mult)
    kb.tt("pool", xo, xo.ap, z, z.ap, lnp[1], lnb_ap.unsqueeze(2).to_broadcast([128, 8, 128]), ALU.add)
    kb.dma("pool", out_ap, xo.ap, [xo], [out_T])


class AttCtx:
    def __init__(self, kb, s_banks, o_banks, bc_bank):
        self.sb_ = [kb.sb([128, 512], F32) for _ in range(2)]
        self.E = [kb.sb([128, 512], BF16) for _ in range(3)]
        self.zr = kb.sb([128, 512], F32)
        self.bcs = kb.sb([64, 512], F32)
        self.s_banks = s_banks
        self.o_banks = o_banks
        self.bc = bc_bank
        self.si = 0
        self.ei = 0
        self.oi = 0
        self.bi = 0


def attn_unit(kb, ac, tiles, scale, out_T, out_ap, ncols=512, sink=None):
    ob = ac.o_banks[ac.oi % len(ac.o_banks)]
    ac.oi += 1
    nt = len(tiles)
    for ti, tl in enumerate(tiles):
        sbk = ac.s_banks[ac.si % len(ac.s_banks)]
        ac.si += 1
        first = True
        for (kT, kap, qT, qap, c0, n) in tl["qk"]:
            kb.mm(sbk, sbk.ap[:, c0:c0 + n], kT, kap, qT, qap, start=True, stop=True, acc=(not first))
            first = False
        E = ac.E[ac.ei % 3]
        ac.ei += 1
        if tl["bias"] is not None:
            bT, bap = tl["bias"]
            sbuf = ac.sb_[ac.bi % 2]
            ac.bi += 1
            kb.stt("dve", sbuf, sbuf.ap[:, 0:ncols], sbk, sbk.ap[:, 0:ncols], scale, bT, bap, ALU.mult, ALU.add)
            kb.act(E, E.ap[:, 0:ncols], sbuf, sbuf.ap[:, 0:ncols], AF.Exp)
        else:
            kb.act(E, E.ap[:, 0:ncols], sbk, sbk.ap[:, 0:ncols], AF.Exp, scale=scale)
        last = (ti == nt - 1) and sink is None
        for pi, (vT, vap, c0, n) in enumerate(tl["pv"]):
            kb.mm(ob, ob.ap[0:65, c0:c0 + n], vT, vap, E, E.ap[:, c0:c0 + n], start=(ti == 0 and pi == 0), stop=last,
                  acc=not (ti == 0 and pi == 0), skip=True, inc=last)
    if sink is not None:
        e64, srow_T, srow_ap = sink
        kb.mm(ob, ob.ap[0:65, 0:ncols], e64, e64.ap, srow_T, srow_ap, start=False, stop=True, acc=True)
    zr = ac.zr
    kb.op("dve", lambda e: e.reciprocal(out=zr.ap[64:65, 0:ncols], in_=ob.ap[64:65, 0:ncols]), [ob], [zr])
    bc = ac.bc
    kb.mm(bc, bc.ap[0:64, 0:ncols], kb.ones_f, kb.ones_f.ap[64:65, 0:64], zr, zr.ap[64:65, 0:ncols])
    kb.cp("act", ac.bcs, ac.bcs.ap[:, 0:ncols], bc, bc.ap[0:64, 0:ncols])
    kb.tt("dve", out_T, out_ap, ob, ob.ap[0:64, 0:ncols], ac.bcs, ac.bcs.ap[:, 0:ncols], ALU.mult)


def phase_l0_attn(kb, d, modT, lng, lnb, x1T, with_ctx=True):
    P = kb.P
    with ExitStack() as st:
        kb.stack = st
        wA = kb.sb([128, 8, 640], BF16)
        wArot = kb.sb([128, 8, 640], BF16)
        wR = kb.sb([128, 8, 1664], BF16)
        wout = kb.sb([64, 16, 1024], BF16)
        win = d["ab_w_in"].rearrange("(kc p) c -> p kc c", p=128)
        for slot, hd in enumerate([0, 4, 1, 5, 2, 6, 3, 7]):
            kb.dma("pool", wA.ap[:, :, slot * 64:(slot + 1) * 64], win[:, :, hd * 64:(hd + 1) * 64], [], [wA])
        kb.dma("pool", wA.ap[:, :, 512:640], win[:, :, 512:640], [], [wA])
        kb.dma("pool", wR.ap[:, :, 0:1024], win[:, :, 768:1792], [], [wR])
        kb.dma("pool", wR.ap[:, :, 1024:1152], win[:, :, 640:768], [], [wR])
        kb.dma("pool", wR.ap[:, :, 1152:1664], win[:, :, 1792:2304], [], [wR])
        kb.dma("pool", wout.ap, d["ab_w_out"].rearrange("(h p) c -> p h c", p=64), [], [wout])
        src = wA.ap.rearrange("p k (g two s) -> p (k g) two s", two=2, s=16)
        dst = wArot.ap.rearrange("p k (g two s) -> p (k g) two s", two=2, s=16)
        kb.ts("dve", wArot, dst[:, :, 0, :], wA, src[:, :, 1, :], -1.0, None, ALU.mult)
        kb.cp("dve", wArot, dst[:, :, 1, :], wA, src[:, :, 0, :])
        biasG = kb.sb([128, 5, 8, 128], BF16)
        biasE = kb.sb([128, 5, 8, 128], BF16)
        kb.dma("pool", biasG.ap, d["biasB"][:, 2], [], [biasG])
        maskA = kb.sb([128, 4, 128], BF16)
        kb.dma("pool", maskA.ap, d["maskA"], [], [maskA])
        sinkrow = kb.sb([1, 8, 128], F32)
        sk = kb.sb([1, 8], F32)
        kb.dma("sp", sk.ap, d["a_sink"], [], [sk])
        kb.act(sk, sk.ap, sk, sk.ap, AF.Exp)
        kb.cp("dve", sinkrow, sinkrow.ap, sk, sk.ap.unsqueeze(2).to_broadcast([1, 8, 128]))
        e64 = kb.sb([1, 65], F32)
        kb.memset("dve", e64, e64.ap, 0.0)
        kb.memset("dve", e64, e64.ap[:, 64:65], 1.0)
        def mkslot():
            s = dict(q=kb.sb([128, 8, 128], BF16), k=kb.sb([128, 5, 128], BF16), v=kb.sb([128, 10, 66], BF16))
            kb.memset("pool", s["v"], s["v"].ap[:, :, 64:65], 1.0)
            return s
        ring = [mkslot() for _ in range(6)]
        cslots = [mkslot() for _ in range(2)]
        xb = [kb.sb([128, 8, 128], F32) for _ in range(2)]
        hTs = [kb.sb([128, 8, 128], BF16) for _ in range(2)]
        tmpf = kb.sb([128, 8, 128], F32)
        ropeT = [kb.sb([128, 2, 128], F32) for _ in range(2)]
        r1 = kb.sb([128, 4, 128], F32)
        r2 = kb.sb([128, 4, 128], F32)
        oT = kb.sb([64, 16, 128], BF16)
        lc = LNCtx(kb)
        ac = AttCtx(kb, [kb.ps[3], kb.ps[4]], [kb.ps[5], kb.ps[6]], kb.ps[7])
        ps0, ps1, ps2 = kb.ps[0], kb.ps[1], kb.ps[2]
        xTv = d["xT"].rearrange("(kc p) t -> p kc t", p=128)
        cTv = d["ctxT"].rearrange("(kc p) t -> p kc t", p=128)
        x1v = x1T.ap.rearrange("(kc p) t -> p kc t", p=128)
        cnt = [0]
        import os
        LIM = int(os.environ.get("LIM", "99"))

        def project(src_ap, col, slot, rope_t):
            i = cnt[0]
            cnt[0] += 1
            xt = xb[i % 2]
            hT = hTs[i % 2]
            kb.dma("sp", xt.ap, src_ap, [], [xt])
            s_b = modT.ap[:, 8:16, col:col + 1].to_broadcast([128, 8, 128])
            sh_b = modT.ap[:, 0:8, col:col + 1].to_broadcast([128, 8, 128])
            kb.tt("dve", tmpf, tmpf.ap, xt, xt.ap, modT, s_b, ALU.mult)
            kb.tt("pool", hT, hT.ap, tmpf, tmpf.ap, modT, sh_b, ALU.add)
            rope = rope_t is not None
            if rope:
                rt = ropeT[i % 2]
                kb.dma("sp", rt.ap, d["ropeT"][:, :, rope_t * 128:(rope_t + 1) * 128], [], [rt])
            for c in range(4):
                for kc in range(8):
                    kb.mm(ps0, ps0.ap[:, c * 128:(c + 1) * 128], wA, wA.ap[:, kc, c * 128:(c + 1) * 128], hT, hT.ap[:, kc, :],
                          start=(kc == 0), stop=(kc == 7), acc=not (c == 0 and kc == 0))
            if rope:
                for c in range(4):
                    for kc in range(8):
                        kb.mm(ps1, ps1.ap[:, c * 128:(c + 1) * 128], wArot, wArot.ap[:, kc, c * 128:(c + 1) * 128], hT, hT.ap[:, kc, :],
                              start=(kc == 0), stop=(kc == 7), acc=not (c == 0 and kc == 0))
            yield
            for kc in range(8):
                kb.mm(ps2, ps2.ap[:, 0:128], wA, wA.ap[:, kc, 512:640], hT, hT.ap[:, kc, :], start=(kc == 0), stop=(kc == 7), acc=(kc != 0))
            if rope:
                for kc in range(8):
                    kb.mm(ps2, ps2.ap[:, 128:256], wArot, wArot.ap[:, kc, 512:640], hT, hT.ap[:, kc, :], start=(kc == 0), stop=(kc == 7), acc=True)
            for kc in range(8):
                kb.mm(ps2, ps2.ap[:, 256:384], hT, hT.ap[:, kc, :], wR, wR.ap[:, kc, 1024:1152], start=(kc == 0), stop=(kc == 7), acc=True)
            q, k, v = slot["q"], slot["k"], slot["v"]
            if rope:
                cosb = rt.ap[:, 0:1, :].to_broadcast([128, 4, 128])
                sinb = rt.ap[:, 1:2, :].to_broadcast([128, 4, 128])
                p0v = ps0.ap.rearrange("p (c t) -> p c t", t=128)
                p1v = ps1.ap.rearrange("p (c t) -> p c t", t=128)
                kb.tt("dve", r1, r1.ap, ps0, p0v, rt, cosb, ALU.mult)
                kb.tt("dve", r2, r2.ap, ps1, p1v, rt, sinb, ALU.mult)
                kb.tt("pool", q, q.ap[:, 0:4, :], r1, r1.ap, r2, r2.ap, ALU.add)
                kb.tt("dve", r1, r1.ap[:, 0, :], ps2, ps2.ap[:, 0:128], rt, rt.ap[:, 0, :], ALU.mult)
                kb.tt("dve", r2, r2.ap[:, 0, :], ps2, ps2.ap[:, 128:256], rt, rt.ap[:, 1, :], ALU.mult)
                kb.tt("pool", k, k.ap[:, 0, :], r1, r1.ap[:, 0, :], r2, r2.ap[:, 0, :], ALU.add)
            else:
                kb.cp("act", q, q.ap[:, 0:4, :], ps0, ps0.ap.rearrange("p (c t) -> p c t", t=128))
                kb.cp("act", k, k.ap[:, 0, :], ps2, ps2.ap[:, 0:128])
            kb.cp("act", v, v.ap[:, 0:2, 0:64], ps2, ps2.ap[:, 256:384].rearrange("p (h e) -> p h e", e=64))
            yield
            for c in range(4):
                for kc in range(8):
                    kb.mm(ps0, ps0.ap[:, c * 128:(c + 1) * 128], wR, wR.ap[:, kc, c * 128:(c + 1) * 128], hT, hT.ap[:, kc, :],
                          start=(kc == 0), stop=(kc == 7), acc=not (c == 0 and kc == 0))
            kb.cp("act", q, q.ap[:, 4:8, :], ps0, ps0.ap.rearrange("p (c t) -> p c t", t=128))
            yield
            for c in range(4):
                for kc in range(8):
                    kb.mm(ps1, ps1.ap[:, c * 128:(c + 1) * 128], wR, wR.ap[:, kc, 512 + c * 128:512 + (c + 1) * 128], hT, hT.ap[:, kc, :],
                          start=(kc == 0), stop=(kc == 7), acc=not (c == 0 and kc == 0))
            kb.cp("act", k, k.ap[:, 1:5, :], ps1, ps1.ap.rearrange("p (c t) -> p c t", t=128))
            yield
            for kc in range(8):
                kb.mm(ps2, ps2.ap, hT, hT.ap[:, kc, :], wR, wR.ap[:, kc, 1152:1664], start=(kc == 0), stop=(kc == 7), acc=(kc != 0))
            kb.cp("act", v, v.ap[:, 2:10, 0:64], ps2, ps2.ap.rearrange("p (h e) -> p h e", e=64))

        def attend(qslot, loc_tiles, bias_tab, mask_prev, mask_next, xres_ap, col, out_col):
            q = qslot["q"]
            for g in range(2):
                pb = 64 * g
                tiles = []
                for cs_ in cslots:
                    tiles.append(dict(qk=[(cs_["k"], cs_["k"].ap[pb:pb + 64, 0, :], q, q.ap[pb:pb + 64, 0:4, :], 0, 512)],
                                      bias=None, pv=[(cs_["v"], cs_["v"].ap[:, g, 0:65], 0, 512)]))
                if loc_tiles is not None:
                    for idx, mk in ((1, mask_prev), (2, None), (3, mask_next)):
                        sl = loc_tiles[idx]
                        bias = None
                        if mk is not None:
                            bias = (maskA, maskA.ap[:, mk:mk + 1, :].to_broadcast([128, 4, 128]))
                        tiles.append(dict(qk=[(sl["k"], sl["k"].ap[pb:pb + 64, 0, :], q, q.ap[pb:pb + 64, 0:4, :], 0, 512)],
                                          bias=bias, pv=[(sl["v"], sl["v"].ap[:, g, 0:65], 0, 512)]))
                for tl in tiles:
                    if tl["bias"] is not None:
                        tl["bias"] = (tl["bias"][0], tl["bias"][1])
                attn_unit_l0(kb, ac, tiles, 0.125, oT, oT.ap[:, 4 * g:4 * g + 4, :],
                             sink=(e64, sinkrow, sinkrow.ap[0:1, 4 * g:4 * g + 4, :]))
                yield
            if LIM == 4:
                return
            for u in range(2):
                tiles = []
                srcs = [(cs_, None) for cs_ in cslots]
                if loc_tiles is not None:
                    srcs += [(loc_tiles[t], t) for t in range(5)]
                for (sl, t) in srcs:
                    qk = []
                    pv = []
                    for hh in range(4):
                        h = 4 * u + hh
                        pb = 64 * (h % 2)
                        qk.append((sl["k"], sl["k"].ap[pb:pb + 64, 1 + h // 2, :], q, q.ap[pb:pb + 64, 4 + h // 2, :], hh * 128, 128))
                        pv.append((sl["v"], sl["v"].ap[:, 2 + h, 0:65], hh * 128, 128))
                    bias = None
                    if t is not None:
                        bias = (bias_tab, bias_tab.ap[:, t, 4 * u:4 * u + 4, :].rearrange("p (a two) q -> p two a q", two=2))
                    tiles.append(dict(qk=qk, bias=bias, pv=pv))
                attn_unit_b(kb, ac, tiles, 0.125, oT, oT.ap[:, 8 + 4 * u:8 + 4 * u + 4, :])
                yield
            if LIM == 5:
                return
            for dc in range(8):
                pb_ = ps0 if dc < 4 else ps1
                c0 = (dc % 4) * 128
                for h in range(16):
                    kb.mm(pb_, pb_.ap[:, c0:c0 + 128], wout, wout.ap[:, h, dc * 128:(dc + 1) * 128], oT, oT.ap[:, h, :],
                          start=(h == 0), stop=(h == 15), acc=not (dc % 4 == 0 and h == 0))

            if LIM == 6:
                return

            def y_fn(dc):
                pb_ = ps0 if dc < 4 else ps1
                return pb_, pb_.ap[:, (dc % 4) * 128:(dc % 4 + 1) * 128]
            ln_epilogue(kb, lc, y_fn, xres_ap, [], modT.ap[:, 16:24, col:col + 1], modT,
                        lng.ap[:, 0, :], lnb.ap[:, 0, :], (lng, lnb), x1v[:, :, out_col:out_col + 128], x1T, kb.ps[7])

        def attn_unit_l0(kb_, ac_, tiles, scale, out_T, out_ap3, sink):
            for tl in tiles:
                if tl["bias"] is not None:
                    tl["bias3"] = True
            attn_unit3(kb_, ac_, tiles, scale, out_T, out_ap3, sink)

        def run(gen):
            for _ in gen:
                pass

        def interleave(ga, gb):
            alive = [ga, gb]
            while alive:
                for g_ in list(alive):
                    try:
                        next(g_)
                    except StopIteration:
                        alive.remove(g_)

        for c in range(2):
            run(project(cTv[:, :, c * 128:(c + 1) * 128], 1, cslots[c], None))
        for Tt in range(5):
            run(project(xTv[:, :, Tt * 128:(Tt + 1) * 128], 0, ring[Tt % 6], Tt))
        for j in range(32):
            if j in (0, 1, 30, 31):
                var = {0: 0, 1: 1, 30: 3, 31: 4}[j]
                kb.dma("pool", biasE.ap, d["biasB"][:, var], [], [biasE])
                btab = biasE
            else:
                btab = biasG
            loc = [ring[(j + t) % 6] for t in range(5)]
            ga = attend(ring[(j + 2) % 6], loc, btab, 0 if j == 0 else 1, 3 if j == 31 else 2,
                        xTv[:, :, (j + 2) * 128:(j + 3) * 128], 0, j * 128)
            Tn = j + 5
            if Tn < 36:
                gp = project(xTv[:, :, Tn * 128:(Tn + 1) * 128], 0, ring[Tn % 6], Tn)
                interleave(ga, gp)
            else:
                run(ga)
        if with_ctx:
            for c in range(2):
                run(attend(cslots[c], None, None, None, None, cTv[:, :, c * 128:(c + 1) * 128], 1, 4096 + c * 128))
        kb.P.barrier()


def attn_unit3(kb, ac, tiles, scale, out_T, out_ap3, sink):
    ob = ac.o_banks[ac.oi % len(ac.o_banks)]
    ac.oi += 1
    nt = len(tiles)
    for ti, tl in enumerate(tiles):
        sbk = ac.s_banks[ac.si % len(ac.s_banks)]
        ac.si += 1
        first = True
        for (kT, kap, qT, qap, c0, n) in tl["qk"]:
            kb.mm(sbk, sbk.ap[:, c0:c0 + n], kT, kap, qT, qap, start=True, stop=True, acc=(not first))
            first = False
        E = ac.E[ac.ei % 3]
        ac.ei += 1
        if tl["bias"] is not None:
            bT, bap = tl["bias"]
            sbuf = ac.sb_[ac.bi % 2]
            ac.bi += 1
            kb.stt("dve", sbuf, sbuf.ap.rearrange("p (c t) -> p c t", t=128), sbk, sbk.ap.rearrange("p (c t) -> p c t", t=128),
                   scale, bT, bap, ALU.mult, ALU.add)
            kb.act(E, E.ap, sbuf, sbuf.ap, AF.Exp)
        else:
            kb.act(E, E.ap, sbk, sbk.ap, AF.Exp, scale=scale)
        last = (ti == nt - 1) and sink is None
        for pi, (vT, vap, c0, n) in enumerate(tl["pv"]):
            kb.mm(ob, ob.ap[0:65, c0:c0 + n], vT, vap, E, E.ap[:, c0:c0 + n], start=(ti == 0 and pi == 0), stop=last,
                  acc=not (ti == 0 and pi == 0), skip=True, inc=last)
    if sink is not None:
        e64, srow_T, srow_ap = sink
        kb.mm(ob, ob.ap[0:65, :], e64, e64.ap, srow_T, srow_ap, start=False, stop=True, acc=True, skip=True)
    zr = ac.zr
    kb.op("dve", lambda e: e.reciprocal(out=zr.ap[64:65, :], in_=ob.ap[64:65, :]), [ob], [zr])
    bc = ac.bc
    kb.mm(bc, bc.ap[0:64, :], kb.ones_f, kb.ones_f.ap[64:65, 0:64], zr, zr.ap[64:65, :])
    kb.cp("act", ac.bcs, ac.bcs.ap, bc, bc.ap[0:64, :])
    kb.tt("dve", out_T, out_ap3, ob, ob.ap[0:64, :].rearrange("p (c t) -> p c t", t=128),
          ac.bcs, ac.bcs.ap.rearrange("p (c t) -> p c t", t=128), ALU.mult)


def phase_moe(kb, d, modT, lng, lnb, xin, passes, ctx_tile0, out_fn):
    BIG = 1.0e30
    with ExitStack() as st:
        kb.stack = st
        PT = max(len(p) for p in passes)
        wr = kb.sb([128, 8, 36], F32)
        kb.dma("sp", wr.ap[:, :, 0:4], d["w_group"].rearrange("(kc p) g -> p kc g", p=128), [], [wr])
        kb.dma("sp", wr.ap[:, :, 4:36], d["w_er"].rearrange("(kc p) g -> p kc g", p=128), [], [wr])
        br = kb.sb([36, 1], F32)
        kb.dma("sp", br.ap[0:4, :], d["b_group"], [], [br])
        kb.dma("sp", br.ap[4:36, :], d["b_er"], [], [br])
        ident = kb.sb([128, 128], F32)
        kb.dma("sp", ident.ap, d["ident"], [], [ident])
        sel = kb.sb([32, 32, 128], BF16)
        kb.dma("pool", sel.ap, d["sel"], [], [sel])
        acc = kb.sb([128, 8, PT * 128], F32)
        hT = kb.sb([128, 8, PT * 128], BF16)
        WdT = kb.sb([32, PT * 128], BF16)
        wgu = [kb.sb([128, 8, 1024], BF16) for _ in range(2)]
        wdn = [kb.sb([128, 4, 1024], BF16) for _ in range(2)]
        actT = [kb.sb([128, 4, 512], BF16) for _ in range(2)]
        sg = [kb.sb([128, 512], F32) for _ in range(2)]
        tmp = [kb.sb([128, 512], F32) for _ in range(2)]
        wbs = [kb.sb([128, 512], F32) for _ in range(2)]
        xt = [kb.sb([128, 8, 128], F32) for _ in range(2)]
        hf = kb.sb([128, 8, 128], F32)
        lT = kb.sb([36, 128], F32)
        Lt = kb.sb([128, 36], F32)
        sm = kb.sb([128, 16], F32)
        gm = kb.sb([128, 4], F32)
        pen = kb.sb([128, 4], F32)
        elm = kb.sb([128, 32], F32)
        elm2 = kb.sb([128, 32], F32)
        m1 = kb.sb([128, 32], F32)
        ew = kb.sb([128, 32], F32)
        Wd = kb.sb([128, 32], F32)
        lc = LNCtx(kb)
        xv = xin.ap.rearrange("(kc p) t -> p kc t", p=128)
        wguv = d["w_gate_up"]
        wdnv = d["w_down"]
        ps = kb.ps
        ecount = 0
        xi = 0
        for tiles in passes:
            for li, tile in enumerate(tiles):
                col = 1 if tile >= ctx_tile0 else 0
                x_ = xt[xi % 2]
                xi += 1
                kb.dma("sp", x_.ap, xv[:, :, tile * 128:(tile + 1) * 128], [xin], [x_])
                s_b = modT.ap[:, 32:40, col:col + 1].to_broadcast([128, 8, 128])
                sh_b = modT.ap[:, 24:32, col:col + 1].to_broadcast([128, 8, 128])
                kb.tt("dve", hf, hf.ap, x_, x_.ap, modT, s_b, ALU.mult)
                kb.tt("pool", hf, hf.ap, hf, hf.ap, modT, sh_b, ALU.add)
                kb.cp("act", hT, hT.ap[:, :, li * 128:(li + 1) * 128], hf, hf.ap)
                p7 = ps[7]
                for kc in range(8):
                    kb.mm(p7, p7.ap[0:36, 0:128], wr, wr.ap[:, kc, :], hf, hf.ap[:, kc, :], start=(kc == 0), stop=(kc == 7))
                kb.act(lT, lT.ap, p7, p7.ap[0:36, 0:128], AF.Identity, scale=1.0, bias=br.ap[:, 0:1], extra=[br])
                kb.mm(p7, p7.ap[:, 128:164], lT, lT.ap, ident, ident.ap[0:36, 0:36], acc=True)
                kb.cp("dve", Lt, Lt.ap, p7, p7.ap[:, 128:164])
                gl = Lt.ap[:, 0:4]
                el = Lt.ap[:, 4:36]
                kb.op("dve", lambda e: e.reduce_max(out=sm.ap[:, 0:1], in_=gl, axis=AX.X), [Lt], [sm])
                kb.ts("dve", sm, sm.ap[:, 1:2], sm, sm.ap[:, 0:1], -1.0, None, ALU.mult)
                kb.op("act", lambda e: e.activation(out=gm.ap, in_=gl, func=AF.Exp, bias=sm.ap[:, 1:2], scale=1.0, accum_out=sm.ap[:, 2:3]),
                      [Lt, sm], [gm, sm])
                kb.op("dve", lambda e: e.reciprocal(out=sm.ap[:, 3:4], in_=sm.ap[:, 2:3]), [sm], [sm])
                kb.ts("dve", gm, gm.ap, Lt, gl, sm.ap[:, 0:1], None, ALU.is_ge, extra=[sm])
                kb.ts("dve", pen, pen.ap, gm, gm.ap, BIG, -BIG, ALU.mult, ALU.add)
                kb.tt("dve", elm, elm.ap.rearrange("p (g e) -> p g e", e=8), Lt, el.rearrange("p (g e) -> p g e", e=8),
                      pen, pen.ap.unsqueeze(2).to_broadcast([128, 4, 8]), ALU.add)
                kb.op("dve", lambda e: e.reduce_max(out=sm.ap[:, 4:5], in_=elm.ap, axis=AX.X), [elm], [sm])
                kb.ts("dve", m1, m1.ap, elm, elm.ap, sm.ap[:, 4:5], None, ALU.is_ge, extra=[sm])
                kb.stt("dve", elm2, elm2.ap, m1, m1.ap, -BIG, elm, elm.ap, ALU.mult, ALU.add)
                kb.op("dve", lambda e: e.reduce_max(out=sm.ap[:, 6:7], in_=elm2.ap, axis=AX.X), [elm2], [sm])
                kb.ts("dve", m1, m1.ap, elm, elm.ap, sm.ap[:, 6:7], None, ALU.is_ge, extra=[sm])
                kb.ts("dve", sm, sm.ap[:, 5:6], sm, sm.ap[:, 4:5], -1.0, None, ALU.mult)
                kb.act(ew, ew.ap, elm, elm.ap, AF.Exp, scale=1.0, bias=sm.ap[:, 5:6], extra=[sm])
                kb.act(sm, sm.ap[:, 7:8], sm, sm.ap[:, 6:7], AF.Exp, scale=1.0, bias=sm.ap[:, 5:6])
                kb.ts("dve", sm, sm.ap[:, 7:8], sm, sm.ap[:, 7:8], 1.0, None, ALU.add)
                kb.op("dve", lambda e: e.reciprocal(out=sm.ap[:, 7:8], in_=sm.ap[:, 7:8]), [sm], [sm])
                kb.tt("dve", sm, sm.ap[:, 8:9], sm, sm.ap[:, 7:8], sm, sm.ap[:, 3:4], ALU.mult)
                kb.stt("dve", Wd, Wd.ap, ew, ew.ap, sm.ap[:, 8:9], m1, m1.ap, ALU.mult, ALU.mult, extra=[sm])
                kb.mm(p7, p7.ap[0:32, 256:384], Wd, Wd.ap, ident, ident.ap, acc=True)
                kb.cp("act", WdT, WdT.ap[:, li * 128:(li + 1) * 128], p7, p7.ap[0:32, 256:384])
            nt = len(tiles)
            groups = []
            c = 0
            while c < nt:
                n = min(4, nt - c)
                groups.append((c * 128, n * 128))
                c += n
            ng = len(groups)
            state = {}

            def load_w(e):
                nonlocal ecount
                wg = wgu[ecount % 2]
                wd = wdn[ecount % 2]
                ecount += 1
                gv = wguv[e].rearrange("(kc p) f -> p kc f", p=128)
                kb.dma("pool", wg.ap[:, 0:4, :], gv[:, 0:4, :], [], [wg])
                kb.dma("pool", wg.ap[:, 4:8, :], gv[:, 4:8, :], [], [wg])
                kb.dma("pool", wd.ap, wdnv[e].rearrange("(kc p) f -> p kc f", p=128), [], [wd])
                state[e] = (wg, wd)

            def emit_gu(e, gidx, gi):
                wg, wd = state[e]
                c0, n = groups[gidx]
                aT = actT[gi % 2]
                wb_ = wbs[gi % 2]
                kb.mm(ps[4], ps[4].ap[:, 0:n], sel, sel.ap[:, e, :], WdT, WdT.ap[:, c0:c0 + n])
                kb.cp("act", wb_, wb_.ap[:, 0:n], ps[4], ps[4].ap[:, 0:n])
                for j in range(4):
                    G = ps[j % 2]
                    U = ps[2 + j % 2]
                    for kc in range(8):
                        kb.mm(G, G.ap[:, 0:n], wg, wg.ap[:, kc, j * 128:(j + 1) * 128], hT, hT.ap[:, kc, c0:c0 + n],
                              start=(kc == 0), stop=(kc == 7))
                    for kc in range(8):
                        kb.mm(U, U.ap[:, 0:n], wg, wg.ap[:, kc, 512 + j * 128:512 + (j + 1) * 128], hT, hT.ap[:, kc, c0:c0 + n],
                              start=(kc == 0), stop=(kc == 7))
                    s_ = sg[j % 2]
                    t_ = tmp[j % 2]
                    kb.act(s_, s_.ap[:, 0:n], G, G.ap[:, 0:n], AF.Silu)
                    kb.tt("dve", t_, t_.ap[:, 0:n], U, U.ap[:, 0:n], s_, s_.ap[:, 0:n], ALU.mult)
                    kb.tt("pool", aT, aT.ap[:, j, 0:n], t_, t_.ap[:, 0:n], wb_, wb_.ap[:, 0:n], ALU.mult)
                return (e, c0, n, aT, wd)

            def emit_y(item):
                e, c0, n, aT, wd = item
                for dc in range(8):
                    Y = ps[5 + dc % 2]
                    for j in range(4):
                        kb.mm(Y, Y.ap[:, 0:n], wd, wd.ap[:, j, dc * 128:(dc + 1) * 128], aT, aT.ap[:, j, 0:n],
                              start=(j == 0), stop=(j == 3))
                    if e == 0:
                        kb.cp("dve", acc, acc.ap[:, dc, c0:c0 + n], Y, Y.ap[:, 0:n])
                    else:
                        kb.tt("dve", acc, acc.ap[:, dc, c0:c0 + n], Y, Y.ap[:, 0:n], acc, acc.ap[:, dc, c0:c0 + n], ALU.add)

            load_w(0)
            prev = None
            gi = 0
            for e in range(32):
                for gidx in range(ng):
                    cur = emit_gu(e, gidx, gi)
                    gi += 1
                    if prev is not None:
                        emit_y(prev)
                    if gidx == 0 and e + 1 < 32:
                        load_w(e + 1)
                    prev = cur
            emit_y(prev)
            for li, tile in enumerate(tiles):
                col = 1 if tile >= ctx_tile0 else 0
                oap, oT_ = out_fn(tile)

                def y_fn(dc, li=li):
                    return acc, acc.ap[:, dc, li * 128:(li + 1) * 128]
                ln_epilogue(kb, lc, y_fn, xv[:, :, tile * 128:(tile + 1) * 128], [xin], modT.ap[:, 40:48, col:col + 1], modT,
                            lng.ap[:, 1, :], lnb.ap[:, 1, :], (lng, lnb), oap, oT_, ps[7])
        kb.P.barrier()


def _rope_np(pos, dim):
    pos = np.asarray(pos)
    pos_r = (pos // 64).astype(np.float32)
    pos_c = (pos % 64).astype(np.float32)
    quarter = dim // 4
    inv = np.power(np.float32(10000.0), -(np.arange(quarter, dtype=np.float32) / np.float32(quarter))).astype(np.float32)
    ang_r = pos_r[:, None] * inv
    ang_c = pos_c[:, None] * inv
    ang = np.concatenate([ang_r, ang_r, ang_c, ang_c], -1).astype(np.float32)
    return np.cos(ang).astype(np.float32), np.sin(ang).astype(np.float32)


def _ext_tokens(r):
    base = r * 4096 - 256 + np.arange(NEXT)
    if r == 0:
        base[0:256] = 256 + np.arange(256)
    if r == 3:
        base[4352:4608] = 248 * 64 + np.arange(256)
    return base


def _bias_tables(rpb, r):
    out = np.full((128, 5, 5, 8, 128), NEG, np.float32)
    kk = np.arange(128)
    kr, kc = kk // 64, kk % 64
    qr, qc = kk // 64, kk % 64
    cs = np.clip(qc - 8, 0, 48)
    validc = (kc[:, None] >= cs[None, :]) & (kc[:, None] < cs[None, :] + 16)
    dc = np.clip(kc[:, None] - qc[None, :], -15, 15) + 15
    for vi, j in enumerate([0, 1, 2, 30, 31]):
        for t in range(5):
            ext_row = 2 * j + 2 * t + kr
            lr = 2 * j + qr
            xw = ext_row[:, None] - lr[None, :]
            validr = (xw >= 0) & (xw <= 7)
            if r == 0:
                act = np.where(ext_row < 4, ext_row + 4, ext_row - 4)
            elif r == 3:
                act = np.where(ext_row >= 68, 248 + (ext_row - 68), 188 + ext_row)
            else:
                act = r * 64 - 4 + ext_row
            rq = r * 64 + lr
            dr = act[:, None] - rq[None, :] + 7
            ws = np.clip(rq - 4, 0, 248)
            inwin = (act[:, None] >= ws[None, :]) & (act[:, None] < ws[None, :] + 8)
            assert np.array_equal(validr & inwin, validr), (r, j, t)
            valid = validr & validc
            drc = np.clip(dr, 0, 14)
            vals = rpb[:, drc, dc]
            out[:, vi, t, :, :] = np.where(valid[:, None, :], vals.transpose(1, 0, 2), np.float32(NEG))
    return out


def _mask_a(r):
    kk = np.arange(128)
    prev = np.where(kk[:, None] >= kk[None, :], 0.0, NEG).astype(np.float32)
    nxt = np.where(kk[:, None] <= kk[None, :], 0.0, NEG).astype(np.float32)
    allneg = np.full((128, 128), NEG, np.float32)
    m = np.stack([allneg if r == 0 else prev, prev, nxt, allneg if r == 3 else nxt], axis=1)
    return np.ascontiguousarray(m)


def _fm(v, nch):
    return np.ascontiguousarray(np.asarray(v, np.float32).reshape(nch, 128).T)


def _moe_inputs(inp, l):
    sel = np.zeros((32, 32, 128), np.float32)
    for e in range(32):
        sel[e, e, :] = 1.0
    return {
        "w_group": np.ascontiguousarray(inp["w_group"][l]),
        "b_group": np.ascontiguousarray(inp["b_group"][l].reshape(4, 1)),
        "w_er": np.ascontiguousarray(inp["w_exp_router"][l]),
        "b_er": np.ascontiguousarray(inp["b_exp_router"][l].reshape(32, 1)),
        "w_gate_up": np.ascontiguousarray(inp["w_gate_up"][l]),
        "w_down": np.ascontiguousarray(inp["w_down"][l]),
        "ident": np.eye(128, dtype=np.float32),
        "sel": sel,
    }


def _common_inputs(inp, l, b):
    cT = np.stack([_fm(inp["c"][b], 8), _fm(inp["c_ctx"], 8)], axis=2)
    lg = np.stack([_fm(inp["ln_g"][l, 0], 8), _fm(inp["ln_g"][l, 1], 8)], axis=1)
    lb = np.stack([_fm(inp["ln_b"][l, 0], 8), _fm(inp["ln_b"][l, 1], 8)], axis=1)
    return {
        "cT": np.ascontiguousarray(cT),
        "w_ada": np.ascontiguousarray(inp["w_ada"][l]),
        "b_adaT": _fm(inp["b_ada"][l], 48),
        "ln_gT": np.ascontiguousarray(lg),
        "ln_bT": np.ascontiguousarray(lb),
    }


def _declare_moe(kb, sfx=""):
    return {
        "w_group": kb.din("w_group" + sfx, [1024, 4]), "b_group": kb.din("b_group" + sfx, [4, 1]),
        "w_er": kb.din("w_er" + sfx, [1024, 32]), "b_er": kb.din("b_er" + sfx, [32, 1]),
        "w_gate_up": kb.din("w_gate_up" + sfx, [32, 1024, 1024]), "w_down": kb.din("w_down" + sfx, [32, 512, 1024]),
        "ident": kb.din("ident" + sfx, [128, 128]), "sel": kb.din("sel" + sfx, [32, 32, 128]),
    }


def _declare_common(kb, sfx=""):
    return {
        "cT": kb.din("cT" + sfx, [128, 8, 2]), "w_ada": kb.din("w_ada" + sfx, [1024, 6144]),
        "b_adaT": kb.din("b_adaT" + sfx, [128, 48]), "ln_gT": kb.din("ln_gT" + sfx, [128, 2, 8]), "ln_bT": kb.din("ln_bT" + sfx, [128, 2, 8]),
    }


def build_launch1(debug=False, stop=None):
    kb = KB()
    d = _declare_common(kb)
    d.update(_declare_moe(kb))
    d.update({
        "xT": kb.din("xT", [1024, NEXT]), "ctxT": kb.din("ctxT", [1024, 256]),
        "ab_w_in": kb.din("ab_w_in", [1024, 2304]), "ab_w_out": kb.din("ab_w_out", [1024, 1024]),
        "a_sink": kb.din("a_sink", [1, 8]), "biasB": kb.din("biasB", [128, 5, 5, 8, 128]),
        "maskA": kb.din("maskA", [128, 4, 128]), "ropeT": kb.din("ropeT", [128, 2, NEXT]),
    })
    out = T(kb.dout("x2T", [1024, 4352]))
    if debug:
        x1T = T(kb.dout("x1T", [1024, 4352]))
    else:
        x1T = kb.dscr("x1T", [1024, 4352], F32)
    modT = phase_mod(kb, d["w_ada"], d["b_adaT"], d["cT"])
    lng, lnb = load_ln(kb, d["ln_gT"], d["ln_bT"])
    if stop == "mod":
        dbg = T(kb.dout("modT", [128, 48, 2]))
        kb.dma("sp", dbg.ap, modT.ap, [modT], [dbg])
        kb.P.barrier()
        return kb
    phase_l0_attn(kb, d, modT, lng, lnb, x1T)
    if stop == "l0":
        kb.P.barrier()
        return kb
    ov = out.ap.rearrange("(kc p) t -> p kc t", p=128)
    passes = [list(range(0, 10)), list(range(10, 18)), list(range(18, 26)), list(range(26, 34))]
    phase_moe(kb, d, modT, lng, lnb, x1T, passes, 32, lambda tile: (ov[:, :, tile * 128:(tile + 1) * 128], out))
    kb.P.barrier()
    return kb


def launch1_inputs(inp):
    maps = []
    rpb = np.asarray(inp["b_rpb"][0], np.float32)
    moe = _moe_inputs(inp, 0)
    tabs = {r: _bias_tables(rpb, r) for r in range(4)}
    for core in range(8):
        b, r = core // 4, core % 4
        et = _ext_tokens(r)
        cos, sin = _rope_np(et, 64)
        rope = np.stack([np.concatenate([cos.T, cos.T], 0), np.concatenate([sin.T, sin.T], 0)], axis=1)
        m = _common_inputs(inp, 0, b)
        m.update(moe)
        m.update({
            "xT": np.ascontiguousarray(inp["x"][b][et].T),
            "ctxT": np.ascontiguousarray(inp["ctx"][b].T),
            "ab_w_in": np.ascontiguousarray(inp["ab_w_in"][0]),
            "ab_w_out": np.ascontiguousarray(inp["ab_w_out"][0]),
            "a_sink": np.ascontiguousarray(inp["a_sink"][0].reshape(1, 8)),
            "biasB": tabs[r], "maskA": _mask_a(r), "ropeT": np.ascontiguousarray(rope),
        })
        maps.append(m)
    return maps


def attn_unit_b(kb, ac, tiles, scale, out_T, out_ap3):
    ob = ac.o_banks[ac.oi % len(ac.o_banks)]
    ac.oi += 1
    X, Y = ac.s_banks[0], ac.s_banks[1]
    bi0 = kb.ps.index(X)
    assert kb.ps.index(Y) == bi0 + 1
    pview = kb.psall[:, bi0 * 512:(bi0 + 2) * 512].rearrange("p (b c) -> p b c", b=2)[:, :, 0:256]
    nt = len(tiles)
    for ti, tl in enumerate(tiles):
        for hh, (kT, kap, qT, qap, c0, n) in enumerate(tl["qk"]):
            bk = X if hh % 2 == 0 else Y
            cc = (hh // 2) * 128
            kb.mm(bk, bk.ap[:, cc:cc + 128], kT, kap, qT, qap, start=True, stop=True, acc=(hh >= 2))
        E = ac.E[ac.ei % 3]
        ac.ei += 1
        ev = E.ap.rearrange("p (b c) -> p b c", b=2)
        if tl["bias"] is not None:
            bT, bap = tl["bias"]
            sbuf = ac.sb_[ac.bi % 2]
            ac.bi += 1
            for b_, bk_ in enumerate((X, Y)):
                kb.stt("dve", sbuf, sbuf.ap[:, b_ * 256:(b_ + 1) * 256].rearrange("p (a q) -> p a q", a=2),
                       bk_, bk_.ap[:, 0:256].rearrange("p (a q) -> p a q", a=2), scale, bT, bap[:, b_, :, :], ALU.mult, ALU.add)
            kb.act(E, E.ap, sbuf, sbuf.ap, AF.Exp)
        else:
            kb.op("act", lambda e: e.activation(out=ev, in_=pview, func=AF.Exp, scale=scale), [X, Y], [E])
        last = (ti == nt - 1)
        for hh, (vT, vap, c0, n) in enumerate(tl["pv"]):
            ec = (hh % 2) * 256 + (hh // 2) * 128
            kb.mm(ob, ob.ap[0:65, hh * 128:(hh + 1) * 128], vT, vap, E, E.ap[:, ec:ec + 128], start=(ti == 0 and hh == 0), stop=last,
                  acc=not (ti == 0 and hh == 0), skip=True, inc=last)
    zr = ac.zr
    kb.op("dve", lambda e: e.reciprocal(out=zr.ap[64:65, :], in_=ob.ap[64:65, :]), [ob], [zr])
    bc = ac.bc
    kb.mm(bc, bc.ap[0:64, :], kb.ones_f, kb.ones_f.ap[64:65, 0:64], zr, zr.ap[64:65, :])
    kb.cp("act", ac.bcs, ac.bcs.ap, bc, bc.ap[0:64, :])
    kb.tt("dve", out_T, out_ap3, ob, ob.ap[0:64, :].rearrange("p (c t) -> p c t", t=128),
          ac.bcs, ac.bcs.ap.rearrange("p (c t) -> p c t", t=128), ALU.mult)


NK = 16640
NKT = NK // 128


class Banks:
    def __init__(self, kb, idx):
        self.kb = kb
        self.idx = list(idx)
        self.i = 0

    def get(self):
        b = self.kb.ps[self.idx[self.i % len(self.idx)]]
        self.i += 1
        return b


def l1_scratch(kb):
    return dict(
        kTm=kb.dscr("kTm", [8, 96, NK], BF16), Vm=kb.dscr("Vm", [8, 128, NKT, 66], BF16),
        kTd=kb.dscr("kTd", [4, 128, NK], BF16), Vd=kb.dscr("Vd", [4, 128, NKT, 128], BF16),
        qTm=kb.dscr("qTm", [8, 96, 4096], BF16), qTd=kb.dscr("qTd", [4, 128, 4096], BF16),
        oTs=kb.dscr("oTs", [12, 128, 4096], BF16), x1b=kb.dscr("x1b", [1024, 4096], F32))


def phase_l1_proj(kb, d, modT, scr):
    with ExitStack() as st:
        kb.stack = st
        win = d["cd_w_in"].rearrange("(kc p) c -> p kc c", p=128)
        wq_c = kb.sb([128, 8, 384], BF16)
        wkv = kb.sb([128, 8, 256], BF16)
        wkr = kb.sb([128, 8, 96], BF16)
        wkr_r = kb.sb([128, 8, 96], BF16)
        wdq = kb.sb([128, 8, 512], BF16)
        wdq_r = kb.sb([128, 8, 512], BF16)
        wdk = kb.sb([128, 8, 512], BF16)
        wdk_r = kb.sb([128, 8, 512], BF16)
        wdv = kb.sb([128, 8, 512], BF16)
        kb.dma("pool", wq_c.ap, win[:, :, 0:384], [], [wq_c])
        kb.dma("pool", wkv.ap, win[:, :, 384:640], [], [wkv])
        kb.memset("dve", wkr, wkr.ap, 0.0)
        kb.memset("dve", wkr_r, wkr_r.ap, 0.0)
        kb.dma("pool", wkr.ap[:, :, 64:96], win[:, :, 640:672], [], [wkr])
        kb.dma("pool", wdq.ap, win[:, :, 672:1184], [], [wdq])
        kb.dma("pool", wdk.ap, win[:, :, 1184:1696], [], [wdk])
        kb.dma("pool", wdv.ap, win[:, :, 1696:2208], [], [wdv])

        def mkrot(dst, src, dap, sap, s_):
            sv = sap.rearrange("p k (g two s) -> p k g two s", two=2, s=s_)
            dv = dap.rearrange("p k (g two s) -> p k g two s", two=2, s=s_)
            for kc in range(sap.shape[1]):
                kb.ts("dve", dst, dv[:, kc, :, 0, :], src, sv[:, kc, :, 1, :], -1.0, None, ALU.mult)
                kb.cp("dve", dst, dv[:, kc, :, 1, :], src, sv[:, kc, :, 0, :])
        mkrot(wkr_r, wkr, wkr_r.ap[:, :, 64:96], wkr.ap[:, :, 64:96], 8)
        mkrot(wdq_r, wdq, wdq_r.ap, wdq.ap, 16)
        mkrot(wdk_r, wdk, wdk_r.ap, wdk.ap, 16)
        wuq = kb.sb([128, 3, 768], BF16)
        wuq_r = kb.sb([128, 3, 768], BF16)
        kb.dma("pool", wuq.ap, d["c_w_uq"].rearrange("(kc p) c -> p kc c", p=128), [], [wuq])
        kb.memset("dve", wuq_r, wuq_r.ap, 0.0)
        for h in range(8):
            mkrot(wuq_r, wuq, wuq_r.ap[:, :, h * 96 + 64:h * 96 + 96], wuq.ap[:, :, h * 96 + 64:h * 96 + 96], 8)
        wukv = kb.sb([128, 2, 1024], BF16)
        kb.dma("pool", wukv.ap, d["c_w_ukv"].rearrange("(kc p) c -> p kc c", p=128), [], [wukv])
        qg = kb.sb([128, 3], F32)
        kvg = kb.sb([128, 2], F32)
        kb.dma("sp", qg.ap, d["c_q_normT"], [], [qg])
        kb.dma("sp", kvg.ap, d["c_kv_normT"], [], [kvg])
        eps6 = kb.sb([128, 1], F32)
        kb.memset("dve", eps6, eps6.ap, 1e-6)

        xb = [kb.sb([128, 8, 512], F32) for _ in range(2)]
        hTs = [kb.sb([128, 8, 512], BF16) for _ in range(2)]
        r64 = [kb.sb([128, 2, 512], F32) for _ in range(2)]
        r32 = [kb.sb([128, 2, 512], F32) for _ in range(2)]
        sq = kb.sb([128, 3, 512], BF16)
        rs = kb.sb([128, 512], F32)
        ckvn = kb.sb([128, 2, 512], BF16)
        cqn = kb.sb([128, 3, 512], BF16)
        kn = kb.sb([64, 8, 512], BF16)
        krs = kb.sb([96, 512], BF16)
        t1 = kb.sb([128, 2, 512], F32)
        t2 = kb.sb([128, 2, 512], F32)
        dks = [kb.sb([128, 4, 512], BF16) for _ in range(2)]
        vms = kb.sb([128, 8, 4, 66], BF16)
        kb.memset("pool", vms, vms.ap[:, :, :, 64:66], 1.0)
        vds = kb.sb([128, 4, 4, 128], BF16)
        qs = [kb.sb([96, 512], BF16) for _ in range(2)]
        bk = Banks(kb, range(8))
        xv = d["x1T"].rearrange("(kc p) t -> p kc t", p=128)
        cv = d["ctx1T"].rearrange("(kc p) t -> p kc t", p=128)
        xrd = d.get("x_reads", [])
        di = [0]

        def rms(chunks, nch, gT, dstT, n):
            for c, (pt, pap) in enumerate(chunks):
                kb.act(sq, sq.ap[:, c, 0:n], pt, pap, AF.Square)
            ssb = bk.get()
            for c in range(nch):
                kb.mm(ssb, ssb.ap[:, 0:n], kb.ones_b, kb.ones_b.ap, sq, sq.ap[:, c, 0:n], start=(c == 0), stop=(c == nch - 1))
            kb.act(rs, rs.ap[:, 0:n], ssb, ssb.ap[:, 0:n], AF.Ln, scale=1.0 / (128 * nch), bias=eps6.ap[:, 0:1], extra=[eps6])
            kb.act(rs, rs.ap[:, 0:n], rs, rs.ap[:, 0:n], AF.Exp, scale=-0.5)
            for c, (pt, pap) in enumerate(chunks):
                kb.stt("dve", dstT, dstT.ap[:, c, 0:n], pt, pap, gT.ap[:, c:c + 1], rs, rs.ap[:, 0:n], ALU.mult, ALU.mult, extra=[gT])

        def proj_fm(w, c0, hT, n, M=128):
            b = bk.get()
            for kc in range(8):
                kb.mm(b, b.ap[0:M, 0:n], w, w.ap[:, kc, c0:c0 + M], hT, hT.ap[:, kc, 0:n], start=(kc == 0), stop=(kc == 7))
            return b

        def rope_pair(dstT, dst_ap, p1, p2, tab, rows, n, eng2="pool"):
            lo, hi = rows
            kb.tt("dve", t1, t1.ap[lo:hi, 0, 0:n], p1, p1.ap[lo:hi, 0:n], tab, tab.ap[lo:hi, 0, 0:n], ALU.mult)
            kb.tt("dve", t2, t2.ap[lo:hi, 0, 0:n], p2, p2.ap[lo:hi, 0:n], tab, tab.ap[lo:hi, 1, 0:n], ALU.mult)
            kb.tt(eng2, dstT, dst_ap, t1, t1.ap[lo:hi, 0, 0:n], t2, t2.ap[lo:hi, 0, 0:n], ALU.add)

        for g in range(33):
            ctx = (g == 32)
            own = (g < 8)
            n = 256 if ctx else 512
            col = 1 if ctx else 0
            k0 = g * 512
            i = di[0]
            di[0] += 1
            xt, hT = xb[i % 2], hTs[i % 2]
            src = cv if ctx else xv[:, :, k0:k0 + 512]
            kb.dma("sp", xt.ap[:, :, 0:n], src, xrd, [xt])
            s_b = modT.ap[:, 8:16, col:col + 1].to_broadcast([128, 8, n])
            sh_b = modT.ap[:, 0:8, col:col + 1].to_broadcast([128, 8, n])
            kb.tt("dve", xt, xt.ap[:, :, 0:n], xt, xt.ap[:, :, 0:n], modT, s_b, ALU.mult)
            kb.tt("pool", hT, hT.ap[:, :, 0:n], xt, xt.ap[:, :, 0:n], modT, sh_b, ALU.add)
            if not ctx:
                a64, a32 = r64[i % 2], r32[i % 2]
                kb.dma("sp", a64.ap, d["rope64"][:, :, k0:k0 + 512], [], [a64])
                kb.dma("sp", a32.ap, d["rope32"][:, :, k0:k0 + 512], [], [a32])
            pc = [proj_fm(wkv, c * 128, hT, n) for c in range(2)]
            rms([(p, p.ap[:, 0:n]) for p in pc], 2, kvg, ckvn, n)
            for h in range(8):
                b = bk.get()
                for c in range(2):
                    kb.mm(b, b.ap[0:64, 0:n], wukv, wukv.ap[:, c, h * 128:h * 128 + 64], ckvn, ckvn.ap[:, c, 0:n], start=(c == 0), stop=(c == 1))
                kb.cp("act", kn, kn.ap[:, h, 0:n], b, b.ap[0:64, 0:n])
            kb.dma("pool", scr["kTm"].ap[:, 0:64, k0:k0 + n].rearrange("h p n -> p h n"), kn.ap[:, :, 0:n], [kn], [scr["kTm"]])
            p1 = proj_fm(wkr, 0, hT, n, M=96)
            if not ctx:
                p2 = proj_fm(wkr_r, 0, hT, n, M=96)
                rope_pair(krs, krs.ap[64:96, 0:n], p1, p2, a32, (64, 96), n)
            else:
                kb.cp("act", krs, krs.ap[64:96, 0:n], p1, p1.ap[64:96, 0:n])
            for h in range(8):
                kb.dma("pool", scr["kTm"].ap[h, 64:96, k0:k0 + n], krs.ap[64:96, 0:n], [krs], [scr["kTm"]])
            nt = n // 128
            for tt in range(nt):
                b = bk.get()
                for c in range(2):
                    kb.mm(b, b.ap, ckvn, ckvn.ap[:, c, tt * 128:(tt + 1) * 128],
                          wukv, wukv.ap[:, c, :].rearrange("p (h x) -> p h x", x=128)[:, :, 64:128], start=(c == 0), stop=(c == 1))
                kb.cp("act", vms, vms.ap[:, :, tt, 0:64], b, b.ap.rearrange("p (h e) -> p h e", e=64))
            kb.dma("pool", scr["Vm"].ap[:, :, g * 4:g * 4 + nt, :].rearrange("h p t e -> p h t e"), vms.ap[:, :, 0:nt, :], [vms], [scr["Vm"]])
            dk_ = dks[i % 2]
            for half in range(2):
                pl = [proj_fm(wdk, (2 * half + c) * 128, hT, n) for c in range(2)]
                if not ctx:
                    pr = [proj_fm(wdk_r, (2 * half + c) * 128, hT, n) for c in range(2)]
                    for c in range(2):
                        rope_pair(dk_, dk_.ap[:, 2 * half + c, 0:n], pl[c], pr[c], a64, (0, 128), n)
                else:
                    for c in range(2):
                        kb.cp("act", dk_, dk_.ap[:, 2 * half + c, 0:n], pl[c], pl[c].ap[:, 0:n])
            kb.dma("pool", scr["kTd"].ap[:, :, k0:k0 + n].rearrange("h p n -> p h n"), dk_.ap[:, :, 0:n], [dk_], [scr["kTd"]])
            for tt in range(nt):
                b = bk.get()
                for kc in range(8):
                    kb.mm(b, b.ap, hT, hT.ap[:, kc, tt * 128:(tt + 1) * 128], wdv, wdv.ap[:, kc, :], start=(kc == 0), stop=(kc == 7))
                kb.cp("act", vds, vds.ap[:, :, tt, :], b, b.ap.rearrange("p (h e) -> p h e", e=128))
            kb.dma("pool", scr["Vd"].ap[:, :, g * 4:g * 4 + nt, :].rearrange("h p t e -> p h t e"), vds.ap[:, :, 0:nt, :], [vds], [scr["Vd"]])
            if own:
                pq = [proj_fm(wq_c, c * 128, hT, n) for c in range(3)]
                rms([(p, p.ap[:, 0:n]) for p in pq], 3, qg, cqn, n)
                for h in range(8):
                    q_ = qs[h % 2]
                    b1 = bk.get()
                    b2 = bk.get()
                    for c in range(3):
                        kb.mm(b1, b1.ap[0:96, 0:n], wuq, wuq.ap[:, c, h * 96:(h + 1) * 96], cqn, cqn.ap[:, c, 0:n], start=(c == 0), stop=(c == 2))
                    for c in range(3):
                        kb.mm(b2, b2.ap[0:96, 0:n], wuq_r, wuq_r.ap[:, c, h * 96:(h + 1) * 96], cqn, cqn.ap[:, c, 0:n], start=(c == 0), stop=(c == 2))
                    kb.cp("act", q_, q_.ap[0:64, 0:n], b1, b1.ap[0:64, 0:n])
                    rope_pair(q_, q_.ap[64:96, 0:n], b1, b2, a32, (64, 96), n)
                    kb.dma("pool", scr["qTm"].ap[h, :, k0:k0 + n], q_.ap[:, 0:n], [q_], [scr["qTm"]])
                dq_ = dks[(i + 1) % 2]
                for half in range(2):
                    pl = [proj_fm(wdq, (2 * half + c) * 128, hT, n) for c in range(2)]
                    pr = [proj_fm(wdq_r, (2 * half + c) * 128, hT, n) for c in range(2)]
                    for c in range(2):
                        rope_pair(dq_, dq_.ap[:, 2 * half + c, 0:n], pl[c], pr[c], a64, (0, 128), n)
                kb.dma("pool", scr["qTd"].ap[:, :, k0:k0 + n].rearrange("h p n -> p h n"), dq_.ap[:, :, 0:n], [dq_], [scr["qTd"]])
        kb.P.barrier()


def phase_l1_attn(kb, d, scr):
    with ExitStack() as st:
        kb.stack = st
        lp = kb.sb([128, 256], F32)
        kb.dma("sp", lp.ap, d["d_lambda"].rearrange("a b -> (a b)").partition_broadcast(128), [], [lp])
        pr_ = kb.sb([128, 128], F32)
        lpv = lp.ap.rearrange("p (a b) -> p a b", b=64)
        kb.tt("dve", pr_, pr_.ap.rearrange("p (a b) -> p a b", b=64), lp, lpv[:, 0:4:2, :], lp, lpv[:, 1:4:2, :], ALU.mult)
        ssum = kb.sb([128, 4], F32)
        kb.op("dve", lambda e: e.reduce_sum(out=ssum.ap[:, 0:2], in_=pr_.ap.rearrange("p (a b) -> p a b", b=64), axis=AX.X), [pr_], [ssum])
        kb.act(ssum, ssum.ap[:, 0:2], ssum, ssum.ap[:, 0:2], AF.Exp)
        kb.tt("dve", ssum, ssum.ap[:, 2:3], ssum, ssum.ap[:, 1:2], ssum, ssum.ap[:, 0:1], ALU.subtract)
        kb.ts("dve", ssum, ssum.ap[:, 2:3], ssum, ssum.ap[:, 2:3], -LAM_INIT, None, ALU.add)
        subl = kb.sb([128, 1], F32)
        kb.dma("sp", subl.ap, d["d_sublnT"], [], [subl])
        kb.ts("dve", subl, subl.ap, subl, subl.ap, 1.0 - LAM_INIT, None, ALU.mult)
        eps6 = kb.sb([128, 1], F32)
        kb.memset("dve", eps6, eps6.ap, 1e-6)

        E = [kb.sb([128, 2, 512], BF16) for _ in range(3)]
        kT = kb.sb([128, NK], BF16)
        V = kb.sb([128, NKT, 128], BF16)
        qT = kb.sb([128, 4096], BF16)
        zr = kb.sb([128, 512], F32)
        bcs = kb.sb([128, 512], F32)
        ta = kb.sb([128, 512], F32)
        tb = kb.sb([128, 512], F32)
        osb = [kb.sb([128, 512], BF16) for _ in range(2)]
        ei = 0
        oi = 0
        sp_i = 0
        ps = kb.ps

        def load(dst, dst_ap_fn, src_T, src_ap_fn, total, nsplit, reads):
            step = total // nsplit
            for s_ in range(nsplit):
                kb.dma("sp", dst_ap_fn(s_ * step, (s_ + 1) * step), src_ap_fn(s_ * step, (s_ + 1) * step), reads, [dst])

        sc_m = 96 ** -0.5
        Vm_v = V.ap.rearrange("p t e -> p (t e)")[:, 0:NKT * 66].rearrange("p (t e) -> p t e", e=66)
        for h in range(8):
            load(kT, lambda a, b: kT.ap[0:96, a:b], scr["kTm"], lambda a, b: scr["kTm"].ap[h, :, a:b], NK, 4, [scr["kTm"]])
            load(V, lambda a, b: Vm_v[:, a:b, :], scr["Vm"], lambda a, b: scr["Vm"].ap[h, :, a:b, :], NKT, 2, [scr["Vm"]])
            kb.dma("sp", qT.ap[0:96, :], scr["qTm"].ap[h], [scr["qTm"]], [qT])
            for qb in range(8):
                ob = ps[4 + oi % 2]
                oi += 1
                qap = qT.ap[0:96, qb * 512:(qb + 1) * 512]

                def s_step(kp):
                    pb_ = 2 * (kp % 2)
                    X, Y = ps[pb_], ps[pb_ + 1]
                    kb.mm(X, X.ap, kT, kT.ap[0:96, (2 * kp) * 128:(2 * kp + 1) * 128], qT, qap)
                    kb.mm(Y, Y.ap, kT, kT.ap[0:96, (2 * kp + 1) * 128:(2 * kp + 2) * 128], qT, qap)
                    return pb_, X, Y
                nxt = s_step(0)
                for kp in range(NKT // 2):
                    pb_, X, Y = nxt
                    if kp + 1 < NKT // 2:
                        nxt = s_step(kp + 1)
                    E_ = E[ei % 3]
                    ei += 1
                    kb.op("act", lambda e: e.activation(out=E_.ap.rearrange("p a b -> p (a b)"), in_=kb.psall[:, pb_ * 512:(pb_ + 2) * 512],
                                                        func=AF.Exp, scale=sc_m), [X, Y], [E_])
                    for a_ in range(2):
                        first = (kp == 0 and a_ == 0)
                        last = (kp == NKT // 2 - 1 and a_ == 1)
                        kb.mm(ob, ob.ap[0:65, :], V, Vm_v[:, 2 * kp + a_, 0:65], E_, E_.ap[:, a_, :], start=first, stop=last, acc=not first)
                kb.op("dve", lambda e: e.reciprocal(out=zr.ap[64:65, :], in_=ob.ap[64:65, :]), [ob], [zr])
                bc = ps[6]
                kb.mm(bc, bc.ap[0:64, :], kb.ones_f, kb.ones_f.ap[64:65, 0:64], zr, zr.ap[64:65, :])
                kb.cp("act", bcs, bcs.ap[0:64, :], bc, bc.ap[0:64, :])
                o_ = osb[oi % 2]
                kb.tt("dve", o_, o_.ap[0:64, :], ob, ob.ap[0:64, :], bcs, bcs.ap[0:64, :], ALU.mult)
                kb.dma("pool", scr["oTs"].ap[h, 0:64, qb * 512:(qb + 1) * 512], o_.ap[0:64, :], [o_], [scr["oTs"]])
        for h in range(4):
            load(kT, lambda a, b: kT.ap[:, a:b], scr["kTd"], lambda a, b: scr["kTd"].ap[h, :, a:b], NK, 4, [scr["kTd"]])
            load(V, lambda a, b: V.ap[:, a:b, :], scr["Vd"], lambda a, b: scr["Vd"].ap[h, :, a:b, :], NKT, 2, [scr["Vd"]])
            kb.dma("sp", qT.ap, scr["qTd"].ap[h], [scr["qTd"]], [qT])
            for qb in range(8):
                o0, o1, Z0, Z1 = ps[4], ps[5], ps[6], ps[7]
                qsl = slice(qb * 512, (qb + 1) * 512)

                def s_step(kt):
                    pb_ = 2 * (kt % 2)
                    X, Y = ps[pb_], ps[pb_ + 1]
                    ksl = slice(kt * 128, (kt + 1) * 128)
                    kb.mm(X, X.ap, kT, kT.ap[0:64, ksl], qT, qT.ap[0:64, qsl])
                    kb.mm(Y, Y.ap, kT, kT.ap[64:128, ksl], qT, qT.ap[64:128, qsl])
                    return pb_, X, Y
                nxt = s_step(0)
                for kt in range(NKT):
                    pb_, X, Y = nxt
                    if kt + 1 < NKT:
                        nxt = s_step(kt + 1)
                    E_ = E[ei % 3]
                    ei += 1
                    kb.op("act", lambda e: e.activation(out=E_.ap.rearrange("p a b -> p (a b)"), in_=kb.psall[:, pb_ * 512:(pb_ + 2) * 512],
                                                        func=AF.Exp, scale=0.125), [X, Y], [E_])
                    first = (kt == 0)
                    last = (kt == NKT - 1)
                    for a_, (o_b, z_b) in enumerate(((o0, Z0), (o1, Z1))):
                        kb.mm(o_b, o_b.ap, V, V.ap[:, kt, :], E_, E_.ap[:, a_, :], start=first, stop=last, acc=not first)
                        kb.mm(z_b, z_b.ap, kb.ones_b, kb.ones_b.ap, E_, E_.ap[:, a_, :], start=first, stop=last, acc=not first)
                kb.op("dve", lambda e: e.reciprocal(out=zr.ap, in_=Z0.ap), [Z0], [zr])
                kb.tt("dve", ta, ta.ap, o0, o0.ap, zr, zr.ap, ALU.mult)
                kb.op("dve", lambda e: e.reciprocal(out=zr.ap, in_=Z1.ap), [Z1], [zr])
                kb.tt("dve", tb, tb.ap, o1, o1.ap, zr, zr.ap, ALU.mult)
                kb.stt("dve", ta, ta.ap, tb, tb.ap, ssum.ap[:, 2:3], ta, ta.ap, ALU.mult, ALU.add, extra=[ssum])
                kb.tt("pool", tb, tb.ap, ta, ta.ap, ta, ta.ap, ALU.mult)
                sb_ = ps[0]
                kb.mm(sb_, sb_.ap, kb.ones_f, kb.ones_f.ap, tb, tb.ap)
                kb.act(bcs, bcs.ap, sb_, sb_.ap, AF.Ln, scale=1.0 / 128, bias=eps6.ap[:, 0:1], extra=[eps6])
                kb.act(bcs, bcs.ap, bcs, bcs.ap, AF.Exp, scale=-0.5)
                o_ = osb[oi % 2]
                oi += 1
                kb.stt("dve", o_, o_.ap, ta, ta.ap, subl.ap[:, 0:1], bcs, bcs.ap, ALU.mult, ALU.mult, extra=[subl])
                kb.dma("pool", scr["oTs"].ap[8 + h, :, qb * 512:(qb + 1) * 512], o_.ap, [o_], [scr["oTs"]])
        kb.P.barrier()


def phase_l1_out(kb, d, modT, lng, lnb, scr):
    with ExitStack() as st:
        kb.stack = st
        wC = kb.sb([64, 8, 1024], BF16)
        wD = kb.sb([128, 4, 1024], BF16)
        kb.dma("pool", wC.ap, d["cd_w_out"][0:512, :].rearrange("(h p) c -> p h c", p=64), [], [wC])
        kb.dma("pool", wD.ap, d["cd_w_out"][512:1024, :].rearrange("(h p) c -> p h c", p=128), [], [wD])
        oTb = [kb.sb([128, 12, 128], BF16) for _ in range(2)]
        lc = LNCtx(kb)
        xv = d["x1T"].rearrange("(kc p) t -> p kc t", p=128)
        ov = scr["x1b"].ap.rearrange("(kc p) t -> p kc t", p=128)
        ps = kb.ps
        for tile in range(32):
            oT = oTb[tile % 2]
            cs_ = slice(tile * 128, (tile + 1) * 128)
            kb.dma("sp", oT.ap[0:64, 0:8, :], scr["oTs"].ap[0:8, 0:64, cs_].rearrange("h p n -> p h n"), [scr["oTs"]], [oT])
            kb.dma("sp", oT.ap[:, 8:12, :], scr["oTs"].ap[8:12, :, cs_].rearrange("h p n -> p h n"), [scr["oTs"]], [oT])
            y0, y1 = ps[(2 * tile) % 4], ps[(2 * tile) % 4 + 1]
            for dc in range(8):
                pb_ = y0 if dc < 4 else y1
                c0 = (dc % 4) * 128
                for h in range(12):
                    if h < 8:
                        l_, lap, rap = wC, wC.ap[:, h, dc * 128:(dc + 1) * 128], oT.ap[0:64, h, :]
                    else:
                        l_, lap, rap = wD, wD.ap[:, h - 8, dc * 128:(dc + 1) * 128], oT.ap[:, h, :]
                    kb.mm(pb_, pb_.ap[:, c0:c0 + 128], l_, lap, oT, rap, start=(h == 0), stop=(h == 11), acc=not (dc % 4 == 0 and h == 0))

            def y_fn(dc, y0=y0, y1=y1):
                pb_ = y0 if dc < 4 else y1
                return pb_, pb_.ap[:, (dc % 4) * 128:(dc % 4 + 1) * 128]
            ln_epilogue(kb, lc, y_fn, xv[:, :, cs_], d.get("x_reads", []), modT.ap[:, 16:24, 0:1], modT, lng.ap[:, 0, :], lnb.ap[:, 0, :], (lng, lnb),
                        ov[:, :, cs_], scr["x1b"], ps[7])
        kb.P.barrier()


def build_launch2(debug=False, stop=None):
    kb = KB()
    d = _declare_common(kb)
    d.update(_declare_moe(kb))
    d.update({
        "x1T": kb.din("x1T", [1024, 16384]), "ctx1T": kb.din("ctx1T", [1024, 256]),
        "cd_w_in": kb.din("cd_w_in", [1024, 2208]), "cd_w_out": kb.din("cd_w_out", [1024, 1024]),
        "c_w_uq": kb.din("c_w_uq", [384, 768]), "c_w_ukv": kb.din("c_w_ukv", [256, 1024]),
        "c_q_normT": kb.din("c_q_normT", [128, 3]), "c_kv_normT": kb.din("c_kv_normT", [128, 2]),
        "d_lambda": kb.din("d_lambda", [4, 64]), "d_sublnT": kb.din("d_sublnT", [128, 1]),
        "rope64": kb.din("rope64", [128, 2, 16384]), "rope32": kb.din("rope32", [128, 2, 16384]),
    })
    out = T(kb.dout("outT", [1024, 4096]))
    scr = l1_scratch(kb)
    if debug:
        scr["x1b"] = T(kb.dout("x1b_dbg", [1024, 4096]))
    modT = phase_mod(kb, d["w_ada"], d["b_adaT"], d["cT"])
    lng, lnb = load_ln(kb, d["ln_gT"], d["ln_bT"])
    phase_l1_proj(kb, d, modT, scr)
    phase_l1_attn(kb, d, scr)
    phase_l1_out(kb, d, modT, lng, lnb, scr)
    if stop == "attn":
        kb.P.barrier()
        return kb
    ov = out.ap.rearrange("(kc p) t -> p kc t", p=128)
    passes = [list(range(8 * i, 8 * i + 8)) for i in range(4)]
    phase_moe(kb, d, modT, lng, lnb, scr["x1b"], passes, 99, lambda tile: (ov[:, :, tile * 128:(tile + 1) * 128], out))
    kb.P.barrier()
    return kb


def _own_first(r):
    idx = np.arange(S)
    own = idx[r * 4096:(r + 1) * 4096]
    rest = np.concatenate([idx[:r * 4096], idx[(r + 1) * 4096:]])
    return np.concatenate([own, rest])


def launch2_inputs(inp, x1, ctx1):
    maps = []
    moe = _moe_inputs(inp, 1)
    for core in range(8):
        b, r = core // 4, core % 4
        perm = _own_first(r)
        cos64, sin64 = _rope_np(perm, 64)
        cos32, sin32 = _rope_np(perm, 32)
        rope64 = np.stack([np.concatenate([cos64.T, cos64.T], 0), np.concatenate([sin64.T, sin64.T], 0)], axis=1)
        z = np.zeros((64, S), np.float32)
        z2 = np.zeros((32, S), np.float32)
        rope32 = np.stack([np.concatenate([z, cos32.T, z2], 0), np.concatenate([z, sin32.T, z2], 0)], axis=1)
        m = _common_inputs(inp, 1, b)
        m.update(moe)
        m.update({
            "x1T": np.ascontiguousarray(x1[b][perm].T), "ctx1T": np.ascontiguousarray(ctx1[b].T),
            "cd_w_in": np.ascontiguousarray(inp["cd_w_in"][0]), "cd_w_out": np.ascontiguousarray(inp["cd_w_out"][0]),
            "c_w_uq": np.ascontiguousarray(inp["c_w_uq"][0]), "c_w_ukv": np.ascontiguousarray(inp["c_w_ukv"][0]),
            "c_q_normT": _fm(inp["c_q_norm"][0], 3), "c_kv_normT": _fm(inp["c_kv_norm"][0], 2),
            "d_lambda": np.ascontiguousarray(inp["d_lambda"][0]), "d_sublnT": _fm(inp["d_subln"][0], 1),
            "rope64": np.ascontiguousarray(rope64), "rope32": np.ascontiguousarray(rope32),
        })
        maps.append(m)
    return maps


_CACHE = {}


def _kernel2(**inp):
    inp = {k: np.asarray(v) for k, v in inp.items()}
    if "k1" not in _CACHE:
        _CACHE["k1"] = build_launch1()
        _CACHE["k2"] = build_launch2()
    k1, k2 = _CACHE["k1"], _CACHE["k2"]
    res1 = run_bass_kernel_spmd(k1.nc, launch1_inputs(inp), core_ids=list(range(8)))
    x1 = np.empty((2, S, D), np.float32)
    ctx1 = np.empty((2, L, D), np.float32)
    for core in range(8):
        b, r = core // 4, core % 4
        o = res1.results[core]["x2T"]
        x1[b, r * 4096:(r + 1) * 4096] = o[:, 0:4096].T
        if r == 0:
            ctx1[b] = o[:, 4096:4352].T
    res2 = run_bass_kernel_spmd(k2.nc, launch2_inputs(inp, x1, ctx1), core_ids=list(range(8)))
    out = np.empty((2, S, D), np.float32)
    for core in range(8):
        b, r = core // 4, core % 4
        out[b, r * 4096:(r + 1) * 4096] = res2.results[core]["outT"].T
    return out


def build_fused():
    kb = KB()
    c0 = _declare_common(kb, "0")
    c1 = _declare_common(kb, "1")
    m0 = _declare_moe(kb, "0")
    m1 = _declare_moe(kb, "1")
    xT = kb.din("xT", [4, 1024, NEXT])
    biasB = kb.din("biasB", [4, 128, 5, 5, 8, 128])
    maskA = kb.din("maskA", [4, 128, 4, 128])
    ropeT = kb.din("ropeT", [4, 128, 2, NEXT])
    l0 = {"ctxT": kb.din("ctxT", [1024, 256]), "ab_w_in": kb.din("ab_w_in", [1024, 2304]),
          "ab_w_out": kb.din("ab_w_out", [1024, 1024]), "a_sink": kb.din("a_sink", [1, 8])}
    l1 = {
        "cd_w_in": kb.din("cd_w_in", [1024, 2208]), "cd_w_out": kb.din("cd_w_out", [1024, 1024]),
        "c_w_uq": kb.din("c_w_uq", [384, 768]), "c_w_ukv": kb.din("c_w_ukv", [256, 1024]),
        "c_q_normT": kb.din("c_q_normT", [128, 3]), "c_kv_normT": kb.din("c_kv_normT", [128, 2]),
        "d_lambda": kb.din("d_lambda", [4, 64]), "d_sublnT": kb.din("d_sublnT", [128, 1]),
        "rope64": kb.din("rope64", [128, 2, 16384]), "rope32": kb.din("rope32", [128, 2, 16384]),
    }
    out = T(kb.dout("outT", [1024, 4096]))
    x1T = kb.dscr("x1T_s", [1024, 4352], F32)
    x2T = kb.dscr("x2T_s", [1024, S + L], F32)
    x2v = x2T.ap.rearrange("(kc p) t -> p kc t", p=128)
    mod0 = phase_mod(kb, c0["w_ada"], c0["b_adaT"], c0["cT"])
    lng0, lnb0 = load_ln(kb, c0["ln_gT"], c0["ln_bT"])
    for seg in range(4):
        dseg = dict(l0)
        dseg.update({"xT": xT[seg], "biasB": biasB[seg], "maskA": maskA[seg], "ropeT": ropeT[seg]})
        phase_l0_attn(kb, dseg, mod0, lng0, lnb0, x1T, with_ctx=(seg == 0))
        ntile = 34 if seg == 0 else 32
        passes = [list(range(0, 12)), list(range(12, 24)), list(range(24, 34))] if seg == 0 else \
            [list(range(0, 12)), list(range(12, 24)), list(range(24, 32))]

        def out_fn(tile, seg=seg):
            if tile < 32:
                c = seg * 4096 + tile * 128
            else:
                c = S + (tile - 32) * 128
            return x2v[:, :, c:c + 128], x2T
        phase_moe(kb, m0, mod0, lng0, lnb0, x1T, passes, 32, out_fn)
    mod1 = phase_mod(kb, c1["w_ada"], c1["b_adaT"], c1["cT"])
    lng1, lnb1 = load_ln(kb, c1["ln_gT"], c1["ln_bT"])
    scr = l1_scratch(kb)
    l1["x1T"] = x2T.ap[:, 0:S]
    l1["ctx1T"] = x2T.ap[:, S:S + L]
    l1["x_reads"] = [x2T]
    phase_l1_proj(kb, l1, mod1, scr)
    phase_l1_attn(kb, l1, scr)
    phase_l1_out(kb, l1, mod1, lng1, lnb1, scr)
    ov = out.ap.rearrange("(kc p) t -> p kc t", p=128)
    passes = [list(range(0, 12)), list(range(12, 24)), list(range(24, 32))]
    phase_moe(kb, m1, mod1, lng1, lnb1, scr["x1b"], passes, 99, lambda tile: (ov[:, :, tile * 128:(tile + 1) * 128], out))
    kb.P.barrier()
    return kb


def fused_inputs(inp):
    maps = []
    rpb = np.asarray(inp["b_rpb"][0], np.float32)
    tabs = {r: _bias_tables(rpb, r) for r in range(4)}
    masks = {r: _mask_a(r) for r in range(4)}
    moe0 = {k + "0": v for k, v in _moe_inputs(inp, 0).items()}
    moe1 = {k + "1": v for k, v in _moe_inputs(inp, 1).items()}
    ropes0 = {}
    xts = {}
    for r in range(4):
        et = _ext_tokens(r)
        cos, sin = _rope_np(et, 64)
        ropes0[r] = np.stack([np.concatenate([cos.T, cos.T], 0), np.concatenate([sin.T, sin.T], 0)], axis=1)
        for b in range(2):
            xts[(b, r)] = np.ascontiguousarray(inp["x"][b][et].T)
    for core in range(8):
        b, r = core // 4, core % 4
        order = [r] + [q for q in range(4) if q != r]
        perm = _own_first(r)
        cos64, sin64 = _rope_np(perm, 64)
        cos32, sin32 = _rope_np(perm, 32)
        rope64 = np.stack([np.concatenate([cos64.T, cos64.T], 0), np.concatenate([sin64.T, sin64.T], 0)], axis=1)
        z = np.zeros((64, S), np.float32)
        z2 = np.zeros((32, S), np.float32)
        rope32 = np.stack([np.concatenate([z, cos32.T, z2], 0), np.concatenate([z, sin32.T, z2], 0)], axis=1)
        m = {k + "0": v for k, v in _common_inputs(inp, 0, b).items()}
        m.update({k + "1": v for k, v in _common_inputs(inp, 1, b).items()})
        m.update(moe0)
        m.update(moe1)
        m.update({
            "xT": np.stack([xts[(b, q)] for q in order], 0),
            "biasB": np.stack([tabs[q] for q in order], 0),
            "maskA": np.stack([masks[q] for q in order], 0),
            "ropeT": np.stack([ropes0[q] for q in order], 0),
            "ctxT": np.ascontiguousarray(inp["ctx"][b].T),
            "ab_w_in": np.ascontiguousarray(inp["ab_w_in"][0]), "ab_w_out": np.ascontiguousarray(inp["ab_w_out"][0]),
            "a_sink": np.ascontiguousarray(inp["a_sink"][0].reshape(1, 8)),
            "cd_w_in": np.ascontiguousarray(inp["cd_w_in"][0]), "cd_w_out": np.ascontiguousarray(inp["cd_w_out"][0]),
            "c_w_uq": np.ascontiguousarray(inp["c_w_uq"][0]), "c_w_ukv": np.ascontiguousarray(inp["c_w_ukv"][0]),
            "c_q_normT": _fm(inp["c_q_norm"][0], 3), "c_kv_normT": _fm(inp["c_kv_norm"][0], 2),
            "d_lambda": np.ascontiguousarray(inp["d_lambda"][0]), "d_sublnT": _fm(inp["d_subln"][0], 1),
            "rope64": np.ascontiguousarray(rope64), "rope32": np.ascontiguousarray(rope32),
        })
        maps.append(m)
    return maps


def kernel_unfused(**inp):
    return _kernel2(**inp)


def kernel(**inp):
    inp = {k: np.asarray(v) for k, v in inp.items()}
    if "kf" not in _CACHE:
        _CACHE["kf"] = build_fused()
    kf = _CACHE["kf"]
    res = run_bass_kernel_spmd(kf.nc, fused_inputs(inp), core_ids=list(range(8)))
    out = np.empty((2, S, D), np.float32)
    for core in range(8):
        b, r = core // 4, core % 4
        out[b, r * 4096:(r + 1) * 4096] = res.results[core]["outT"].T
    return out
```
